# Optimizing a Trainium2 kernel written in Bass

```python
import jax, jax.numpy as jnp
from jax import lax
import numpy as np

D_MODEL = 2048
BATCH = 2
SEQ = 8192
DEPTH = 4

GRID_W = 64
CTX_LEN = 256
N_MIXERS = 3
NORM_EPS = 1e-6
ROPE_THETA = 10000.0
NEG_INF = -1e30
ADA_CHUNKS = 6

MLA_HEADS = 16
MLA_Q_RANK = 512
MLA_KV_RANK = 512
MLA_NOPE = 128
MLA_ROPE = 64
MLA_V = 128
Q_BLOCK = 128

SSM_D_INNER = 2 * D_MODEL
SSM_HEADDIM = 64
SSM_HEADS = SSM_D_INNER // SSM_HEADDIM
SSM_GROUPS = 8
SSM_STATE = 128
SSM_CONV = 5
SSM_CHUNK = 128
SSM_CONV_DIM = SSM_D_INNER + 2 * SSM_GROUPS * SSM_STATE
SSM_IN_DIM = SSM_D_INNER + SSM_CONV_DIM + 2 * SSM_HEADS

WINDOW = 128
W_BLOCK = WINDOW
WQ_HEADS = 32
WKV_HEADS = 8
W_HEAD_DIM = 64

PEER_HEADS = 8
PEER_N_KEYS = 128
PEER_N_EXPERTS = PEER_N_KEYS * PEER_N_KEYS
PEER_TOPK = 16
PEER_KEY_DIM = 256
PEER_TOKEN_BLOCK = 64

kernel_name = 'hybrid_mla_ssd_swa_peer_dit'


def rms_norm(x, g):
    xf = x.astype(jnp.float32)
    y = xf * lax.rsqrt(jnp.mean(xf * xf, axis=-1, keepdims=True) + NORM_EPS)
    return (y * g.astype(jnp.float32)).astype(x.dtype)


def adaln(cvec, w, b):
    m = jax.nn.silu(cvec) @ w + b
    return jnp.split(m[:, None, :], ADA_CHUNKS, axis=-1)


def modulate(h, shift, scale):
    return h * (1 + scale) + shift


def grid_positions(n_tokens):
    n_rows = n_tokens // GRID_W
    rows = jnp.repeat(jnp.arange(n_rows, dtype=jnp.int32), GRID_W)
    cols = jnp.tile(jnp.arange(GRID_W, dtype=jnp.int32), n_rows)
    return rows, cols


def rope_1d(x, pos):
    half = x.shape[-1] // 2
    freqs = ROPE_THETA ** (-jnp.arange(half, dtype=jnp.float32) / half)
    ang = pos.astype(jnp.float32)[:, None] * freqs
    cos = jnp.cos(ang)[:, None, :].astype(x.dtype)
    sin = jnp.sin(ang)[:, None, :].astype(x.dtype)
    x1, x2 = x[..., :half], x[..., half:]
    return jnp.concatenate([x1 * cos - x2 * sin, x2 * cos + x1 * sin], axis=-1)


def rope_2d(x, rows, cols):
    half = x.shape[-1] // 2
    return jnp.concatenate([rope_1d(x[..., :half], rows), rope_1d(x[..., half:], cols)], axis=-1)


def softmax_attend(q, k, v, scale, mask=None, sink=None):
    B_, Lq, H, d = q.shape
    KH = k.shape[2]
    G = H // KH
    qg = q.reshape(B_, Lq, KH, G, d)
    s = jnp.einsum('bqkgd,bskd->bkgqs', qg, k).astype(jnp.float32) * scale
    if mask is not None:
        s = jnp.where(mask, s, NEG_INF)
    if sink is not None:
        sk = jnp.broadcast_to(sink.astype(jnp.float32).reshape(1, KH, G, 1, 1), s.shape[:-1] + (1,))
        s = jnp.concatenate([s, sk], axis=-1)
    p = jax.nn.softmax(s, axis=-1)
    if sink is not None:
        p = p[..., :-1]
    o = jnp.einsum('bkgqs,bskd->bqkgd', p.astype(v.dtype), v)
    return o.reshape(B_, Lq, H, v.shape[-1])


def sweep_query_blocks(q, k, v, scale):
    B_, L, H, d = q.shape
    nb = L // Q_BLOCK
    qb = jnp.moveaxis(q.reshape(B_, nb, Q_BLOCK, H, d), 1, 0)
    o = lax.map(lambda t: softmax_attend(t, k, v, scale), qb)
    return jnp.moveaxis(o, 0, 1).reshape(B_, L, H, v.shape[-1])


def mla_mixer(h_lat, h_ctx, w_a, q_g, kv_g, w_uq, w_ukv, w_o, rows, cols, need_ctx):
    scale = (MLA_NOPE + MLA_ROPE) ** -0.5

    def compress(h):
        return jnp.split(h @ w_a, [MLA_Q_RANK, MLA_Q_RANK + MLA_KV_RANK], axis=-1)

    def queries(c_q, rotate):
        B_, L, _ = c_q.shape
        q = (rms_norm(c_q, q_g) @ w_uq).reshape(B_, L, MLA_HEADS, MLA_NOPE + MLA_ROPE)
        q_nope, q_rope = jnp.split(q, [MLA_NOPE], axis=-1)
        if rotate:
            q_rope = rope_2d(q_rope, rows, cols)
        return jnp.concatenate([q_nope, q_rope], axis=-1)

    def keys_values(c_kv, k_r, rotate):
        B_, L, _ = c_kv.shape
        kv = (rms_norm(c_kv, kv_g) @ w_ukv).reshape(B_, L, MLA_HEADS, MLA_NOPE + MLA_V)
        k_nope, v = jnp.split(kv, [MLA_NOPE], axis=-1)
        k_rope = k_r[:, :, None, :]
        if rotate:
            k_rope = rope_2d(k_rope, rows, cols)
        k = jnp.concatenate([k_nope, jnp.broadcast_to(k_rope, (B_, L, MLA_HEADS, MLA_ROPE))], axis=-1)
        return k, v

    cq_l, ckv_l, kr_l = compress(h_lat)
    cq_c, ckv_c, kr_c = compress(h_ctx)
    q_l = queries(cq_l, True)
    k_l, v_l = keys_values(ckv_l, kr_l, True)
    k_c, v_c = keys_values(ckv_c, kr_c, False)
    k_all = jnp.concatenate([k_l, k_c], axis=1)
    v_all = jnp.concatenate([v_l, v_c], axis=1)
    o_l = sweep_query_blocks(q_l, k_all, v_all, scale)
    B_, L = h_lat.shape[:2]
    out_l = o_l.reshape(B_, L, MLA_HEADS * MLA_V) @ w_o
    out_c = None
    if need_ctx:
        o_c = softmax_attend(queries(cq_c, False), k_c, v_c, scale)
        out_c = o_c.reshape(B_, h_ctx.shape[1], MLA_HEADS * MLA_V) @ w_o
    return out_l, out_c


def depthwise_conv_centred(x, w, b):
    ch = x.shape[-1]
    pad = (w.shape[0] - 1) // 2
    y = lax.conv_general_dilated(x, w[:, None, :].astype(x.dtype), (1,), [(pad, pad)],
                                 dimension_numbers=('NWC', 'WIO', 'NWC'), feature_group_count=ch)
    return y + b


def ssd_scan(xs, dt, A, bm, cm, h0):
    B_, L, H, P = xs.shape
    G = bm.shape[2]
    HG = H // G
    Q = SSM_CHUNK
    nc = L // Q

    def chunks(t):
        return jnp.moveaxis(t.reshape((B_, nc, Q) + t.shape[2:]), 1, 0)

    x_c = chunks(xs.reshape(B_, L, G, HG, P))
    dt_c = chunks(dt.reshape(B_, L, G, HG))
    b_c = chunks(bm)
    c_c = chunks(cm)
    A_g = A.reshape(G, HG)
    tri = jnp.tril(jnp.ones((Q, Q), dtype=bool))[None, :, :, None, None]

    def step(h, inp):
        xq, dtq, bq, cq = inp
        acs = jnp.cumsum(dtq * A_g, axis=1)
        seg = acs[:, :, None] - acs[:, None, :]
        decay = jnp.exp(jnp.where(tri, seg, -jnp.inf))
        cb = jnp.einsum('bign,bjgn->bijg', cq, bq)
        y_diag = jnp.einsum('bijg,bijgh,bjgh,bjghp->bighp', cb, decay, dtq, xq)
        y_off = jnp.einsum('bign,bghpn,bigh->bighp', cq, h, jnp.exp(acs))
        last = acs[:, -1]
        w_state = jnp.exp(last[:, None] - acs) * dtq
        h_new = jnp.exp(last)[..., None, None] * h + jnp.einsum('bjgn,bjgh,bjghp->bghpn', bq, w_state, xq)
        return h_new, y_diag + y_off

    h_fin, y = lax.scan(step, h0, (x_c, dt_c, b_c, c_c))
    y = jnp.moveaxis(y, 0, 1).reshape(B_, L, H, P)
    return y, h_fin


def ssd_inputs(h, w_in, conv_w, conv_b, dt_bias):
    B_, L, _ = h.shape
    z, xbc, dt_raw = jnp.split(h @ w_in, [SSM_D_INNER, SSM_D_INNER + SSM_CONV_DIM], axis=-1)
    xbc = jax.nn.silu(depthwise_conv_centred(xbc, conv_w, conv_b))
    xs, bm, cm = jnp.split(xbc, [SSM_D_INNER, SSM_D_INNER + SSM_GROUPS * SSM_STATE], axis=-1)
    xs = xs.reshape(B_, L, SSM_HEADS, SSM_HEADDIM)
    bm = bm.reshape(B_, L, SSM_GROUPS, SSM_STATE)
    cm = cm.reshape(B_, L, SSM_GROUPS, SSM_STATE)
    dt = jax.nn.softplus(dt_raw.astype(jnp.float32).reshape(B_, L, 2, SSM_HEADS) + dt_bias.astype(jnp.float32))
    return z, xs, bm, cm, dt


def bidirectional_ssd(xs, bm, cm, dt, A, init_f, init_b):
    flip = lambda t: jnp.flip(t, axis=1)
    y_f, s_f = ssd_scan(xs, dt[:, :, 0], A[0], bm, cm, init_f)
    y_b, s_b = ssd_scan(flip(xs), flip(dt[:, :, 1]), A[1], flip(bm), flip(cm), init_b)
    return y_f + flip(y_b), s_f, s_b


def ssd_mixer(h_lat, h_ctx, w_in, conv_w, conv_b, a_log, dt_bias, d_skip, norm_g, w_out, need_ctx):
    A = -jnp.exp(a_log.astype(jnp.float32))
    z_c, x_c, b_c, c_c, dt_c = ssd_inputs(h_ctx, w_in, conv_w, conv_b, dt_bias)
    z_l, x_l, b_l, c_l, dt_l = ssd_inputs(h_lat, w_in, conv_w, conv_b, dt_bias)
    B_ = h_lat.shape[0]
    h0 = jnp.zeros((B_, SSM_GROUPS, SSM_HEADS // SSM_GROUPS, SSM_HEADDIM, SSM_STATE), jnp.float32)
    y_c, s_f, s_b = bidirectional_ssd(x_c, b_c, c_c, dt_c, A, h0, h0)
    y_l, _, _ = bidirectional_ssd(x_l, b_l, c_l, dt_l, A, s_f, s_b)

    def finish(y, xs, z):
        y = y + d_skip.astype(jnp.float32)[:, None] * xs
        y = y.reshape(z.shape[0], z.shape[1], SSM_D_INNER).astype(z.dtype)
        return rms_norm(y * jax.nn.silu(z), norm_g) @ w_out

    out_l = finish(y_l, x_l, z_l)
    out_c = finish(y_c, x_c, z_c) if need_ctx else None
    return out_l, out_c


def banded_window_attention(q, k, v, k_ctx, v_ctx, sinks, scale):
    B_, L, H, d = q.shape
    nb = L // W_BLOCK
    pad = ((0, 0), (W_BLOCK, W_BLOCK), (0, 0), (0, 0))
    kp = jnp.pad(k, pad)
    vp = jnp.pad(v, pad)
    q_off = jnp.arange(W_BLOCK)
    k_off = jnp.arange(3 * W_BLOCK)
    ctx_mask = jnp.ones((W_BLOCK, k_ctx.shape[1]), dtype=bool)

    def one_block(i):
        start = i * W_BLOCK
        qb = lax.dynamic_slice_in_dim(q, start, W_BLOCK, axis=1)
        kb = lax.dynamic_slice_in_dim(kp, start, 3 * W_BLOCK, axis=1)
        vb = lax.dynamic_slice_in_dim(vp, start, 3 * W_BLOCK, axis=1)
        qpos = start + q_off
        kpos = start - W_BLOCK + k_off
        band = (jnp.abs(qpos[:, None] - kpos[None, :]) <= WINDOW) & ((kpos >= 0) & (kpos < L))[None, :]
        mask = jnp.concatenate([band, ctx_mask], axis=1)
        return softmax_attend(qb, jnp.concatenate([kb, k_ctx], axis=1), jnp.concatenate([vb, v_ctx], axis=1),
                              scale, mask=mask, sink=sinks)

    o = lax.map(one_block, jnp.arange(nb))
    return jnp.moveaxis(o, 0, 1).reshape(B_, L, H, v.shape[-1])


def window_mixer(h_lat, h_ctx, w_qkv, sinks, w_o, rows, cols, need_ctx):
    scale = W_HEAD_DIM ** -0.5

    def project(h):
        B_, L, _ = h.shape
        q, k, v = jnp.split(h @ w_qkv, [WQ_HEADS * W_HEAD_DIM, (WQ_HEADS + WKV_HEADS) * W_HEAD_DIM], axis=-1)
        return (q.reshape(B_, L, WQ_HEADS, W_HEAD_DIM), k.reshape(B_, L, WKV_HEADS, W_HEAD_DIM),
                v.reshape(B_, L, WKV_HEADS, W_HEAD_DIM))

    q_l, k_l, v_l = project(h_lat)
    q_l = rope_2d(q_l, rows, cols)
    k_l = rope_2d(k_l, rows, cols)
    q_c, k_c, v_c = project(h_ctx)
    o_l = banded_window_attention(q_l, k_l, v_l, k_c, v_c, sinks, scale)
    B_, L = h_lat.shape[:2]
    out_l = o_l.reshape(B_, L, WQ_HEADS * W_HEAD_DIM) @ w_o
    out_c = None
    if need_ctx:
        o_c = softmax_attend(q_c, k_c, v_c, scale, sink=sinks)
        out_c = o_c.reshape(B_, h_ctx.shape[1], WQ_HEADS * W_HEAD_DIM) @ w_o
    return out_l, out_c


def peer_ffn(h, w_q, k1, k2, u, v):
    B_, L, D = h.shape
    T = B_ * L
    hf = h.reshape(T, D)
    q = (hf @ w_q).reshape(T, PEER_HEADS, 2, PEER_KEY_DIM // 2)
    s1 = jnp.einsum('thd,hkd->thk', q[:, :, 0], k1).astype(jnp.float32)
    s2 = jnp.einsum('thd,hkd->thk', q[:, :, 1], k2).astype(jnp.float32)
    v1, i1 = lax.top_k(s1, PEER_TOPK)
    v2, i2 = lax.top_k(s2, PEER_TOPK)
    n_cand = PEER_TOPK * PEER_TOPK
    cand_s = (v1[..., :, None] + v2[..., None, :]).reshape(T, PEER_HEADS, n_cand)
    cand_i = (i1[..., :, None] * PEER_N_KEYS + i2[..., None, :]).reshape(T, PEER_HEADS, n_cand)
    top_s, pos = lax.top_k(cand_s, PEER_TOPK)
    experts = jnp.take_along_axis(cand_i, pos, axis=-1)
    gates = jax.nn.softmax(top_s, axis=-1).astype(h.dtype)
    nb = T // PEER_TOKEN_BLOCK

    def block(args):
        hb, eb, gb = args
        act = jax.nn.gelu(jnp.einsum('thkd,td->thk', u[eb], hb))
        return jnp.einsum('thk,thkd->td', gb * act, v[eb])

    out = lax.map(block, (hf.reshape(nb, PEER_TOKEN_BLOCK, D),
                          experts.reshape(nb, PEER_TOKEN_BLOCK, PEER_HEADS, PEER_TOPK),
                          gates.reshape(nb, PEER_TOKEN_BLOCK, PEER_HEADS, PEER_TOPK)))
    return out.reshape(B_, L, D)


def setup_inputs(seed: int = 0) -> dict:
    key = jax.random.key(seed)
    ks = iter(jax.random.split(key, 64))
    n_mla = (DEPTH + 2) // N_MIXERS
    n_ssd = (DEPTH + 1) // N_MIXERS
    n_win = DEPTH // N_MIXERS
    f32 = jnp.float32

    def nrm(shape, fan_in, g=1.0):
        return jax.random.normal(next(ks), shape, f32) * (g * fan_in ** -0.5)

    def gain(shape):
        return 1.0 + 0.05 * jax.random.normal(next(ks), shape, f32)

    def small(shape, s):
        return s * jax.random.normal(next(ks), shape, f32)

    dt0 = jnp.exp(jax.random.uniform(next(ks), (n_ssd, 2, SSM_HEADS), f32, jnp.log(1e-3), jnp.log(1e-1)))
    return {
        'x': jax.random.normal(next(ks), (BATCH, SEQ, D_MODEL), f32),
        'c': jax.random.normal(next(ks), (BATCH, D_MODEL), f32),
        'ctx': jax.random.normal(next(ks), (BATCH, CTX_LEN, D_MODEL), f32),
        'c_ctx': jax.random.normal(next(ks), (D_MODEL,), f32),
        'ada_w': nrm((DEPTH, D_MODEL, ADA_CHUNKS * D_MODEL), D_MODEL, 0.5),
        'ada_b': small((DEPTH, ADA_CHUNKS * D_MODEL), 0.02),
        'norm_mix_g': gain((DEPTH, D_MODEL)),
        'norm_ffn_g': gain((DEPTH, D_MODEL)),
        'mla_w_a': nrm((n_mla, D_MODEL, MLA_Q_RANK + MLA_KV_RANK + MLA_ROPE), D_MODEL),
        'mla_q_norm_g': gain((n_mla, MLA_Q_RANK)),
        'mla_kv_norm_g': gain((n_mla, MLA_KV_RANK)),
        'mla_w_uq': nrm((n_mla, MLA_Q_RANK, MLA_HEADS * (MLA_NOPE + MLA_ROPE)), MLA_Q_RANK),
        'mla_w_ukv': nrm((n_mla, MLA_KV_RANK, MLA_HEADS * (MLA_NOPE + MLA_V)), MLA_KV_RANK),
        'mla_w_o': nrm((n_mla, MLA_HEADS * MLA_V, D_MODEL), MLA_HEADS * MLA_V),
        'ssd_w_in': nrm((n_ssd, D_MODEL, SSM_IN_DIM), D_MODEL),
        'ssd_conv_w': nrm((n_ssd, SSM_CONV, SSM_CONV_DIM), SSM_CONV),
        'ssd_conv_b': small((n_ssd, SSM_CONV_DIM), 0.02),
        'ssd_a_log': jnp.log(jax.random.uniform(next(ks), (n_ssd, 2, SSM_HEADS), f32, 1.0, 16.0)),
        'ssd_dt_bias': dt0 + jnp.log(-jnp.expm1(-dt0)),
        'ssd_d_skip': gain((n_ssd, SSM_HEADS)),
        'ssd_norm_g': gain((n_ssd, SSM_D_INNER)),
        'ssd_w_out': nrm((n_ssd, SSM_D_INNER, D_MODEL), SSM_D_INNER),
        'win_w_qkv': nrm((n_win, D_MODEL, (WQ_HEADS + 2 * WKV_HEADS) * W_HEAD_DIM), D_MODEL),
        'win_sinks': small((n_win, WQ_HEADS), 0.5),
        'win_w_o': nrm((n_win, WQ_HEADS * W_HEAD_DIM, D_MODEL), WQ_HEADS * W_HEAD_DIM),
        'peer_w_q': nrm((DEPTH, D_MODEL, PEER_HEADS * PEER_KEY_DIM), D_MODEL),
        'peer_k1': nrm((DEPTH, PEER_HEADS, PEER_N_KEYS, PEER_KEY_DIM // 2), PEER_KEY_DIM // 2),
        'peer_k2': nrm((DEPTH, PEER_HEADS, PEER_N_KEYS, PEER_KEY_DIM // 2), PEER_KEY_DIM // 2),
        'peer_u': nrm((DEPTH, PEER_N_EXPERTS, D_MODEL), D_MODEL),
        'peer_v': nrm((DEPTH, PEER_N_EXPERTS, D_MODEL), PEER_HEADS),
        'final_norm_g': gain((D_MODEL,)),
    }


def reference(x, c, ctx, c_ctx, ada_w, ada_b, norm_mix_g, norm_ffn_g, mla_w_a, mla_q_norm_g, mla_kv_norm_g,
              mla_w_uq, mla_w_ukv, mla_w_o, ssd_w_in, ssd_conv_w, ssd_conv_b, ssd_a_log, ssd_dt_bias, ssd_d_skip,
              ssd_norm_g, ssd_w_out, win_w_qkv, win_sinks, win_w_o, peer_w_q, peer_k1, peer_k2, peer_u, peer_v,
              final_norm_g):
    rows, cols = grid_positions(x.shape[1])
    for i in range(DEPTH):
        need_ctx = i < DEPTH - 1
        sh1, sc1, g1, sh2, sc2, g2 = adaln(c, ada_w[i], ada_b[i])
        csh1, csc1, cg1, csh2, csc2, cg2 = adaln(c_ctx[None, :], ada_w[i], ada_b[i])
        h_l = modulate(rms_norm(x, norm_mix_g[i]), sh1, sc1)
        h_c = modulate(rms_norm(ctx, norm_mix_g[i]), csh1, csc1)
        kind, j = i % N_MIXERS, i // N_MIXERS
        if kind == 0:
            mix_l, mix_c = mla_mixer(h_l, h_c, mla_w_a[j], mla_q_norm_g[j], mla_kv_norm_g[j], mla_w_uq[j],
                                     mla_w_ukv[j], mla_w_o[j], rows, cols, need_ctx)
        elif kind == 1:
            mix_l, mix_c = ssd_mixer(h_l, h_c, ssd_w_in[j], ssd_conv_w[j], ssd_conv_b[j], ssd_a_log[j],
                                     ssd_dt_bias[j], ssd_d_skip[j], ssd_norm_g[j], ssd_w_out[j], need_ctx)
        else:
            mix_l, mix_c = window_mixer(h_l, h_c, win_w_qkv[j], win_sinks[j], win_w_o[j], rows, cols, need_ctx)
        x = x + g1 * mix_l
        x = x + g2 * peer_ffn(modulate(rms_norm(x, norm_ffn_g[i]), sh2, sc2),
                              peer_w_q[i], peer_k1[i], peer_k2[i], peer_u[i], peer_v[i])
        if need_ctx:
            ctx = ctx + cg1 * mix_c
            ctx = ctx + cg2 * peer_ffn(modulate(rms_norm(ctx, norm_ffn_g[i]), csh2, csc2),
                                       peer_w_q[i], peer_k1[i], peer_k2[i], peer_u[i], peer_v[i])
    return rms_norm(x, final_norm_g)
```

```python
import numpy as np
from contextlib import ExitStack
import concourse.bass as bass
import concourse.mybir as mybir

F32 = mybir.dt.float32
F32R = mybir.dt.float32r
BF16 = mybir.dt.bfloat16
U32 = mybir.dt.uint32
AF = mybir.ActivationFunctionType
ALU = mybir.AluOpType
AX = mybir.AxisListType

ENGS = ["tensor", "vector", "scalar", "gpsimd", "sync"]
NDMA_SEMS = 12


class Prog:
    def __init__(self):
        self.nc = bass.Bass("TRN2", target_bir_lowering=False)
        self.es = ExitStack()
        self.ops = {e: [] for e in ENGS}
        self.cnt = {e: 0 for e in ENGS}
        self.sems = {}
        for e in ENGS:
            self.sems["c_" + e] = self.es.enter_context(self.nc.semaphore("c_" + e))
        self.dma_sems = {}
        self.dma_n = {}
        for q in ["sync", "gpsimd", "scalar"]:
            self.dma_sems[q] = [self.es.enter_context(self.nc.semaphore(f"d_{q}{i}")) for i in range(NDMA_SEMS)]
            self.dma_n[q] = 0
        self.lastw = {}
        self.readers = {}
        self.waited = {e: {} for e in ENGS}
        self.semobj = {}
        for k, v in self.sems.items():
            self.semobj[k] = v
        for q, lst in self.dma_sems.items():
            for i, s in enumerate(lst):
                self.semobj[f"d_{q}{i}"] = s
        self.ntiles = 0
        self.sb_bytes = 0
        self.cur = self.es
        self.scopes = []

    def sb(self, shape, dt=F32, name=None):
        self.ntiles += 1
        name = (name or "t") + f"_{self.ntiles}"
        per = int(np.prod(shape[1:])) * (2 if dt == BF16 else 4)
        self.sb_bytes += per
        return self.cur.enter_context(self.nc.sbuf_tensor(name, list(shape), dt))

    def ps(self, shape, dt=F32, name=None):
        self.ntiles += 1
        name = (name or "p") + f"_{self.ntiles}"
        return self.cur.enter_context(self.nc.psum_tensor(name, list(shape), dt))

    def push_scope(self):
        self.scopes.append((self.cur, self.sb_bytes))
        self.cur = ExitStack()

    def pop_scope(self):
        self.barrier()
        self.cur.close()
        self.cur, self.sb_bytes = self.scopes.pop()

    def barrier(self):
        evs = []
        for e in ENGS:
            if self.cnt[e] > 0:
                evs.append(("c_" + e, self.cnt[e], e))
        for q in self.dma_sems:
            n = self.dma_n[q]
            for si in range(NDMA_SEMS):
                k = (n - 1 - si) // NDMA_SEMS + 1 if n > si else 0
                if k > 0:
                    evs.append((f"d_{q}{si}", 16 * k, "dma"))
        for e in ENGS:
            waits = []
            for ev in evs:
                self._need_force(e, ev, waits)
            if waits:
                self.ops[e].append((waits, None, None))

    def dram(self, name, shape, dt=F32, kind="Internal"):
        return self.nc.dram_tensor(name, list(shape), dt, kind=kind).ap()

    def _need(self, eng, ev, waits):
        if ev is None:
            return
        sem, val, src = ev
        if src == "tensor" and eng == "tensor":
            return
        if self.waited[eng].get(sem, 0) >= val:
            return
        self.waited[eng][sem] = val
        waits.append((sem, val))

    def _need_force(self, eng, ev, waits):
        sem, val, src = ev
        if self.waited[eng].get(sem, 0) >= val:
            return
        self.waited[eng][sem] = val
        waits.append((sem, val))

    def op(self, eng, fn, reads=(), writes=()):
        waits = []
        for k in reads:
            self._need(eng, self.lastw.get(k), waits)
        for k in writes:
            self._need(eng, self.lastw.get(k), waits)
            for ev in self.readers.get(k, ()):
                self._need(eng, ev, waits)
        self.cnt[eng] += 1
        ev = ("c_" + eng, self.cnt[eng], eng)
        for k in reads:
            self.readers.setdefault(k, []).append(ev)
        for k in writes:
            self.lastw[k] = ev
            self.readers[k] = []
        self.ops[eng].append((waits, fn, ("c_" + eng, 1)))
        return ev

    def dma(self, q, out, in_, reads=(), writes=(), **kw):
        waits = []
        n = self.dma_n[q]
        self.dma_n[q] += 1
        si = n % NDMA_SEMS
        sem = f"d_{q}{si}"
        rnd = n // NDMA_SEMS
        if rnd > 0:
            self._need(q, (sem, 16 * rnd, "dma"), waits)
        for k in reads:
            self._need(q, self.lastw.get(k), waits)
        for k in writes:
            self._need(q, self.lastw.get(k), waits)
            for ev in self.readers.get(k, ()):
                self._need(q, ev, waits)
        ev = (sem, 16 * (rnd + 1), "dma")
        for k in reads:
            self.readers.setdefault(k, []).append(ev)
        for k in writes:
            self.lastw[k] = ev
            self.readers[k] = []
        fn = lambda e, out=out, in_=in_, kw=kw: e.dma_start(out=out, in_=in_, **kw)
        self.ops[q].append((waits, fn, (sem, 16)))
        return ev

    def finish(self, final_events):
        waits = []
        for ev in final_events:
            self._need("sync", ev, waits)
        self.ops["sync"].append((waits, None, None))

    def emit(self):
        nc = self.nc
        with nc.Block() as block:
            def mk(engname):
                def body(e):
                    for waits, fn, inc in self.ops[engname]:
                        for sem, val in waits:
                            e.wait_ge(self.semobj[sem], val)
                        if fn is not None:
                            ins = fn(e)
                            ins.then_inc(self.semobj[inc[0]], inc[1])
                return body
            block.tensor(mk("tensor"))
            block.vector(mk("vector"))
            block.scalar(mk("scalar"))
            block.gpsimd(mk("gpsimd"))
            block.sync(mk("sync"))
        self.es.close()
        return nc


D = 2048
NCH = 16
NEXP = 16384
EB = 256
KPB = EB // 128
NEB = NEXP // EB


def emit_consts(P):
    C = {}
    C["ident_f"] = P.sb([128, 128], F32, "ident_f")
    C["ident_b"] = P.sb([128, 128], BF16, "ident_b")
    C["ones_b"] = P.sb([128, 128], BF16, "ones_b")
    ident_d = P.dram("ident_in", [128, 128], F32, kind="ExternalInput")
    P.dma("sync", C["ident_f"][:], ident_d, writes=["ident_f"])
    P.op("vector", lambda e: e.tensor_copy(out=C["ident_b"][:], in_=C["ident_f"][:]), reads=["ident_f"], writes=["ident_b"])
    P.op("vector", lambda e: e.memset(C["ones_b"][:], 1.0), writes=["ones_b"])
    return C


def emit_peer(P, C, xin, xout, tile_kinds, modv, normg, wq_d, k1T_d, k2T_d, uT_d, v_d, pfx="pe"):
    nc = P.nc
    nt = len(tile_kinds)
    xin_v = xin.rearrange("(c p) t -> p c t", p=128)
    xout_v = xout.rearrange("(c p) t -> p c t", p=128)
    wq_v = wq_d.rearrange("(c p) n -> p c n", p=128)
    uT_v = uT_d.rearrange("(c p) e -> p c e", p=128)
    v_v = v_d.rearrange("(b k p) d -> b p k d", p=128, k=KPB)

    modA = P.sb([128, 2, NCH], F32, pfx + "modA")
    for s in range(2):
        P.op("vector", lambda e, s=s: e.scalar_tensor_tensor(
            out=modA[:, s, :], in0=modv[:, 64:80, s], scalar=1.0, in1=normg[:, :], op0=ALU.add, op1=ALU.mult),
            reads=["modv", "normg"], writes=[pfx + "modA"])
    k1T = P.sb([128, 8, 128], BF16, pfx + "k1T")
    k2T = P.sb([128, 8, 128], BF16, pfx + "k2T")
    P.dma("gpsimd", k1T[:], k1T_d.rearrange("h d k -> d h k"), writes=[pfx + "k1T"])
    P.dma("gpsimd", k2T[:], k2T_d.rearrange("h d k -> d h k"), writes=[pfx + "k2T"])

    x_t = [P.sb([128, NCH, 128], F32, f"{pfx}x{i}") for i in range(2)]
    sq = P.sb([128, NCH, 128], BF16, pfx + "sq")
    xo1 = P.sb([128, NCH, 128], F32, pfx + "xo")
    tmp = xo1
    rstd = P.sb([128, 128], F32, pfx + "rstd")
    hT = P.sb([128, NCH, 128], BF16, pfx + "hT")
    wq_t = [P.sb([128, NCH, 256], BF16, f"{pfx}wq{i}") for i in range(2)]
    qT = P.sb([128, 16, 128], BF16, pfx + "qT")
    S = P.sb([128, 16, 128], F32, pfx + "S")
    E = P.sb([128, 16, 128], F32, pfx + "E")
    t16 = P.sb([128, 16, 16], F32, pfx + "t16")
    e16 = P.sb([128, 16, 16], F32, pfx + "e16")
    negm = P.sb([128, 16], F32, pfx + "negm")
    mr = P.sb([128, 128], F32, pfx + "mr")
    cand = P.sb([128, 256], F32, pfx + "cand")
    cand2 = P.sb([128, 256], F32, pfx + "cand2")
    c16 = P.sb([128, 8, 16], F32, pfx + "c16")
    Zs = P.sb([128, 8], F32, pfx + "Zs")
    rZ = P.sb([128, 8], F32, pfx + "rZ")
    Pp = [P.sb([128, 32, 128], F32, f"{pfx}Pp0")]
    G = P.sb([128, NEXP], BF16, pfx + "G")
    u_t = [P.sb([128, NCH, EB], BF16, f"{pfx}u{i}") for i in range(2)]
    v_t = [P.sb([128, KPB, D], BF16, f"{pfx}v{i}") for i in range(2)]
    gl = [P.sb([128, EB], F32, f"{pfx}gl{i}") for i in range(2)]
    Wb = [P.sb([128, EB], BF16, f"{pfx}Wb{i}") for i in range(2)]
    WT = [P.sb([128, KPB, 128], BF16, f"{pfx}WT{i}") for i in range(2)]
    fo = P.sb([128, D], F32, pfx + "fo")
    out_ps = [P.ps([128, 512], F32, f"{pfx}ops{i}") for i in range(4)]
    a_ps = [P.ps([128, 512], F32, f"{pfx}aps{i}") for i in range(2)]
    t_ps = P.ps([128, KPB, 128], BF16, pfx + "tps")
    m_ps = P.ps([128, 512], F32, pfx + "mps")

    ublk = 0
    for t in range(nt):
        s = tile_kinds[t]
        xb = x_t[t % 2]; xk = f"{pfx}x{t%2}"
        P.dma("sync", xb[:], xin_v[:, :, t * 128:(t + 1) * 128], writes=[xk])
        P.op("scalar", lambda e, xb=xb: e.activation(out=sq[:], in_=xb[:], func=AF.Square), reads=[xk], writes=[pfx + "sq"])
        for c in range(NCH):
            P.op("tensor", lambda e, c=c: e.matmul(m_ps[:, 0:128], C["ones_b"][:], sq[:, c, :], start=(c == 0), stop=(c == NCH - 1)),
                 reads=[pfx + "sq", "ones_b"], writes=[pfx + "mps"])
        P.op("vector", lambda e: e.tensor_scalar(out=rstd[:], in0=m_ps[:, 0:128], scalar1=1.0 / D, scalar2=1e-6, op0=ALU.mult, op1=ALU.add),
             reads=[pfx + "mps"], writes=[pfx + "rstd"])
        P.op("scalar", lambda e: e.activation(out=rstd[:], in_=rstd[:], func=AF.Sqrt), reads=[pfx + "rstd"], writes=[pfx + "rstd"])
        P.op("vector", lambda e: e.reciprocal(out=rstd[:], in_=rstd[:]), reads=[pfx + "rstd"], writes=[pfx + "rstd"])
        P.op("vector", lambda e, xb=xb: e.tensor_tensor(out=tmp[:], in0=xb[:], in1=rstd[:].unsqueeze(1).to_broadcast([128, NCH, 128]), op=ALU.mult),
             reads=[xk, pfx + "rstd"], writes=[pfx + "tmp"])
        P.op("vector", lambda e, s=s: e.tensor_tensor(out=tmp[:], in0=tmp[:], in1=modA[:, s, :].unsqueeze(2).to_broadcast([128, NCH, 128]), op=ALU.mult),
             reads=[pfx + "tmp", pfx + "modA"], writes=[pfx + "tmp"])
        P.op("vector", lambda e, s=s: e.tensor_tensor(out=hT[:], in0=tmp[:], in1=modv[:, 48:64, s].unsqueeze(2).to_broadcast([128, NCH, 128]), op=ALU.add),
             reads=[pfx + "tmp", "modv"], writes=[pfx + "hT"])
        for nb in range(8):
            wb = wq_t[nb % 2]; wk = f"{pfx}wq{nb%2}"
            P.dma("gpsimd", wb[:], wq_v[:, :, nb * 256:(nb + 1) * 256], writes=[wk])
            for jj in range(2):
                for c in range(NCH):
                    P.op("tensor", lambda e, wb=wb, jj=jj, c=c: e.matmul(m_ps[:, jj * 128:(jj + 1) * 128], wb[:, c, jj * 128:(jj + 1) * 128], hT[:, c, :],
                                                                       start=(c == 0), stop=(c == NCH - 1)),
                         reads=[wk, pfx + "hT"], writes=[pfx + "mps"])
            P.op("scalar", lambda e, nb=nb: e.copy(out=qT[:, nb * 2:(nb + 1) * 2, :], in_=m_ps[:, 0:256].rearrange("p (j t) -> p j t", j=2)),
                 reads=[pfx + "mps"], writes=[pfx + "qT"])
        for jb in range(4):
            for jj in range(4):
                j = jb * 4 + jj
                h, side = j // 2, j % 2
                kT = k1T if side == 0 else k2T
                P.op("tensor", lambda e, j=j, jj=jj, kT=kT, h=h: e.matmul(m_ps[:, jj * 128:(jj + 1) * 128], qT[:, j, :], kT[:, h, :], start=True, stop=True),
                     reads=[pfx + "qT", pfx + "k1T", pfx + "k2T"], writes=[pfx + "mps"])
            P.op("scalar", lambda e, jb=jb: e.copy(out=S[:, jb * 4:(jb + 1) * 4, :], in_=m_ps[:].rearrange("p (j t) -> p j t", j=4)),
                 reads=[pfx + "mps"], writes=[pfx + "S"])
        for j in range(16):
            P.op("vector", lambda e, j=j: e.max(out=t16[:, j, 0:8], in_=S[:, j, :]), reads=[pfx + "S"], writes=[pfx + "t16"])
            P.op("vector", lambda e, j=j: e.match_replace(out=mr[:], in_to_replace=t16[:, j, 0:8], in_values=S[:, j, :], imm_value=-1e30),
                 reads=[pfx + "S", pfx + "t16"], writes=[pfx + "mr"])
            P.op("vector", lambda e, j=j: e.max(out=t16[:, j, 8:16], in_=mr[:]), reads=[pfx + "mr"], writes=[pfx + "t16"])
        P.op("vector", lambda e: e.tensor_scalar(out=negm[:], in0=t16[:, :, 0], scalar1=-1.0, scalar2=None, op0=ALU.mult),
             reads=[pfx + "t16"], writes=[pfx + "negm"])
        for j in range(16):
            P.op("scalar", lambda e, j=j: e.activation(out=E[:, j, :], in_=S[:, j, :], func=AF.Exp, bias=negm[:, j:j + 1], scale=1.0),
                 reads=[pfx + "S", pfx + "negm"], writes=[pfx + "E"])
            P.op("scalar", lambda e, j=j: e.activation(out=e16[:, j, :], in_=t16[:, j, :], func=AF.Exp, bias=negm[:, j:j + 1], scale=1.0),
                 reads=[pfx + "t16", pfx + "negm"], writes=[pfx + "e16"])
        for h in range(8):
            P.op("vector", lambda e, h=h: e.tensor_tensor(out=cand[:].rearrange("p (a b) -> p a b", a=16),
                                                        in0=e16[:, 2 * h, :].unsqueeze(2).to_broadcast([128, 16, 16]),
                                                        in1=e16[:, 2 * h + 1, :].unsqueeze(1).to_broadcast([128, 16, 16]), op=ALU.mult),
                 reads=[pfx + "e16"], writes=[pfx + "cand"])
            P.op("vector", lambda e, h=h: e.max(out=c16[:, h, 0:8], in_=cand[:]), reads=[pfx + "cand"], writes=[pfx + "c16"])
            P.op("vector", lambda e, h=h: e.match_replace(out=cand2[:], in_to_replace=c16[:, h, 0:8], in_values=cand[:], imm_value=-1.0),
                 reads=[pfx + "cand", pfx + "c16"], writes=[pfx + "cand2"])
            P.op("vector", lambda e, h=h: e.max(out=c16[:, h, 8:16], in_=cand2[:]), reads=[pfx + "cand2"], writes=[pfx + "c16"])
        P.op("vector", lambda e: e.tensor_reduce(out=Zs[:], in_=c16[:], axis=AX.X, op=ALU.add), reads=[pfx + "c16"], writes=[pfx + "Zs"])
        P.op("vector", lambda e: e.reciprocal(out=rZ[:], in_=Zs[:]), reads=[pfx + "Zs"], writes=[pfx + "rZ"])
        pi = 0
        for h in range(8):
            for q4 in range(4):
                pp = Pp[0]; pk = f"{pfx}Pp0"; pi += 1
                gk = f"{pfx}G{q4}"
                Gs = G[:, q4 * 4096:(q4 + 1) * 4096].rearrange("p (a b) -> p a b", a=32)
                P.op("vector", lambda e, h=h, q4=q4, pp=pp: e.tensor_tensor(
                    out=pp[:], in0=E[:, 2 * h, q4 * 32:(q4 + 1) * 32].unsqueeze(2).to_broadcast([128, 32, 128]),
                    in1=E[:, 2 * h + 1, :].unsqueeze(1).to_broadcast([128, 32, 128]), op=ALU.mult),
                    reads=[pfx + "E"], writes=[pk])
                P.op("vector", lambda e, h=h, pp=pp: e.scalar_tensor_tensor(
                    out=pp[:], in0=pp[:], scalar=c16[:, h, 15:16], in1=pp[:], op0=ALU.is_ge, op1=ALU.mult),
                    reads=[pk, pfx + "c16"], writes=[pk])
                if h == 0:
                    P.op("vector", lambda e, h=h, pp=pp, Gs=Gs: e.tensor_scalar(
                        out=Gs, in0=pp[:], scalar1=rZ[:, h:h + 1], scalar2=None, op0=ALU.mult),
                        reads=[pk, pfx + "rZ"], writes=[gk])
                else:
                    P.op("vector", lambda e, h=h, pp=pp, Gs=Gs: e.scalar_tensor_tensor(
                        out=Gs, in0=pp[:], scalar=rZ[:, h:h + 1], in1=Gs, op0=ALU.mult, op1=ALU.add),
                        reads=[pk, pfx + "rZ", gk], writes=[gk])
        for b in range(NEB):
            ub = u_t[ublk % 2]; uk = f"{pfx}u{ublk%2}"
            vb = v_t[ublk % 2]; vk = f"{pfx}v{ublk%2}"
            ap_ = a_ps[ublk % 2]; ak = f"{pfx}aps{ublk%2}"
            glb = gl[ublk % 2]; glk = f"{pfx}gl{ublk%2}"
            wbb = Wb[ublk % 2]; wbk = f"{pfx}Wb{ublk%2}"
            wtb = WT[ublk % 2]; wtk = f"{pfx}WT{ublk%2}"
            ublk += 1
            P.dma("gpsimd", ub[:], uT_v[:, :, b * EB:(b + 1) * EB], writes=[uk])
            P.dma("gpsimd", vb[:], v_v[b], writes=[vk])
            for c in range(NCH):
                P.op("tensor", lambda e, c=c, ub=ub, ap_=ap_: e.matmul(ap_[:, 0:EB], hT[:, c, :], ub[:, c, :], start=(c == 0), stop=(c == NCH - 1)),
                     reads=[pfx + "hT", uk], writes=[ak])
            P.op("scalar", lambda e, ap_=ap_, glb=glb: e.activation(out=glb[:], in_=ap_[:, 0:EB], func=AF.Gelu_apprx_tanh), reads=[ak], writes=[glk])
            P.op("vector", lambda e, glb=glb, wbb=wbb, b=b: e.tensor_tensor(out=wbb[:], in0=glb[:], in1=G[:, b * EB:(b + 1) * EB], op=ALU.mult),
                 reads=[glk, f"{pfx}G{(b * EB) // 4096}"], writes=[wbk])
            for k in range(KPB):
                P.op("tensor", lambda e, k=k, wbb=wbb: e.transpose(out=t_ps[:, k, :], in_=wbb[:, k * 128:(k + 1) * 128], identity=C["ident_b"][:]),
                     reads=[wbk, "ident_b"], writes=[pfx + "tps"])
            P.op("scalar", lambda e, wtb=wtb: e.copy(out=wtb[:], in_=t_ps[:]), reads=[pfx + "tps"], writes=[wtk])
            for k in range(KPB):
                for n in range(4):
                    P.op("tensor", lambda e, k=k, n=n, wtb=wtb, vb=vb, b=b: e.matmul(out_ps[n][:], wtb[:, k, :], vb[:, k, n * 512:(n + 1) * 512],
                                                                                  start=(b == 0 and k == 0), stop=(b == NEB - 1 and k == KPB - 1)),
                         reads=[wtk, vk], writes=[f"{pfx}ops{n}"])
        for n in range(4):
            P.op("scalar", lambda e, n=n: e.copy(out=fo[:, n * 512:(n + 1) * 512], in_=out_ps[n][:]), reads=[f"{pfx}ops{n}"], writes=[pfx + "fo"])
        xob = xo1; xok = pfx + "tmp"
        for cb in range(4):
            for cc in range(4):
                c = cb * 4 + cc
                P.op("tensor", lambda e, c=c, cc=cc: e.transpose(out=m_ps[:, cc * 128:(cc + 1) * 128], in_=fo[:, c * 128:(c + 1) * 128], identity=C["ident_f"][:]),
                     reads=[pfx + "fo", "ident_f"], writes=[pfx + "mps"])
            for cc in range(4):
                c = cb * 4 + cc
                P.op("vector", lambda e, c=c, cc=cc, s=s, xb=xb, xob=xob: e.scalar_tensor_tensor(
                    out=xob[:, c, :], in0=m_ps[:, cc * 128:(cc + 1) * 128], scalar=modv[:, 80 + c:81 + c, s], in1=xb[:, c, :], op0=ALU.mult, op1=ALU.add),
                    reads=[pfx + "mps", "modv", xk], writes=[xok])
        P.dma("sync", xout_v[:, :, t * 128:(t + 1) * 128], xob[:], reads=[xok], writes=[pfx + "xout"])
    return pfx + "xout"


D = 2048
NCH = 16
DEBUG = False


def load_layer_consts(P, modv_d, g_d, pfx=""):
    modv = P.sb([128, 96, 2], F32, pfx + "modv_sb")
    g = P.sb([128, NCH], F32, pfx + "g_sb")
    P.dma("sync", modv[:], modv_d, writes=["modv"])
    P.dma("sync", g[:], g_d, writes=["normg"])
    return modv, g


def make_modA(P, modv, g, sc_off, pfx):
    modA = P.sb([128, 2, NCH], F32, pfx + "modA")
    for s in range(2):
        P.op("vector", lambda e, s=s: e.scalar_tensor_tensor(
            out=modA[:, s, :], in0=modv[:, sc_off:sc_off + 16, s], scalar=1.0, in1=g[:, :], op0=ALU.add, op1=ALU.mult),
            reads=["modv", "normg"], writes=[pfx + "modA"])
    return modA


def rms_rstd(P, C, src, srck, nch, N, dim, sq, sqk, m_ps, mk, rstd, rk):
    P.op("scalar", lambda e: e.activation(out=sq[:, 0:nch, 0:N], in_=src[:, 0:nch, 0:N], func=AF.Square), reads=[srck], writes=[sqk])
    for c in range(nch):
        P.op("tensor", lambda e, c=c: e.matmul(m_ps[:, 0:N], C["ones_b"][:], sq[:, c, 0:N], start=(c == 0), stop=(c == nch - 1)),
             reads=[sqk, "ones_b"], writes=[mk])
    P.op("vector", lambda e: e.tensor_scalar(out=rstd[:, 0:N], in0=m_ps[:, 0:N], scalar1=1.0 / dim, scalar2=1e-6, op0=ALU.mult, op1=ALU.add),
         reads=[mk], writes=[rk])
    P.op("scalar", lambda e: e.activation(out=rstd[:, 0:N], in_=rstd[:, 0:N], func=AF.Sqrt), reads=[rk], writes=[rk])
    P.op("vector", lambda e: e.reciprocal(out=rstd[:, 0:N], in_=rstd[:, 0:N]), reads=[rk], writes=[rk])


def normmod(P, C, xb, xk, N, s, modA, modAk, modv, sh_off, tmp, tk, sq, sqk, m_ps, mk, rstd, rk, hT, hk):
    rms_rstd(P, C, xb, xk, NCH, N, D, sq, sqk, m_ps, mk, rstd, rk)
    P.op("vector", lambda e: e.tensor_tensor(out=tmp[:, :, 0:N], in0=xb[:, :, 0:N], in1=rstd[:, 0:N].unsqueeze(1).to_broadcast([128, NCH, N]), op=ALU.mult),
         reads=[xk, rk], writes=[tk])
    P.op("vector", lambda e: e.tensor_tensor(out=tmp[:, :, 0:N], in0=tmp[:, :, 0:N], in1=modA[:, s, :].unsqueeze(2).to_broadcast([128, NCH, N]), op=ALU.mult),
         reads=[tk, modAk], writes=[tk])
    P.op("vector", lambda e: e.tensor_tensor(out=hT[:, :, 0:N], in0=tmp[:, :, 0:N], in1=modv[:, sh_off:sh_off + 16, s].unsqueeze(2).to_broadcast([128, NCH, N]), op=ALU.add),
         reads=[tk, "modv"], writes=[hk])


def build_ada():
    P = Prog()
    cv_d = P.dram("cv", [128, NCH, 2], F32, kind="ExternalInput")
    w_d = P.dram("ada_w", [4, D, 3072], F32, kind="ExternalInput")
    b_d = P.dram("ada_b", [4, 128, 24], F32, kind="ExternalInput")
    o_d = P.dram("modv_out", [4, 128, 24, 2], F32, kind="ExternalOutput")
    cv = P.sb([128, NCH, 2], F32, "cv")
    sc = P.sb([128, NCH, 2], BF16, "sc")
    bb = P.sb([128, 4, 24], F32, "bb")
    res = P.sb([128, 4, 24, 2], F32, "res")
    wt = [P.sb([128, NCH, 512], BF16, f"w{i}") for i in range(2)]
    ps = [P.ps([128, 4, 2], F32, f"ps{i}") for i in range(2)]
    P.dma("sync", cv[:], cv_d, writes=["cv"])
    P.dma("sync", bb[:], b_d.rearrange("l p j -> p l j"), writes=["bb"])
    P.op("scalar", lambda e: e.activation(out=sc[:], in_=cv[:], func=AF.Silu), reads=["cv"], writes=["sc"])
    n = 0
    for l in range(4):
        wv = w_d[l].rearrange("(c p) n -> p c n", p=128)
        for jb in range(6):
            w = wt[n % 2]; wk = f"w{n%2}"; pt = ps[n % 2]; pk = f"ps{n%2}"; n += 1
            P.dma("gpsimd", w[:], wv[:, :, jb * 512:(jb + 1) * 512], writes=[wk])
            for jj in range(4):
                for c in range(NCH):
                    P.op("tensor", lambda e, w=w, pt=pt, jj=jj, c=c: e.matmul(pt[:, jj, :], w[:, c, jj * 128:(jj + 1) * 128], sc[:, c, :],
                                                                         start=(c == 0), stop=(c == NCH - 1)), reads=[wk, "sc"], writes=[pk])
            P.op("vector", lambda e, pt=pt, l=l, jb=jb: e.tensor_tensor(
                out=res[:, l, jb * 4:(jb + 1) * 4, :], in0=pt[:], in1=bb[:, l, jb * 4:(jb + 1) * 4].unsqueeze(2).to_broadcast([128, 4, 2]), op=ALU.add),
                reads=[pk, "bb"], writes=["res"])
    ev = P.dma("sync", o_d.rearrange("l p j s -> p l j s"), res[:], reads=["res"], writes=["out"])
    P.finish([ev])
    P.emit()
    return P.nc


def proj_fm(P, hT, hk, N, w_sb, wk, col0, ncols, ps_ap, pk, nk=NCH):
    for c in range(nk):
        P.op("tensor", lambda e, c=c: e.matmul(ps_ap, w_sb[:, c, col0:col0 + ncols], hT[:, c, 0:N], start=(c == 0), stop=(c == nk - 1)),
             reads=[wk, hk], writes=[pk])


TILES_STD = [(0, 512, 0), (512, 512, 0), (1024, 512, 0), (1536, 512, 0), (2048, 256, 1)]
NT_STD = 2304


def build_mla_a(tiles=TILES_STD, NT=NT_STD):
    P = Prog()
    C = emit_consts(P)
    xin = P.dram("xin", [D, NT], F32, kind="ExternalInput")
    modv_d = P.dram("modv", [128, 96, 2], F32, kind="ExternalInput")
    g_d = P.dram("normg", [128, NCH], F32, kind="ExternalInput")
    wa_d = P.dram("w_a", [D, 1152], F32, kind="ExternalInput")
    qg_d = P.dram("qkvg", [128, 8], F32, kind="ExternalInput")
    cos_d = P.dram("cosT", [64, NT], F32, kind="ExternalInput")
    sin_d = P.dram("sinT", [64, NT], F32, kind="ExternalInput")
    cn_d = P.dram("cnT", [1024, NT], F32, kind="ExternalOutput")
    kr_d = P.dram("krT", [64, NT], F32, kind="ExternalOutput")
    modv, g = load_layer_consts(P, modv_d, g_d)
    modA = make_modA(P, modv, g, 16, "a")
    wa = P.sb([128, NCH, 1152], BF16, "wa")
    P.dma("gpsimd", wa[:], wa_d.rearrange("(c p) n -> p c n", p=128), writes=["wa"])
    qg = P.sb([128, 8], F32, "qg")
    P.dma("sync", qg[:], qg_d, writes=["qg"])
    xb = [P.sb([128, NCH, 512], F32, f"x{i}") for i in range(2)]
    sq = P.sb([128, NCH, 512], BF16, "sq")
    rstd = P.sb([128, 512], F32, "rstd")
    hT = P.sb([128, NCH, 512], BF16, "hT")
    cT = P.sb([128, 8, 512], F32, "cT")
    cn = P.sb([128, 8, 512], F32, "cn")
    cs = P.sb([64, 2, 512], F32, "cs")
    kr = P.sb([64, 2, 512], F32, "kr")
    m_ps = P.ps([128, 512], F32, "mps")
    pp = [P.ps([128, 512], F32, f"pp{i}") for i in range(2)]
    xv = xin.rearrange("(c p) t -> p c t", p=128)
    n = 0
    nbox = [0]
    def tile_body(ti, t0, N, s):
        n = nbox[0]
        x = xb[ti % 2]; xk = f"x{ti%2}"
        P.dma("sync", x[:, :, 0:N], xv[:, :, t0:t0 + N], writes=[xk])
        P.dma("sync", cs[:, 0, 0:N], cos_d[:, t0:t0 + N], writes=["cs"])
        P.dma("sync", cs[:, 1, 0:N], sin_d[:, t0:t0 + N], writes=["cs"])
        normmod(P, C, x, xk, N, s, modA, "amodA", modv, 0, x, xk, sq, "sq", m_ps, "mps", rstd, "rstd", hT, "hT")
        if DEBUG and ti == 0:
            dh = P.dram("dbg_h", [128, NCH, 512], BF16, kind="ExternalOutput")
            dr = P.dram("dbg_r", [128, 512], F32, kind="ExternalOutput")
            P.dma("sync", dh, hT[:], reads=["hT"], writes=["dbgh"])
            P.dma("sync", dr, rstd[:], reads=["rstd"], writes=["dbgr"])
        for oc in range(8):
            p = pp[n % 2]; pk = f"pp{n%2}"; n += 1
            proj_fm(P, hT, "hT", N, wa, "wa", oc * 128, 128, p[:, 0:N], pk)
            P.op("scalar", lambda e, p=p, oc=oc: e.copy(out=cT[:, oc, 0:N], in_=p[:, 0:N]), reads=[pk], writes=["cT"])
        for v in range(2):
            p = pp[n % 2]; pk = f"pp{n%2}"; n += 1
            proj_fm(P, hT, "hT", N, wa, "wa", 1024 + v * 64, 64, p[0:64, 0:N], pk)
            P.op("vector", lambda e, p=p, v=v: e.tensor_tensor(out=kr[:, v, 0:N], in0=p[0:64, 0:N], in1=cs[:, v, 0:N], op=ALU.mult),
                 reads=[pk, "cs"], writes=["kr"])
        P.op("vector", lambda e: e.tensor_tensor(out=kr[:, 0, 0:N], in0=kr[:, 0, 0:N], in1=kr[:, 1, 0:N], op=ALU.add), reads=["kr"], writes=["kr"])
        P.dma("sync", kr_d[:, t0:t0 + N], kr[:, 0, 0:N], reads=["kr"], writes=["kr_out"])
        for half in range(2):
            rms_rstd(P, C, cT[:, half * 4:(half + 1) * 4, :], "cT", 4, N, 512, sq, "sq", m_ps, "mps", rstd, "rstd")
            P.op("vector", lambda e, half=half: e.tensor_tensor(out=cn[:, half * 4:(half + 1) * 4, 0:N], in0=cT[:, half * 4:(half + 1) * 4, 0:N],
                                                              in1=rstd[:, 0:N].unsqueeze(1).to_broadcast([128, 4, N]), op=ALU.mult),
                 reads=["cT", "rstd"], writes=["cn"])
            P.op("vector", lambda e, half=half: e.tensor_tensor(out=cn[:, half * 4:(half + 1) * 4, 0:N], in0=cn[:, half * 4:(half + 1) * 4, 0:N],
                                                              in1=qg[:, half * 4:(half + 1) * 4].unsqueeze(2).to_broadcast([128, 4, N]), op=ALU.mult),
                 reads=["cn", "qg"], writes=["cn"])
        P.dma("sync", cn_d.rearrange("(c p) t -> p c t", p=128)[:, :, t0:t0 + N], cn[:, :, 0:N], reads=["cn"], writes=["cn_out"])
        nbox[0] = n
    for ti, (t0, N, s) in enumerate(tiles):
        tile_body(ti, t0, N, s)
    P.finish([P.lastw["cn_out"], P.lastw["kr_out"]])
    P.emit()
    return P.nc


QB_STD = [(0, 512, 0), (512, 512, 0), (1024, 512, 0), (1536, 512, 0), (2048, 256, 1)]


def emit_outproj_residual(P, C, o_scr, nh, xin, xout, qblocks, modv, wo_d, pfx="op"):
    P.push_scope()
    wo = P.sb([128, nh, D], BF16, pfx + "wo")
    P.dma("gpsimd", wo[:], wo_d.rearrange("(h p) n -> p h n", p=128), writes=[pfx + "wo"])
    ot = [P.sb([128, nh, 512], BF16, f"{pfx}o{i}") for i in range(2)]
    xt = [P.sb([128, NCH, 512], F32, f"{pfx}x{i}") for i in range(2)]
    pp = [P.ps([128, 512], F32, f"{pfx}pp{i}") for i in range(2)]
    xv = xin.rearrange("(c p) t -> p c t", p=128)
    xov = xout.rearrange("(c p) t -> p c t", p=128)
    nb = [0]

    def body(qi, t0, N, s):
        o = ot[qi % 2]; ok = f"{pfx}o{qi%2}"; x = xt[qi % 2]; xk = f"{pfx}x{qi%2}"
        P.dma("sync", o[:, :, 0:N], o_scr.rearrange("h p t -> p h t")[:, :, t0:t0 + N], reads=["o_scr"], writes=[ok])
        P.dma("sync", x[:, :, 0:N], xv[:, :, t0:t0 + N], writes=[xk])
        for dc in range(NCH):
            p = pp[nb[0] % 2]; pk = f"{pfx}pp{nb[0]%2}"; nb[0] += 1
            for h in range(nh):
                P.op("tensor", lambda e, h=h, p=p, dc=dc: e.matmul(p[:, 0:N], wo[:, h, dc * 128:(dc + 1) * 128], o[:, h, 0:N], start=(h == 0), stop=(h == nh - 1)),
                     reads=[pfx + "wo", ok], writes=[pk])
            P.op("vector", lambda e, p=p, dc=dc: e.scalar_tensor_tensor(out=x[:, dc, 0:N], in0=p[:, 0:N], scalar=modv[:, 32 + dc:33 + dc, s], in1=x[:, dc, 0:N],
                                                                     op0=ALU.mult, op1=ALU.add), reads=[pk, "modv", xk], writes=[xk])
        P.dma("sync", xov[:, :, t0:t0 + N], x[:, :, 0:N], reads=[xk], writes=["xout"])
    for qi, (t0, N, s) in enumerate(qblocks):
        body(qi, t0, N, s)
    P.pop_scope()


def build_mla_b(qblocks=QB_STD, NT=NT_STD, NKL=8192, NKC=256):
    NK = NKL + NKC
    nkt = NK // 128
    P = Prog()
    C = emit_consts(P)
    xin = P.dram("xin", [D, NT], F32, kind="ExternalInput")
    xout = P.dram("xout", [D, NT], F32, kind="ExternalOutput")
    modv_d = P.dram("modv", [128, 96, 2], F32, kind="ExternalInput")
    cq_d = P.dram("cqT", [512, NT], F32, kind="ExternalInput")
    ckv_d = P.dram("ckvT", [512, NK], F32, kind="ExternalInput")
    kr_d = P.dram("krT", [64, NK], F32, kind="ExternalInput")
    cos_d = P.dram("cosT", [64, NT], F32, kind="ExternalInput")
    sin_d = P.dram("sinT", [64, NT], F32, kind="ExternalInput")
    wuq_d = P.dram("w_uq", [16, 512, 256], F32, kind="ExternalInput")
    wukv_d = P.dram("w_ukv", [16, 512, 256], F32, kind="ExternalInput")
    wo_d = P.dram("w_o", [D, D], F32, kind="ExternalInput")
    o_scr = P.dram("o_scr", [16, 128, NT], BF16)
    modv = P.sb([128, 96, 2], F32, "modv_sb")
    P.dma("sync", modv[:], modv_d, writes=["modv"])
    scale = 192.0 ** -0.5
    P.push_scope()
    ckv = P.sb([128, 4, NK], BF16, "ckv")
    cq = P.sb([128, 4, NT], BF16, "cq")
    kr = P.sb([64, NK], BF16, "kr")
    cs = P.sb([64, 2, NT], F32, "cs")
    P.dma("gpsimd", ckv[:], ckv_d.rearrange("(c p) t -> p c t", p=128), writes=["ckv"])
    P.dma("gpsimd", cq[:], cq_d.rearrange("(c p) t -> p c t", p=128), writes=["cq"])
    P.dma("gpsimd", kr[:], kr_d, writes=["kr"])
    P.dma("sync", cs[:, 0, :], cos_d, writes=["cs"])
    P.dma("sync", cs[:, 1, :], sin_d, writes=["cs"])
    KhT = P.sb([128, NK], BF16, "KhT")
    Vh = P.sb([128, nkt, 128], BF16, "Vh")
    QhT = P.sb([128, NT], BF16, "QhT")
    QrT = P.sb([64, NT], BF16, "QrT")
    qr1 = P.sb([64, 512], F32, "qr1")
    qr2 = P.sb([64, 512], F32, "qr2")
    wkv = [P.sb([128, 4, 256], BF16, f"wkv{i}") for i in range(2)]
    wq = [P.sb([128, 4, 256], BF16, f"wq{i}") for i in range(2)]
    PT = [P.sb([128, 512], BF16, f"PT{i}") for i in range(2)]
    oh = [P.sb([128, 512], BF16, f"oh{i}") for i in range(2)]
    rden = P.sb([128, 512], F32, "rden")
    s_ps = [P.ps([128, 512], F32, f"sps{i}") for i in range(2)]
    o_ps = P.ps([128, 512], F32, "ops")
    d_ps = P.ps([128, 512], F32, "dps")
    m_ps = [P.ps([128, 512], F32, f"mps{i}") for i in range(2)]
    cnt = {"m": 0, "s": 0, "o": 0}

    def head(h):
        wk_ = wkv[h % 2]; wkk = f"wkv{h%2}"; wq_ = wq[h % 2]; wqk = f"wq{h%2}"
        P.dma("gpsimd", wk_[:], wukv_d[h].rearrange("(c p) n -> p c n", p=128), writes=[wkk])
        P.dma("gpsimd", wq_[:], wuq_d[h].rearrange("(c p) n -> p c n", p=128), writes=[wqk])
        for kb in range((NK + 511) // 512):
            k0 = kb * 512; N = min(512, NK - k0)
            p = m_ps[cnt["m"] % 2]; pk = f"mps{cnt['m']%2}"; cnt["m"] += 1
            for c in range(4):
                P.op("tensor", lambda e, c=c, p=p, k0=k0, N=N: e.matmul(p[:, 0:N], wk_[:, c, 0:128], ckv[:, c, k0:k0 + N], start=(c == 0), stop=(c == 3)),
                     reads=[wkk, "ckv"], writes=[pk])
            P.op("scalar", lambda e, p=p, k0=k0, N=N: e.copy(out=KhT[:, k0:k0 + N], in_=p[:, 0:N]), reads=[pk], writes=["KhT"])
        for kg in range((nkt + 3) // 4):
            p = m_ps[cnt["m"] % 2]; pk = f"mps{cnt['m']%2}"; cnt["m"] += 1
            nn = min(4, nkt - kg * 4)
            for j in range(nn):
                kt = kg * 4 + j
                for c in range(4):
                    P.op("tensor", lambda e, c=c, p=p, kt=kt, j=j: e.matmul(p[:, j * 128:(j + 1) * 128], ckv[:, c, kt * 128:(kt + 1) * 128], wk_[:, c, 128:256],
                                                                         start=(c == 0), stop=(c == 3)), reads=[wkk, "ckv"], writes=[pk])
            P.op("vector", lambda e, p=p, kg=kg, nn=nn: e.tensor_copy(out=Vh[:, kg * 4:kg * 4 + nn, :], in_=p[:, 0:nn * 128].rearrange("p (j v) -> p j v", j=nn)),
                 reads=[pk], writes=["Vh"])
        for (t0, N, s) in qblocks:
            p = m_ps[cnt["m"] % 2]; pk = f"mps{cnt['m']%2}"; cnt["m"] += 1
            for c in range(4):
                P.op("tensor", lambda e, c=c, p=p, t0=t0, N=N: e.matmul(p[:, 0:N], wq_[:, c, 0:128], cq[:, c, t0:t0 + N], start=(c == 0), stop=(c == 3)),
                     reads=[wqk, "cq"], writes=[pk])
            P.op("scalar", lambda e, p=p, t0=t0, N=N: e.activation(out=QhT[:, t0:t0 + N], in_=p[:, 0:N], func=AF.Copy, scale=scale), reads=[pk], writes=["QhT"])
            for v, dst in ((0, qr1), (1, qr2)):
                p = m_ps[cnt["m"] % 2]; pk = f"mps{cnt['m']%2}"; cnt["m"] += 1
                for c in range(4):
                    P.op("tensor", lambda e, c=c, p=p, t0=t0, N=N, v=v: e.matmul(p[0:64, 0:N], wq_[:, c, 128 + 64 * v:192 + 64 * v], cq[:, c, t0:t0 + N], start=(c == 0), stop=(c == 3)),
                         reads=[wqk, "cq"], writes=[pk])
                P.op("vector", lambda e, p=p, t0=t0, N=N, v=v, dst=dst: e.tensor_tensor(out=dst[:, 0:N], in0=p[0:64, 0:N], in1=cs[:, v, t0:t0 + N], op=ALU.mult),
                     reads=[pk, "cs"], writes=["qr%d" % v])
            P.op("vector", lambda e, t0=t0, N=N: e.tensor_tensor(out=qr1[:, 0:N], in0=qr1[:, 0:N], in1=qr2[:, 0:N], op=ALU.add), reads=["qr0", "qr1"], writes=["qr0"])
            P.op("scalar", lambda e, t0=t0, N=N: e.activation(out=QrT[:, t0:t0 + N], in_=qr1[:, 0:N], func=AF.Copy, scale=scale), reads=["qr0"], writes=["QrT"])
        for (t0, N, s) in qblocks:
            kts = list(range(nkt)) if s == 0 else list(range(NKL // 128, nkt))
            for ii, kt in enumerate(kts):
                sp = s_ps[cnt["s"] % 2]; sk = f"sps{cnt['s']%2}"; pt = PT[cnt["s"] % 2]; ptk = f"PT{cnt['s']%2}"; cnt["s"] += 1
                P.op("tensor", lambda e, sp=sp, kt=kt, t0=t0, N=N: e.matmul(sp[:, 0:N], KhT[:, kt * 128:(kt + 1) * 128], QhT[:, t0:t0 + N], start=True, stop=False),
                     reads=["KhT", "QhT"], writes=[sk])
                P.op("tensor", lambda e, sp=sp, kt=kt, t0=t0, N=N: e.matmul(sp[:, 0:N], kr[:, kt * 128:(kt + 1) * 128], QrT[:, t0:t0 + N], start=False, stop=True),
                     reads=["kr", "QrT"], writes=[sk])
                P.op("scalar", lambda e, sp=sp, pt=pt, N=N: e.activation(out=pt[:, 0:N], in_=sp[:, 0:N], func=AF.Exp), reads=[sk], writes=[ptk])
                P.op("tensor", lambda e, pt=pt, kt=kt, N=N, ii=ii, last=(ii == len(kts) - 1): e.matmul(o_ps[:, 0:N], Vh[:, kt, :], pt[:, 0:N], start=(ii == 0), stop=last),
                     reads=["Vh", ptk], writes=["ops"])
                P.op("tensor", lambda e, pt=pt, N=N, ii=ii, last=(ii == len(kts) - 1): e.matmul(d_ps[:, 0:N], C["ones_b"][:], pt[:, 0:N], start=(ii == 0), stop=last),
                     reads=["ones_b", ptk], writes=["dps"])
            o_ = oh[cnt["o"] % 2]; ok = f"oh{cnt['o']%2}"; cnt["o"] += 1
            P.op("vector", lambda e, N=N: e.reciprocal(out=rden[:, 0:N], in_=d_ps[:, 0:N]), reads=["dps"], writes=["rden"])
            P.op("vector", lambda e, N=N, o_=o_: e.tensor_tensor(out=o_[:, 0:N], in0=o_ps[:, 0:N], in1=rden[:, 0:N], op=ALU.mult), reads=["ops", "rden"], writes=[ok])
            P.dma("sync", o_scr[h, :, t0:t0 + N], o_[:, 0:N], reads=[ok], writes=["o_scr"])
    for h in range(16):
        head(h)
    P.pop_scope()
    emit_outproj_residual(P, C, o_scr, 16, xin, xout, qblocks, modv, wo_d)
    P.finish([P.lastw["xout"]])
    print("mla_b ops", {e: len(v_) for e, v_ in P.ops.items()})
    P.emit()
    return P.nc


def emit_front_h(P, C, xin, tiles, NT, modv, g, sc_off, sh_off, pfx="f"):
    modA = make_modA(P, modv, g, sc_off, pfx)
    hT = P.sb([128, NCH, NT], BF16, pfx + "hT")
    P.push_scope()
    xb = [P.sb([128, NCH, 512], F32, f"{pfx}x{i}") for i in range(2)]
    sq = P.sb([128, NCH, 512], BF16, pfx + "sq")
    rstd = P.sb([128, 512], F32, pfx + "rstd")
    m_ps = P.ps([128, 512], F32, pfx + "mps")
    xv = xin.rearrange("(c p) t -> p c t", p=128)

    def body(ti, t0, N, s):
        x = xb[ti % 2]; xk = f"{pfx}x{ti%2}"
        P.dma("sync", x[:, :, 0:N], xv[:, :, t0:t0 + N], writes=[xk])
        normmod(P, C, x, xk, N, s, modA, pfx + "modA", modv, sh_off, x, xk, sq, pfx + "sq", m_ps, pfx + "mps", rstd, pfx + "rstd",
                hT[:, :, t0:t0 + N], pfx + "hT")
    for ti, (t0, N, s) in enumerate(tiles):
        body(ti, t0, N, s)
    P.pop_scope()
    return hT


def build_win_a(tiles=TILES_STD, NT=NT_STD):
    P = Prog()
    C = emit_consts(P)
    xin = P.dram("xin", [D, NT], F32, kind="ExternalInput")
    modv_d = P.dram("modv", [128, 96, 2], F32, kind="ExternalInput")
    g_d = P.dram("normg", [128, NCH], F32, kind="ExternalInput")
    wqk_d = P.dram("w_qk", [D, 20, 256], F32, kind="ExternalInput")
    wv_d = P.dram("w_v", [D, 512], F32, kind="ExternalInput")
    cos_d = P.dram("cos2", [128, NT], F32, kind="ExternalInput")
    sin_d = P.dram("sin2", [128, NT], F32, kind="ExternalInput")
    qk_d = P.dram("QKT", [20 * 128, NT], F32, kind="ExternalOutput")
    v_d = P.dram("V", [NT, 512], F32, kind="ExternalOutput")
    modv, g = load_layer_consts(P, modv_d, g_d)
    hT = emit_front_h(P, C, xin, tiles, NT, modv, g, 16, 0)
    cs = P.sb([128, 2, NT], F32, "cs")
    P.dma("sync", cs[:, 0, :], cos_d, writes=["cs"])
    P.dma("sync", cs[:, 1, :], sin_d, writes=["cs"])
    w = [P.sb([128, NCH, 256], BF16, f"w{i}") for i in range(2)]
    wv = P.sb([128, NCH, 512], BF16, "wv")
    P.dma("gpsimd", wv[:], wv_d.rearrange("(c p) n -> p c n", p=128), writes=["wv"])
    r1 = [P.sb([128, 512], F32, f"r1_{i}") for i in range(2)]
    r2 = P.sb([128, 512], F32, "r2")
    vt = [P.sb([128, 512], F32, f"vt{i}") for i in range(2)]
    pp = [P.ps([128, 512], F32, f"pp{i}") for i in range(4)]
    cnt = [0, 0]
    scale = 64.0 ** -0.5

    def chunk(oc):
        wb = w[oc % 2]; wk = f"w{oc%2}"
        P.dma("gpsimd", wb[:], wqk_d.rearrange("(c p) o n -> p c o n", p=128)[:, :, oc, :], writes=[wk])
        for (t0, N, s) in tiles:
            pa = pp[cnt[0] % 4]; pak = f"pp{cnt[0]%4}"; cnt[0] += 1
            pb = pp[cnt[0] % 4]; pbk = f"pp{cnt[0]%4}"; cnt[0] += 1
            proj_fm(P, hT[:, :, t0:t0 + N], "fhT", N, wb, wk, 0, 128, pa[:, 0:N], pak)
            proj_fm(P, hT[:, :, t0:t0 + N], "fhT", N, wb, wk, 128, 128, pb[:, 0:N], pbk)
            ra = r1[cnt[1] % 2]; rak = f"r1_{cnt[1]%2}"; cnt[1] += 1
            P.op("vector", lambda e, pa=pa, ra=ra, t0=t0, N=N: e.tensor_tensor(out=ra[:, 0:N], in0=pa[:, 0:N], in1=cs[:, 0, t0:t0 + N], op=ALU.mult), reads=[pak, "cs"], writes=[rak])
            P.op("vector", lambda e, pb=pb, t0=t0, N=N: e.tensor_tensor(out=r2[:, 0:N], in0=pb[:, 0:N], in1=cs[:, 1, t0:t0 + N], op=ALU.mult), reads=[pbk, "cs"], writes=["r2"])
            P.op("vector", lambda e, ra=ra, N=N: e.scalar_tensor_tensor(out=ra[:, 0:N], in0=ra[:, 0:N], scalar=(scale if oc < 16 else 1.0), in1=r2[:, 0:N], op0=ALU.mult, op1=ALU.add)
                 if False else e.tensor_tensor(out=ra[:, 0:N], in0=ra[:, 0:N], in1=r2[:, 0:N], op=ALU.add), reads=[rak, "r2"], writes=[rak])
            if oc < 16:
                P.op("scalar", lambda e, ra=ra, N=N: e.mul(out=ra[:, 0:N], in_=ra[:, 0:N], mul=scale), reads=[rak], writes=[rak])
            P.dma("sync", qk_d[oc * 128:(oc + 1) * 128, t0:t0 + N], ra[:, 0:N], reads=[rak], writes=["qk_out"])
    for oc in range(20):
        chunk(oc)

    def vtile(i):
        p = pp[cnt[0] % 4]; pk = f"pp{cnt[0]%4}"; cnt[0] += 1
        for c in range(NCH):
            P.op("tensor", lambda e, c=c, p=p: e.matmul(p[:], hT[:, c, i * 128:(i + 1) * 128], wv[:, c, :], start=(c == 0), stop=(c == NCH - 1)),
                 reads=["fhT", "wv"], writes=[pk])
        v_ = vt[i % 2]; vk = f"vt{i%2}"
        P.op("scalar", lambda e, p=p, v_=v_: e.copy(out=v_[:], in_=p[:]), reads=[pk], writes=[vk])
        P.dma("sync", v_d[i * 128:(i + 1) * 128, :], v_[:], reads=[vk], writes=["v_out"])
    for i in range(NT // 128):
        vtile(i)
    P.finish([P.lastw["qk_out"], P.lastw["v_out"]])
    P.emit()
    return P.nc


def build_win_b(qblocks=QB_STD, NT=NT_STD):
    nlat = sum(N for (_, N, s) in qblocks if s == 0)
    nown = nlat // 128
    nke = nown + 4
    NKE = nke * 128
    nqb = len(qblocks)
    P = Prog()
    C = emit_consts(P)
    xin = P.dram("xin", [D, NT], F32, kind="ExternalInput")
    xout = P.dram("xout", [D, NT], F32, kind="ExternalOutput")
    modv_d = P.dram("modv", [128, 96, 2], F32, kind="ExternalInput")
    q_d = P.dram("QT", [2048, NT], F32, kind="ExternalInput")
    k_d = P.dram("KT", [512, NKE], F32, kind="ExternalInput")
    v_d = P.dram("V", [NKE, 512], F32, kind="ExternalInput")
    mask_d = P.dram("masks", [nqb, 6, 128, 512], F32, kind="ExternalInput")
    sink_d = P.dram("sinks", [64, 32], F32, kind="ExternalInput")
    wo_d = P.dram("w_o", [D, D], F32, kind="ExternalInput")
    o_scr = P.dram("o_scr", [16, 128, NT], BF16)
    modv = P.sb([128, 96, 2], F32, "modv_sb")
    P.dma("sync", modv[:], modv_d, writes=["modv"])
    P.push_scope()
    masks = P.sb([128, nqb, 6, 512], BF16, "masks")
    P.dma("gpsimd", masks[:], mask_d.rearrange("b j p q -> p b j q"), writes=["masks"])
    sk = P.sb([64, 32], F32, "sk")
    P.dma("sync", sk[:], sink_d, writes=["sk"])
    P.op("scalar", lambda e: e.activation(out=sk[:], in_=sk[:], func=AF.Exp), reads=["sk"], writes=["sk"])
    Kk = [P.sb([64, NKE], BF16, f"Kk{i}") for i in range(2)]
    Vv = [P.sb([128, nke, 64], BF16, f"Vv{i}") for i in range(2)]
    Qh = [P.sb([64, NT], BF16, f"Qh{i}") for i in range(2)]
    PT = [P.sb([128, 512], BF16, f"PT{i}") for i in range(2)]
    oh = [P.sb([64, 512], BF16, f"oh{i}") for i in range(2)]
    rden = P.sb([64, 512], F32, "rden")
    s_ps = [P.ps([128, 512], F32, f"sps{i}") for i in range(2)]
    o_ps = P.ps([128, 512], F32, "ops")
    d_ps = P.ps([128, 512], F32, "dps")
    cnt = {"s": 0, "o": 0}

    def head(h):
        kvh = h // 4
        kk = Kk[kvh % 2]; kkk = f"Kk{kvh%2}"; vv = Vv[kvh % 2]; vvk = f"Vv{kvh%2}"
        if h % 4 == 0:
            P.dma("gpsimd", kk[:], k_d[kvh * 64:(kvh + 1) * 64, :], writes=[kkk])
            P.dma("gpsimd", vv[:], v_d.rearrange("(t p) d -> p t d", p=128)[:, :, kvh * 64:(kvh + 1) * 64], writes=[vvk])
        qh = Qh[h % 2]; qk = f"Qh{h%2}"
        P.dma("gpsimd", qh[:], q_d[h * 64:(h + 1) * 64, :], writes=[qk])
        for bi, (t0, N, s) in enumerate(qblocks):
            if s == 0:
                b = t0 // 512
                slots = [(4 * b + j, j) for j in range(6)] + [(nown + 2, None), (nown + 3, None)]
            else:
                slots = [(nown + 2, None), (nown + 3, None)]
            for ii, (e_, mj) in enumerate(slots):
                sp = s_ps[cnt["s"] % 2]; spk = f"sps{cnt['s']%2}"; pt = PT[cnt["s"] % 2]; ptk = f"PT{cnt['s']%2}"; cnt["s"] += 1
                last = (ii == len(slots) - 1)
                P.op("tensor", lambda e, sp=sp, e_=e_, t0=t0, N=N: e.matmul(sp[:, 0:N], kk[:, e_ * 128:(e_ + 1) * 128], qh[:, t0:t0 + N], start=True, stop=True),
                     reads=[kkk, qk], writes=[spk])
                P.op("scalar", lambda e, sp=sp, pt=pt, N=N: e.activation(out=pt[:, 0:N], in_=sp[:, 0:N], func=AF.Exp), reads=[spk], writes=[ptk])
                if mj is not None:
                    P.op("vector", lambda e, pt=pt, N=N, bi=bi, mj=mj: e.tensor_tensor(out=pt[:, 0:N], in0=pt[:, 0:N], in1=masks[:, bi, mj, 0:N], op=ALU.mult),
                         reads=[ptk, "masks"], writes=[ptk])
                P.op("tensor", lambda e, pt=pt, e_=e_, N=N, ii=ii, last=last: e.matmul(o_ps[0:64, 0:N], vv[:, e_, :], pt[:, 0:N], start=(ii == 0), stop=last),
                     reads=[vvk, ptk], writes=["ops"])
                P.op("tensor", lambda e, pt=pt, N=N, ii=ii, last=last: e.matmul(d_ps[0:64, 0:N], C["ones_b"][:, 0:64], pt[:, 0:N], start=(ii == 0), stop=last),
                     reads=["ones_b", ptk], writes=["dps"])
            o_ = oh[cnt["o"] % 2]; ok = f"oh{cnt['o']%2}"; cnt["o"] += 1
            P.op("vector", lambda e, N=N: e.tensor_scalar(out=rden[:, 0:N], in0=d_ps[0:64, 0:N], scalar1=sk[:, h:h + 1], scalar2=None, op0=ALU.add), reads=["dps", "sk"], writes=["rden"])
            P.op("vector", lambda e, N=N: e.reciprocal(out=rden[:, 0:N], in_=rden[:, 0:N]), reads=["rden"], writes=["rden"])
            P.op("vector", lambda e, N=N, o_=o_: e.tensor_tensor(out=o_[:, 0:N], in0=o_ps[0:64, 0:N], in1=rden[:, 0:N], op=ALU.mult), reads=["ops", "rden"], writes=[ok])
            P.dma("sync", o_scr[h // 2, (h % 2) * 64:(h % 2) * 64 + 64, t0:t0 + N], o_[:, 0:N], reads=[ok], writes=["o_scr"])
    for h in range(32):
        head(h)
    P.pop_scope()
    emit_outproj_residual(P, C, o_scr, 16, xin, xout, qblocks, modv, wo_d)
    P.finish([P.lastw["xout"]])
    print("win_b ops", {e: len(v_) for e, v_ in P.ops.items()})
    P.emit()
    return P.nc


def build_final(tiles=TILES_STD[:4], NT=2048):
    P = Prog()
    C = emit_consts(P)
    xin = P.dram("xin", [D, NT], F32, kind="ExternalInput")
    g_d = P.dram("normg", [128, NCH], F32, kind="ExternalInput")
    xout = P.dram("xout", [D, NT], F32, kind="ExternalOutput")
    g = P.sb([128, NCH], F32, "g_sb")
    P.dma("sync", g[:], g_d, writes=["normg"])
    xb = [P.sb([128, NCH, 512], F32, f"x{i}") for i in range(2)]
    sq = P.sb([128, NCH, 512], BF16, "sq")
    rstd = P.sb([128, 512], F32, "rstd")
    m_ps = P.ps([128, 512], F32, "mps")
    xv = xin.rearrange("(c p) t -> p c t", p=128)
    xov = xout.rearrange("(c p) t -> p c t", p=128)

    def body(ti, t0, N):
        x = xb[ti % 2]; xk = f"x{ti%2}"
        P.dma("sync", x[:, :, 0:N], xv[:, :, t0:t0 + N], writes=[xk])
        rms_rstd(P, C, x, xk, NCH, N, D, sq, "sq", m_ps, "mps", rstd, "rstd")
        P.op("vector", lambda e: e.tensor_tensor(out=x[:, :, 0:N], in0=x[:, :, 0:N], in1=rstd[:, 0:N].unsqueeze(1).to_broadcast([128, NCH, N]), op=ALU.mult),
             reads=[xk, "rstd"], writes=[xk])
        P.op("vector", lambda e: e.tensor_tensor(out=x[:, :, 0:N], in0=x[:, :, 0:N], in1=g[:, :].unsqueeze(2).to_broadcast([128, NCH, N]), op=ALU.mult),
             reads=[xk, "normg"], writes=[xk])
        P.dma("sync", xov[:, :, t0:t0 + N], x[:, :, 0:N], reads=[xk], writes=["xout"])
    for ti, (t0, N, s) in enumerate(tiles):
        body(ti, t0, N)
    P.finish([P.lastw["xout"]])
    P.emit()
    return P.nc


def build_peer(kinds, NT):
    P = Prog()
    C = emit_consts(P)
    xin = P.dram("xin", [D, NT], F32, kind="ExternalInput")
    xout = P.dram("xout", [D, NT], F32, kind="ExternalOutput")
    modv_d = P.dram("modv", [128, 96, 2], F32, kind="ExternalInput")
    g_d = P.dram("normg", [128, NCH], F32, kind="ExternalInput")
    wq_d = P.dram("wq", [D, 2048], F32, kind="ExternalInput")
    k1T_d = P.dram("k1T", [8, 128, 128], F32, kind="ExternalInput")
    k2T_d = P.dram("k2T", [8, 128, 128], F32, kind="ExternalInput")
    uT_d = P.dram("uT", [D, 16384], F32, kind="ExternalInput")
    v_d = P.dram("v", [16384, D], F32, kind="ExternalInput")
    modv, g = load_layer_consts(P, modv_d, g_d)
    k = emit_peer(P, C, xin, xout, kinds, modv, g, wq_d, k1T_d, k2T_d, uT_d, v_d)
    P.finish([P.lastw[k]])
    P.emit()
    return P.nc


def build_ssd_s(n_ctx_chunks=2, n_lat_chunks=64):
    NCHK = n_ctx_chunks + n_lat_chunks
    NTS = NCHK * 128
    fwd_order = list(range(NCHK))
    bwd_order = list(range(n_ctx_chunks - 1, -1, -1)) + list(range(NCHK - 1, n_ctx_chunks - 1, -1))
    P = Prog()
    C = emit_consts(P)
    x_d = P.dram("x_tm", [NCHK, 128, 1024], F32, kind="ExternalInput")
    b_d = P.dram("B_tm", [NCHK, 128, 256], F32, kind="ExternalInput")
    bt_d = P.dram("BT", [2, 128, NTS], F32, kind="ExternalInput")
    ct_d = P.dram("CT", [2, 128, NTS], F32, kind="ExternalInput")
    dt_d = P.dram("dt_tm", [NCHK, 128, 32], F32, kind="ExternalInput")
    al_d = P.dram("alog", [128, 32], F32, kind="ExternalInput")
    dr_d = P.dram("Drep", [128, 16], F32, kind="ExternalInput")
    tri_d = P.dram("tri", [2, 128, 128], F32, kind="ExternalInput")
    y_d = P.dram("y_tm", [NCHK, 128, 1024], F32, kind="ExternalOutput")

    A = P.sb([128, 32], F32, "A")
    Dr = P.sb([128, 16], F32, "Dr")
    tri = P.sb([128, 2, 128], F32, "tri")
    ones_f = P.sb([128, 128], F32, "ones_f")
    P.dma("sync", A[:], al_d, writes=["A"])
    P.dma("sync", Dr[:], dr_d, writes=["Dr"])
    P.dma("sync", tri[:], tri_d.rearrange("d j i -> j d i"), writes=["tri"])
    P.op("scalar", lambda e: e.activation(out=A[:], in_=A[:], func=AF.Exp), reads=["A"], writes=["A"])
    P.op("vector", lambda e: e.tensor_scalar(out=A[:], in0=A[:], scalar1=-1.0, scalar2=None, op0=ALU.mult), reads=["A"], writes=["A"])
    P.op("vector", lambda e: e.memset(ones_f[:], 1.0), writes=["ones_f"])

    x32 = [P.sb([128, 1024], F32, f"x32_{i}") for i in range(2)]
    xcb = [P.sb([128, 1024], BF16, f"xcb{i}") for i in range(2)]
    Bc = [P.sb([128, 256], BF16, f"Bc{i}") for i in range(2)]
    BTc = [P.sb([128, 2, 128], BF16, f"BTc{i}") for i in range(2)]
    CTc = [P.sb([128, 2, 128], BF16, f"CTc{i}") for i in range(2)]
    CTf = [P.sb([128, 2, 128], F32, f"CTf{i}") for i in range(2)]
    dtc = [P.sb([128, 32], F32, f"dtc{i}") for i in range(2)]
    dA = P.sb([128, 16], F32, "dA")
    dAb = P.sb([128, 16, 128], F32, "dAb")
    acs = P.sb([128, 16], F32, "acs")
    wst = P.sb([128, 16], F32, "wst")
    etot = P.sb([128, 16], F32, "etot")
    cbm = P.sb([128, 2, 128], F32, "cbm")
    dec = [P.sb([128, 128], F32, f"dec{i}") for i in range(2)]
    Mh = [P.sb([128, 128], BF16, f"Mh{i}") for i in range(2)]
    erow = [P.sb([128, 128], F32, f"erow{i}") for i in range(2)]
    CTe = [P.sb([128, 128], BF16, f"CTe{i}") for i in range(2)]
    S = P.sb([128, 2, 512], F32, "S")
    Sb = P.sb([128, 2, 512], BF16, "Sb")
    xw = P.sb([128, 2, 512], BF16, "xw")
    xD = P.sb([128, 1024], F32, "xD")
    ysb = [P.sb([128, 1024], F32, f"ysb{i}") for i in range(2)]
    yprev = [P.sb([128, 1024], F32, f"yprev{i}") for i in range(2)]
    row_ps = [P.ps([128, 512], F32, f"rowps{i}") for i in range(2)]
    y_ps = [P.ps([128, 512], F32, f"yps{i}") for i in range(2)]
    cb_ps = P.ps([128, 2, 128], F32, "cbps")
    sn_ps = P.ps([128, 512], F32, "snps")
    sm_ps = P.ps([128, 2, 16], F32, "smps")
    cnt = {"c": 0, "h": 0}

    def chunk(d, c):
        i2 = cnt["c"] % 2; cnt["c"] += 1
        x3 = x32[i2]; x3k = f"x32_{i2}"; xb = xcb[i2]; xbk = f"xcb{i2}"
        bc = Bc[i2]; bck = f"Bc{i2}"; btc = BTc[i2]; btk = f"BTc{i2}"; ctc = CTc[i2]; ctk = f"CTc{i2}"; ctf = CTf[i2]; ctfk = f"CTf{i2}"
        dc = dtc[i2]; dck = f"dtc{i2}"
        P.dma("sync", x3[:], x_d[c], writes=[x3k])
        P.dma("gpsimd", bc[:], b_d[c], writes=[bck])
        P.dma("gpsimd", btc[:], bt_d[:, :, c * 128:(c + 1) * 128].rearrange("g n t -> n g t"), writes=[btk])
        P.dma("sync", ctf[:], ct_d[:, :, c * 128:(c + 1) * 128].rearrange("g n t -> n g t"), writes=[ctfk])
        P.dma("sync", dc[:], dt_d[c], writes=[dck])
        P.op("scalar", lambda e: e.copy(out=xb[:], in_=x3[:]), reads=[x3k], writes=[xbk])
        P.op("scalar", lambda e: e.copy(out=ctc[:], in_=ctf[:]), reads=[ctfk], writes=[ctk])
        dts = dc[:, d * 16:(d + 1) * 16]
        P.op("vector", lambda e: e.tensor_tensor(out=dA[:], in0=dts, in1=A[:, d * 16:(d + 1) * 16], op=ALU.mult), reads=[dck, "A"], writes=["dA"])
        P.op("vector", lambda e: e.tensor_copy(out=dAb[:], in_=dA[:].unsqueeze(2).to_broadcast([128, 16, 128])), reads=["dA"], writes=["dAb"])
        P.op("tensor", lambda e: e.matmul(sm_ps[:, 0, :], tri[:, d, :], dA[:], start=True, stop=True), reads=["tri", "dA"], writes=["smps"])
        P.op("tensor", lambda e: e.matmul(sm_ps[:, 1, :], ones_f[:], dA[:], start=True, stop=True), reads=["ones_f", "dA"], writes=["smps"])
        P.op("vector", lambda e: e.tensor_copy(out=acs[:], in_=sm_ps[:, 0, :]), reads=["smps"], writes=["acs"])
        P.op("vector", lambda e: e.tensor_tensor(out=wst[:], in0=sm_ps[:, 1, :], in1=acs[:], op=ALU.subtract), reads=["smps", "acs"], writes=["wst"])
        P.op("scalar", lambda e: e.activation(out=wst[:], in_=wst[:], func=AF.Exp), reads=["wst"], writes=["wst"])
        P.op("vector", lambda e: e.tensor_tensor(out=wst[:], in0=wst[:], in1=dts, op=ALU.mult), reads=["wst", dck], writes=["wst"])
        P.op("scalar", lambda e: e.activation(out=etot[:], in_=sm_ps[:, 1, :], func=AF.Exp), reads=["smps"], writes=["etot"])
        for g in range(2):
            P.op("tensor", lambda e, g=g: e.matmul(cb_ps[:, g, :], btc[:, g, :], ctc[:, g, :], start=True, stop=True), reads=[btk, ctk], writes=["cbps"])
        P.op("vector", lambda e: e.tensor_tensor(out=cbm[:], in0=cb_ps[:], in1=tri[:, d, :].unsqueeze(1).to_broadcast([128, 2, 128]), op=ALU.mult),
             reads=["cbps", "tri"], writes=["cbm"])
        for h in range(16):
            g = h // 8
            j2 = cnt["h"] % 2; cnt["h"] += 1
            rp = row_ps[j2]; rpk = f"rowps{j2}"; de = dec[j2]; dek = f"dec{j2}"; mh = Mh[j2]; mhk = f"Mh{j2}"
            er = erow[j2]; erk = f"erow{j2}"; ce = CTe[j2]; cek = f"CTe{j2}"
            P.op("tensor", lambda e, h=h, rp=rp: e.matmul(rp[:, 0:128], dAb[:, h, :], tri[:, d, :], start=True, stop=True), reads=["dAb", "tri"], writes=[rpk])
            P.op("vector", lambda e, h=h, rp=rp, de=de: e.tensor_scalar(out=de[:], in0=rp[:, 0:128], scalar1=acs[:, h:h + 1], scalar2=0.0, op0=ALU.subtract, op1=ALU.min),
                 reads=[rpk, "acs"], writes=[dek])
            P.op("scalar", lambda e, de=de: e.activation(out=de[:], in_=de[:], func=AF.Exp), reads=[dek], writes=[dek])
            P.op("vector", lambda e, h=h, de=de, mh=mh, g=g: e.scalar_tensor_tensor(out=mh[:], in0=de[:], scalar=dts[:, h:h + 1], in1=cbm[:, g, :], op0=ALU.mult, op1=ALU.mult),
                 reads=[dek, dck, "cbm"], writes=[mhk])
            P.op("scalar", lambda e, rp=rp, er=er: e.activation(out=er[:], in_=rp[:, 0:128], func=AF.Exp), reads=[rpk], writes=[erk])
            P.op("vector", lambda e, er=er, ce=ce, g=g: e.tensor_tensor(out=ce[:], in0=er[:], in1=ctf[:, g, :], op=ALU.mult), reads=[erk, ctfk], writes=[cek])
            hs = slice((h % 8) * 64, (h % 8) * 64 + 64)
            P.op("tensor", lambda e, h=h, g=g, mh=mh, hs=hs: e.matmul(y_ps[g][:, hs], mh[:], xb[:, h * 64:(h + 1) * 64], start=True, stop=False),
                 reads=[mhk, xbk], writes=[f"yps{g}"])
            P.op("tensor", lambda e, h=h, g=g, ce=ce, hs=hs: e.matmul(y_ps[g][:, hs], ce[:], Sb[:, g, hs], start=False, stop=True),
                 reads=[cek, "Sb"], writes=[f"yps{g}"])
        ys = ysb[i2]; ysk = f"ysb{i2}"
        if d == 0:
            P.op("vector", lambda e: e.tensor_tensor(out=xD[:].rearrange("p (h q) -> p h q", h=16), in0=x3[:].rearrange("p (h q) -> p h q", h=16),
                                                   in1=Dr[:].unsqueeze(2).to_broadcast([128, 16, 64]), op=ALU.mult), reads=[x3k, "Dr"], writes=["xD"])
            for g in range(2):
                P.op("vector", lambda e, g=g: e.tensor_tensor(out=ys[:, g * 512:(g + 1) * 512], in0=y_ps[g][:], in1=xD[:, g * 512:(g + 1) * 512], op=ALU.add),
                     reads=[f"yps{g}", "xD"], writes=[ysk])
        else:
            yp = yprev[i2]; ypk = f"yprev{i2}"
            P.dma("sync", yp[:], y_d[c], reads=[f"y{c}"], writes=[ypk])
            for g in range(2):
                P.op("vector", lambda e, g=g: e.tensor_tensor(out=ys[:, g * 512:(g + 1) * 512], in0=y_ps[g][:], in1=yp[:, g * 512:(g + 1) * 512], op=ALU.add),
                     reads=[f"yps{g}", ypk], writes=[ysk])
        P.dma("sync", y_d[c], ys[:], reads=[ysk], writes=[f"y{c}"])
        for g in range(2):
            P.op("vector", lambda e, g=g: e.tensor_tensor(out=xw[:, g, :].rearrange("p (h q) -> p h q", h=8),
                                                        in0=xb[:, g * 512:(g + 1) * 512].rearrange("p (h q) -> p h q", h=8),
                                                        in1=wst[:, g * 8:(g + 1) * 8].unsqueeze(2).to_broadcast([128, 8, 64]), op=ALU.mult),
                 reads=[xbk, "wst"], writes=["xw"])
            P.op("tensor", lambda e, g=g: e.matmul(sn_ps[:], bc[:, g * 128:(g + 1) * 128], xw[:, g, :], start=True, stop=True), reads=[bck, "xw"], writes=["snps"])
            P.op("vector", lambda e, g=g: e.tensor_tensor(out=S[:, g, :].rearrange("p (h q) -> p h q", h=8), in0=S[:, g, :].rearrange("p (h q) -> p h q", h=8),
                                                        in1=etot[:, g * 8:(g + 1) * 8].unsqueeze(2).to_broadcast([128, 8, 64]), op=ALU.mult),
                 reads=["S", "etot"], writes=["S"])
            P.op("vector", lambda e, g=g: e.tensor_tensor(out=S[:, g, :], in0=S[:, g, :], in1=sn_ps[:], op=ALU.add), reads=["S", "snps"], writes=["S"])
            P.op("scalar", lambda e, g=g: e.copy(out=Sb[:, g, :], in_=S[:, g, :]), reads=["S"], writes=["Sb"])

    for d, order in ((0, fwd_order), (1, bwd_order)):
        P.op("vector", lambda e: e.memset(S[:], 0.0), writes=["S"])
        P.op("vector", lambda e: e.memset(Sb[:], 0.0), writes=["Sb"])
        for c in order:
            chunk(d, c)
    P.finish([P.lastw[f"y{c}"] for c in range(NCHK)])
    print("ssd_s ops", {e: len(v_) for e, v_ in P.ops.items()})
    P.emit()
    return P.nc


def build_ssd_a1(tiles=TILES_STD, NT=NT_STD):
    NOUT = 10368
    P = Prog()
    C = emit_consts(P)
    xin = P.dram("xin", [D, NT], F32, kind="ExternalInput")
    modv_d = P.dram("modv", [128, 96, 2], F32, kind="ExternalInput")
    g_d = P.dram("normg", [128, NCH], F32, kind="ExternalInput")
    w_d = P.dram("w_in", [D, NOUT], F32, kind="ExternalInput")
    z_d = P.dram("ZT", [NOUT, NT], F32, kind="ExternalOutput")
    modv, g = load_layer_consts(P, modv_d, g_d)
    hT = emit_front_h(P, C, xin, tiles, NT, modv, g, 16, 0)
    w = [P.sb([128, NCH, 512], BF16, f"w{i}") for i in range(2)]
    st = [P.sb([128, 4, 512], F32, f"st{i}") for i in range(2)]
    pp = [P.ps([128, 512], F32, f"pp{i}") for i in range(4)]
    cnt = [0, 0]
    wv = w_d.rearrange("(c p) n -> p c n", p=128)
    zv = z_d.rearrange("(o p) t -> p o t", p=128)

    def block(ob):
        c0 = ob * 512; ncols = min(512, NOUT - c0); nch = ncols // 128
        wb = w[ob % 2]; wk = f"w{ob%2}"
        P.dma("gpsimd", wb[:, :, 0:ncols], wv[:, :, c0:c0 + ncols], writes=[wk])
        for (t0, N, s) in tiles:
            sb_ = st[cnt[1] % 2]; sk = f"st{cnt[1]%2}"; cnt[1] += 1
            for j in range(nch):
                p = pp[cnt[0] % 4]; pk = f"pp{cnt[0]%4}"; cnt[0] += 1
                proj_fm(P, hT[:, :, t0:t0 + N], "fhT", N, wb, wk, j * 128, 128, p[:, 0:N], pk)
                P.op("scalar", lambda e, p=p, j=j, N=N, sb_=sb_: e.copy(out=sb_[:, j, 0:N], in_=p[:, 0:N]), reads=[pk], writes=[sk])
            P.dma("sync", zv[:, ob * 4:ob * 4 + nch, t0:t0 + N], sb_[:, 0:nch, 0:N], reads=[sk], writes=["z_out"])
    for ob in range((NOUT + 511) // 512):
        block(ob)
    P.finish([P.lastw["z_out"]])
    P.emit()
    return P.nc


def build_ssd_a2(segs=((0, 256, 0), (260, 8192, 256)), LP=8456, NTS=8448):
    P = Prog()
    xp_d = P.dram("xbc_pre", [1536, LP], F32, kind="ExternalInput")
    cw_d = P.dram("conv_w", [128, 12, 5], F32, kind="ExternalInput")
    cb_d = P.dram("conv_b", [128, 12], F32, kind="ExternalInput")
    dr_d = P.dram("dt_raw", [32, NTS], F32, kind="ExternalInput")
    db_d = P.dram("dt_bias", [32, 1], F32, kind="ExternalInput")
    xo_d = P.dram("xbc_post", [1536, NTS], F32, kind="ExternalOutput")
    dt_d = P.dram("dt", [32, NTS], F32, kind="ExternalOutput")
    cw = P.sb([128, 12, 5], F32, "cw"); cb = P.sb([128, 12], F32, "cb"); db = P.sb([32, 1], F32, "db")
    P.dma("sync", cw[:], cw_d, writes=["cw"]); P.dma("sync", cb[:], cb_d, writes=["cb"]); P.dma("sync", db[:], db_d, writes=["db"])
    BL = 2048
    xin = [P.sb([128, BL + 4], F32, f"xin{i}") for i in range(2)]
    acc = [P.sb([128, BL], F32, f"acc{i}") for i in range(2)]
    dtt = P.sb([32, NTS], F32, "dtt")
    cnt = [0]

    def block(cc, ioff, n, ooff):
        i2 = cnt[0] % 2; cnt[0] += 1
        xi = xin[i2]; xk = f"xin{i2}"; ac = acc[i2]; ak = f"acc{i2}"
        P.dma("sync", xi[:, 0:n + 4], xp_d[cc * 128:(cc + 1) * 128, ioff:ioff + n + 4], writes=[xk])
        P.op("vector", lambda e: e.tensor_scalar(out=ac[:, 0:n], in0=xi[:, 0:n], scalar1=cw[:, cc, 0:1], scalar2=None, op0=ALU.mult), reads=[xk, "cw"], writes=[ak])
        for k in range(1, 5):
            P.op("vector", lambda e, k=k: e.scalar_tensor_tensor(out=ac[:, 0:n], in0=xi[:, k:k + n], scalar=cw[:, cc, k:k + 1], in1=ac[:, 0:n], op0=ALU.mult, op1=ALU.add),
                 reads=[xk, "cw", ak], writes=[ak])
        P.op("scalar", lambda e: e.activation(out=ac[:, 0:n], in_=ac[:, 0:n], func=AF.Silu, bias=cb[:, cc:cc + 1], scale=1.0), reads=[ak, "cb"], writes=[ak])
        P.dma("sync", xo_d[cc * 128:(cc + 1) * 128, ooff:ooff + n], ac[:, 0:n], reads=[ak], writes=["xo_out"])
    for cc in range(12):
        for (ioff, ln, ooff) in segs:
            for b0 in range(0, ln, BL):
                n = min(BL, ln - b0)
                block(cc, ioff + b0, n, ooff + b0)
    P.dma("sync", dtt[:], dr_d, writes=["dtt"])
    P.op("scalar", lambda e: e.activation(out=dtt[:], in_=dtt[:], func=AF.Exp, bias=db[:, 0:1], scale=1.0), reads=["dtt", "db"], writes=["dtt"])
    P.op("scalar", lambda e: e.activation(out=dtt[:], in_=dtt[:], func=AF.Ln, bias=1.0, scale=1.0), reads=["dtt"], writes=["dtt"])
    P.dma("sync", dt_d, dtt[:], reads=["dtt"], writes=["dt_out"])
    P.finish([P.lastw["xo_out"], P.lastw["dt_out"]])
    P.emit()
    return P.nc


def build_ssd_b(qblocks=QB_STD, NT=NT_STD):
    P = Prog()
    C = emit_consts(P)
    xin = P.dram("xin", [D, NT], F32, kind="ExternalInput")
    xout = P.dram("xout", [D, NT], F32, kind="ExternalOutput")
    modv_d = P.dram("modv", [128, 96, 2], F32, kind="ExternalInput")
    y_d = P.dram("yT", [4096, NT], F32, kind="ExternalInput")
    z_d = P.dram("zT", [4096, NT], F32, kind="ExternalInput")
    ng_d = P.dram("norm_g", [128, 32], F32, kind="ExternalInput")
    wo_d = P.dram("w_out", [4096, D], F32, kind="ExternalInput")
    o_scr = P.dram("o_scr", [32, 128, NT], BF16)
    modv = P.sb([128, 96, 2], F32, "modv_sb")
    ng = P.sb([128, 32], F32, "ng")
    P.dma("sync", modv[:], modv_d, writes=["modv"])
    P.dma("sync", ng[:], ng_d, writes=["ng"])
    yv = y_d.rearrange("(c p) t -> p c t", p=128)
    zv = z_d.rearrange("(c p) t -> p c t", p=128)
    P.push_scope()
    ya = P.sb([128, 32, 512], F32, "ya")
    zt = [P.sb([128, 16, 512], F32, f"zt{i}") for i in range(2)]
    sq = P.sb([128, 16, 512], BF16, "sq")
    ob = P.sb([128, 32, 512], BF16, "ob")
    rstd = P.sb([128, 512], F32, "rstd")
    m_ps = P.ps([128, 512], F32, "mps")

    def gate(t0, N, s):
        for hf in range(2):
            cs_ = slice(hf * 16, (hf + 1) * 16)
            z = zt[hf]; zk = f"zt{hf}"
            P.dma("sync", ya[:, cs_, 0:N], yv[:, cs_, t0:t0 + N], writes=[f"ya{hf}"])
            P.dma("sync", z[:, :, 0:N], zv[:, cs_, t0:t0 + N], writes=[zk])
            P.op("scalar", lambda e, z=z: e.activation(out=z[:, :, 0:N], in_=z[:, :, 0:N], func=AF.Silu), reads=[zk], writes=[zk])
            P.op("vector", lambda e, z=z, cs_=cs_: e.tensor_tensor(out=ya[:, cs_, 0:N], in0=ya[:, cs_, 0:N], in1=z[:, :, 0:N], op=ALU.mult), reads=[f"ya{hf}", zk], writes=[f"ya{hf}"])
            P.op("scalar", lambda e, cs_=cs_: e.activation(out=sq[:, :, 0:N], in_=ya[:, cs_, 0:N], func=AF.Square), reads=[f"ya{hf}"], writes=["sq"])
            for c in range(16):
                P.op("tensor", lambda e, c=c, hf=hf: e.matmul(m_ps[:, 0:N], C["ones_b"][:], sq[:, c, 0:N], start=(hf == 0 and c == 0), stop=(hf == 1 and c == 15)),
                     reads=["sq", "ones_b"], writes=["mps"])
        P.op("vector", lambda e: e.tensor_scalar(out=rstd[:, 0:N], in0=m_ps[:, 0:N], scalar1=1.0 / 4096, scalar2=1e-6, op0=ALU.mult, op1=ALU.add), reads=["mps"], writes=["rstd"])
        P.op("scalar", lambda e: e.activation(out=rstd[:, 0:N], in_=rstd[:, 0:N], func=AF.Sqrt), reads=["rstd"], writes=["rstd"])
        P.op("vector", lambda e: e.reciprocal(out=rstd[:, 0:N], in_=rstd[:, 0:N]), reads=["rstd"], writes=["rstd"])
        for hf in range(2):
            cs_ = slice(hf * 16, (hf + 1) * 16)
            P.op("vector", lambda e, cs_=cs_: e.tensor_tensor(out=ya[:, cs_, 0:N], in0=ya[:, cs_, 0:N], in1=rstd[:, 0:N].unsqueeze(1).to_broadcast([128, 16, N]), op=ALU.mult),
                 reads=[f"ya{hf}", "rstd"], writes=[f"ya{hf}"])
            P.op("vector", lambda e, cs_=cs_: e.tensor_tensor(out=ob[:, cs_, 0:N], in0=ya[:, cs_, 0:N], in1=ng[:, cs_].unsqueeze(2).to_broadcast([128, 16, N]), op=ALU.mult),
                 reads=[f"ya{hf}", "ng"], writes=["ob"])
        P.dma("sync", o_scr.rearrange("h p t -> p h t")[:, :, t0:t0 + N], ob[:, :, 0:N], reads=["ob"], writes=["o_scr"])
    for (t0, N, s) in qblocks:
        gate(t0, N, s)
    P.pop_scope()
    P.push_scope()
    wo = [P.sb([128, 32, 128], BF16, f"wo{i}") for i in range(2)]
    ot = [P.sb([128, 32, 512], BF16, f"o{i}") for i in range(2)]
    xt = [P.sb([128, NCH, 512], F32, f"x{i}") for i in range(2)]
    pp = [P.ps([128, 512], F32, f"pp{i}") for i in range(2)]
    xv = xin.rearrange("(c p) t -> p c t", p=128)
    xov = xout.rearrange("(c p) t -> p c t", p=128)
    wov = wo_d.rearrange("(h p) n -> p h n", p=128)
    nb = [0]

    def body(qi, t0, N, s):
        o = ot[qi % 2]; ok = f"o{qi%2}"; x = xt[qi % 2]; xk = f"x{qi%2}"
        P.dma("sync", o[:, :, 0:N], o_scr.rearrange("h p t -> p h t")[:, :, t0:t0 + N], reads=["o_scr"], writes=[ok])
        P.dma("sync", x[:, :, 0:N], xv[:, :, t0:t0 + N], writes=[xk])
        for dc in range(NCH):
            w_ = wo[nb[0] % 2]; wk = f"wo{nb[0]%2}"; p = pp[nb[0] % 2]; pk = f"pp{nb[0]%2}"; nb[0] += 1
            P.dma("gpsimd", w_[:], wov[:, :, dc * 128:(dc + 1) * 128], writes=[wk])
            for h in range(32):
                P.op("tensor", lambda e, h=h, p=p, w_=w_: e.matmul(p[:, 0:N], w_[:, h, :], o[:, h, 0:N], start=(h == 0), stop=(h == 31)), reads=[wk, ok], writes=[pk])
            P.op("vector", lambda e, p=p, dc=dc: e.scalar_tensor_tensor(out=x[:, dc, 0:N], in0=p[:, 0:N], scalar=modv[:, 32 + dc:33 + dc, s], in1=x[:, dc, 0:N],
                                                                     op0=ALU.mult, op1=ALU.add), reads=[pk, "modv", xk], writes=[xk])
        P.dma("sync", xov[:, :, t0:t0 + N], x[:, :, 0:N], reads=[xk], writes=["xout"])
    for qi, (t0, N, s) in enumerate(qblocks):
        body(qi, t0, N, s)
    P.pop_scope()
    P.finish([P.lastw["xout"]])
    P.emit()
    return P.nc


from concourse.bass_utils import run_bass_kernel_spmd

_PROGS = {}
_DEPTH = 4


def _prog(name, fn):
    if name not in _PROGS:
        _PROGS[name] = fn
    return _PROGS[name]()


def _run(nc, in_maps):
    res = run_bass_kernel_spmd(nc, in_maps, core_ids=list(range(8)))
    return res.results


def _rope_tables(pos_r, pos_c, valid):
    n = len(pos_r)
    cosT = np.ones((64, n), np.float32); sinT = np.zeros((64, n), np.float32)
    freqs = (10000.0 ** (-np.arange(16, dtype=np.float32) / 16)).astype(np.float32)
    for f in range(64):
        seg, r = f // 32, f % 32
        i, first = r % 16, r < 16
        pos = pos_r if seg == 0 else pos_c
        ang = pos.astype(np.float32) * freqs[i]
        cosT[f] = np.where(valid, np.cos(ang), 1.0)
        sinT[f] = np.where(valid, (-1.0 if first else 1.0) * np.sin(ang), 0.0)
    return cosT, sinT


_PARTNER = np.array([f + 16 if (f % 32) < 16 else f - 16 for f in range(64)])
_IDENT = np.eye(128, dtype=np.float32)


def _pvec(v):
    return np.ascontiguousarray(np.asarray(v, np.float32).reshape(16, 128).T)


_TRI = np.zeros((2, 128, 128), np.float32)
_jj, _ii = np.meshgrid(np.arange(128), np.arange(128), indexing="ij")
_TRI[0] = (_jj <= _ii); _TRI[1] = (_jj >= _ii)


def ssd_mixer_host(run, xT_cores, mv_cores, gmix, NB, QN, LAT, tiles, qblocks, NT,
                   w_in, conv_w, conv_b, a_log, dt_bias, d_skip, norm_g, w_out):
    f32 = np.float32
    LQ = LAT // QN
    ntc = NB * QN
    nc = build_ssd_a1(tiles, NT)
    w_in = np.ascontiguousarray(np.asarray(w_in, f32))
    oa = run(nc, [{"ident_in": _IDENT, "xin": xT_cores[r], "modv": mv_cores[r], "normg": gmix, "w_in": w_in} for r in range(ntc)], ntc)
    Z = [oa[r]["ZT"] for r in range(ntc)]
    NTS = 256 + LAT
    Zb = []
    for b in range(NB):
        Zb.append(np.concatenate([Z[b * QN][:, LQ:]] + [Z[b * QN + q][:, :LQ] for q in range(QN)], 1))
    LP = 2 + 256 + 2 + 2 + LAT + 2
    segs = ((0, 256, 0), (260, LAT, 256))
    conv_w = np.asarray(conv_w, f32); conv_b = np.asarray(conv_b, f32)
    ims = []
    for b in range(NB):
        for gp in range(4):
            ch = np.concatenate([4096 + np.arange(1024 * gp, 1024 * (gp + 1)), 4096 + 4096 + np.arange(256 * gp, 256 * (gp + 1)),
                                 4096 + 4096 + 1024 + np.arange(256 * gp, 256 * (gp + 1))])
            pre = np.zeros((1536, LP), f32)
            pre[:, 2:258] = Zb[b][ch, :256]
            pre[:, 262:262 + LAT] = Zb[b][ch, 256:]
            cch = ch - 4096
            dtr = np.concatenate([10240 + 16 * gp + np.arange(16), 10240 + 64 + 16 * gp + np.arange(16)])
            dtb = np.concatenate([np.asarray(dt_bias, f32)[0, 16 * gp:16 * gp + 16], np.asarray(dt_bias, f32)[1, 16 * gp:16 * gp + 16]])
            ims.append({"xbc_pre": pre, "conv_w": np.ascontiguousarray(conv_w[:, cch].T.reshape(12, 128, 5).transpose(1, 0, 2)),
                        "conv_b": np.ascontiguousarray(conv_b[cch].reshape(12, 128).T), "dt_raw": np.ascontiguousarray(Zb[b][dtr]),
                        "dt_bias": np.ascontiguousarray(dtb.reshape(32, 1))})
    nc = build_ssd_a2(segs, LP, NTS)
    o2 = run(nc, ims, NB * 4)
    nchk = NTS // 128
    ims = []
    for b in range(NB):
        for gp in range(4):
            xp = o2[b * 4 + gp]["xbc_post"]; dtp = o2[b * 4 + gp]["dt"]
            al = np.concatenate([np.asarray(a_log, f32)[0, 16 * gp:16 * gp + 16], np.asarray(a_log, f32)[1, 16 * gp:16 * gp + 16]])
            ims.append({"ident_in": _IDENT,
                        "x_tm": np.ascontiguousarray(xp[0:1024].T.reshape(nchk, 128, 1024)),
                        "B_tm": np.ascontiguousarray(xp[1024:1280].T.reshape(nchk, 128, 256)),
                        "BT": np.ascontiguousarray(xp[1024:1280].reshape(2, 128, NTS)),
                        "CT": np.ascontiguousarray(xp[1280:1536].reshape(2, 128, NTS)),
                        "dt_tm": np.ascontiguousarray(dtp.T.reshape(nchk, 128, 32)),
                        "alog": np.ascontiguousarray(np.tile(al[None, :], (128, 1))),
                        "Drep": np.ascontiguousarray(np.tile(np.asarray(d_skip, f32)[None, 16 * gp:16 * gp + 16], (128, 1))),
                        "tri": _TRI})
    nc = build_ssd_s(2, LAT // 128)
    o3 = run(nc, ims, NB * 4)
    ng = np.ascontiguousarray(np.asarray(norm_g, f32).reshape(32, 128).T)
    wo = np.ascontiguousarray(np.asarray(w_out, f32))
    ims = []
    for b in range(NB):
        yb = np.concatenate([o3[b * 4 + gp]["y_tm"].reshape(NTS, 1024) for gp in range(4)], 1)
        for q in range(QN):
            r = b * QN + q
            yT = np.ascontiguousarray(np.concatenate([yb[256 + q * LQ:256 + (q + 1) * LQ], yb[:256]], 0).T)
            ims.append({"ident_in": _IDENT, "xin": xT_cores[r], "modv": mv_cores[r], "yT": yT, "zT": np.ascontiguousarray(Z[r][:4096]),
                        "norm_g": ng, "w_out": wo})
    nc = build_ssd_b(qblocks, NT)
    o4 = run(nc, ims, ntc)
    return [o4[r]["xout"] for r in range(ntc)]


def kernel(x, c, ctx, c_ctx, ada_w, ada_b, norm_mix_g, norm_ffn_g, mla_w_a, mla_q_norm_g, mla_kv_norm_g,
           mla_w_uq, mla_w_ukv, mla_w_o, ssd_w_in, ssd_conv_w, ssd_conv_b, ssd_a_log, ssd_dt_bias, ssd_d_skip,
           ssd_norm_g, ssd_w_out, win_w_qkv, win_sinks, win_w_o, peer_w_q, peer_k1, peer_k2, peer_u, peer_v,
           final_norm_g):
    f32 = np.float32
    xs = [np.array(x[b], f32) for b in range(2)]
    cs = [np.array(ctx[b], f32) for b in range(2)]
    c = np.asarray(c, f32); c_ctx = np.asarray(c_ctx, f32)
    cores = [(r // 4, r % 4) for r in range(8)]

    def xT_core(r):
        b, q = cores[r]
        return np.ascontiguousarray(np.concatenate([xs[b][q * 2048:(q + 1) * 2048], cs[b]], 0).T)

    def absorb(outs):
        for r, (b, q) in enumerate(cores):
            o = outs[r]["xout"]
            xs[b][q * 2048:(q + 1) * 2048] = o[:, :2048].T
            if q == 0:
                cs[b] = np.ascontiguousarray(o[:, 2048:].T)

    nc = build_ada()
    ims = []
    for r, (b, q) in enumerate(cores):
        cv = np.stack([c[b], c_ctx], 1).reshape(16, 128, 2).transpose(1, 0, 2)
        ims.append({"cv": np.ascontiguousarray(cv),
                    "ada_w": np.ascontiguousarray(np.asarray(ada_w, f32)[:, :, q * 3072:(q + 1) * 3072]),
                    "ada_b": np.ascontiguousarray(np.asarray(ada_b, f32)[:, q * 3072:(q + 1) * 3072].reshape(4, 24, 128).transpose(0, 2, 1))})
    outs = _run(nc, ims)
    modv = []
    for b in range(2):
        modv.append(np.concatenate([outs[b * 4 + q]["modv_out"] for q in range(4)], 2))

    pos_lat = [np.arange(q * 2048, (q + 1) * 2048) for q in range(4)]
    valid = np.array([True] * 2048 + [False] * 256)
    ropes = []
    for q in range(4):
        p = np.concatenate([pos_lat[q], np.zeros(256, np.int64)])
        ropes.append(_rope_tables(p // 64, p % 64, valid))

    kinds18 = [0] * 16 + [1] * 2
    n_mla = n_ssd = n_win = 0
    for i in range(_DEPTH):
        kind = i % 3
        mv = [np.ascontiguousarray(modv[b][i]) for b in range(2)]
        gmix = _pvec(norm_mix_g[i])
        if kind == 0:
            j = i // 3
            w_a = np.asarray(mla_w_a[j], f32)
            wa_ext = np.ascontiguousarray(np.concatenate([w_a, w_a[:, 1024 + _PARTNER]], 1))
            qkvg = np.ascontiguousarray(np.concatenate([np.asarray(mla_q_norm_g[j], f32), np.asarray(mla_kv_norm_g[j], f32)]).reshape(8, 128).T)
            nc = build_mla_a()
            ims = [{"ident_in": _IDENT, "xin": xT_core(r), "modv": mv[cores[r][0]], "normg": gmix, "w_a": wa_ext, "qkvg": qkvg,
                    "cosT": ropes[cores[r][1]][0], "sinT": ropes[cores[r][1]][1]} for r in range(8)]
            oa = _run(nc, ims)
            ckv_all, kr_all = [], []
            for b in range(2):
                ckv_all.append(np.ascontiguousarray(np.concatenate([oa[b * 4 + q]["cnT"][512:, :2048] for q in range(4)] + [oa[b * 4]["cnT"][512:, 2048:]], 1)))
                kr_all.append(np.ascontiguousarray(np.concatenate([oa[b * 4 + q]["krT"][:, :2048] for q in range(4)] + [oa[b * 4]["krT"][:, 2048:]], 1)))
            wuq_h = np.asarray(mla_w_uq[j], f32).reshape(512, 16, 192).transpose(1, 0, 2)
            wuq_ext = np.ascontiguousarray(np.concatenate([wuq_h, wuq_h[:, :, 128 + _PARTNER]], 2))
            wukv_h = np.ascontiguousarray(np.asarray(mla_w_ukv[j], f32).reshape(512, 16, 256).transpose(1, 0, 2))
            wo = np.ascontiguousarray(np.asarray(mla_w_o[j], f32))
            nc = build_mla_b()
            ims = [{"ident_in": _IDENT, "xin": xT_core(r), "modv": mv[cores[r][0]], "cqT": np.ascontiguousarray(oa[r]["cnT"][:512]),
                    "ckvT": ckv_all[cores[r][0]], "krT": kr_all[cores[r][0]], "cosT": ropes[cores[r][1]][0], "sinT": ropes[cores[r][1]][1],
                    "w_uq": wuq_ext, "w_ukv": wukv_h, "w_o": wo} for r in range(8)]
            absorb(_run(nc, ims))
        elif kind == 1:
            j = i // 3
            run3 = lambda nc_, ims_, n_: run_bass_kernel_spmd(nc_, ims_, core_ids=list(range(n_))).results
            xo = ssd_mixer_host(run3, [xT_core(r) for r in range(8)], [mv[cores[r][0]] for r in range(8)], gmix, 2, 4, 8192,
                                TILES_STD, QB_STD, NT_STD, ssd_w_in[j], ssd_conv_w[j], ssd_conv_b[j], ssd_a_log[j], ssd_dt_bias[j],
                                ssd_d_skip[j], ssd_norm_g[j], ssd_w_out[j])
            absorb([{"xout": o_} for o_ in xo])
        else:
            j = i // 3
            w_qkv = np.asarray(win_w_qkv[j], f32)
            wq, wk, wv = w_qkv[:, :2048], w_qkv[:, 2048:2560], w_qkv[:, 2560:]

            def cws(w, nh):
                ws = w.reshape(2048, nh, 64)[:, :, _PARTNER].reshape(2048, nh * 64)
                return np.stack([w.reshape(2048, -1, 128), ws.reshape(2048, -1, 128)], 2).reshape(2048, -1, 256)
            wqk = np.ascontiguousarray(np.concatenate([cws(wq, 32), cws(wk, 8)], 1))
            nc = build_win_a()
            ims = [{"ident_in": _IDENT, "xin": xT_core(r), "modv": mv[cores[r][0]], "normg": gmix, "w_qk": wqk, "w_v": np.ascontiguousarray(wv),
                    "cos2": np.ascontiguousarray(np.concatenate([ropes[cores[r][1]][0]] * 2, 0)),
                    "sin2": np.ascontiguousarray(np.concatenate([ropes[cores[r][1]][1]] * 2, 0))} for r in range(8)]
            oa = _run(nc, ims)
            wo = np.ascontiguousarray(np.asarray(win_w_o[j], f32))
            sk = np.ascontiguousarray(np.tile(np.asarray(win_sinks[j], f32)[None, :], (64, 1)))
            ims = []
            for r, (b, q) in enumerate(cores):
                KT = oa[r]["QKT"][2048:]; V = oa[r]["V"]
                if q > 0:
                    kp, vp = oa[r - 1]["QKT"][2048:, 1920:2048], oa[r - 1]["V"][1920:2048]
                else:
                    kp, vp = np.zeros((512, 128), f32), np.zeros((128, 512), f32)
                if q < 3:
                    kn, vn = oa[r + 1]["QKT"][2048:, 0:128], oa[r + 1]["V"][0:128]
                else:
                    kn, vn = np.zeros((512, 128), f32), np.zeros((128, 512), f32)
                KT_ext = np.ascontiguousarray(np.concatenate([kp, KT[:, :2048], kn, KT[:, 2048:]], 1))
                V_ext = np.ascontiguousarray(np.concatenate([vp, V[:2048], vn, V[2048:]], 0))
                masks = np.zeros((5, 6, 128, 512), f32)
                for bq in range(4):
                    qp = 512 * bq + np.arange(512)
                    for jj in range(6):
                        e_ = 4 * bq + jj
                        if (e_ == 0 and q == 0) or (e_ == 17 and q == 3):
                            continue
                        kpz = (e_ - 1) * 128 + np.arange(128)
                        masks[bq, jj] = (np.abs(kpz[:, None] - qp[None, :]) <= 128)
                ims.append({"ident_in": _IDENT, "xin": xT_core(r), "modv": mv[b], "QT": np.ascontiguousarray(oa[r]["QKT"][:2048]),
                            "KT": KT_ext, "V": V_ext, "masks": masks, "sinks": sk, "w_o": wo})
            nc = build_win_b()
            absorb(_run(nc, ims))
        nc = build_peer(kinds18, 2304)
        uT = np.ascontiguousarray(np.asarray(peer_u[i], f32).T)
        vv = np.ascontiguousarray(np.asarray(peer_v[i], f32))
        wq_ = np.ascontiguousarray(np.asarray(peer_w_q[i], f32))
        k1T = np.ascontiguousarray(np.asarray(peer_k1[i], f32).transpose(0, 2, 1))
        k2T = np.ascontiguousarray(np.asarray(peer_k2[i], f32).transpose(0, 2, 1))
        gffn = _pvec(norm_ffn_g[i])
        ims = [{"ident_in": _IDENT, "xin": xT_core(r), "modv": mv[cores[r][0]], "normg": gffn, "wq": wq_, "k1T": k1T, "k2T": k2T,
                "uT": uT, "v": vv} for r in range(8)]
        absorb(_run(nc, ims))
    nc = build_final()
    gf = _pvec(final_norm_g)
    ims = [{"ident_in": _IDENT, "xin": np.ascontiguousarray(xs[b][q * 2048:(q + 1) * 2048].T), "normg": gf} for (b, q) in cores]
    outs = _run(nc, ims)
    out = np.zeros((2, 8192, 2048), f32)
    for r, (b, q) in enumerate(cores):
        out[b, q * 2048:(q + 1) * 2048] = outs[r]["xout"].T
    return out
```

```python
import numpy as np
from contextlib import ExitStack
import concourse.bass as bass
import concourse.mybir as mybir

F32 = mybir.dt.float32
F32R = mybir.dt.float32r
BF16 = mybir.dt.bfloat16
U32 = mybir.dt.uint32
AF = mybir.ActivationFunctionType
ALU = mybir.AluOpType
AX = mybir.AxisListType

ENGS = ["tensor", "vector", "scalar", "gpsimd", "sync"]
NDMA_SEMS = 12


class Prog:
    def __init__(self):
        self.nc = bass.Bass("TRN2", target_bir_lowering=False)
        self.es = ExitStack()
        self.ops = {e: [] for e in ENGS}
        self.cnt = {e: 0 for e in ENGS}
        self.sems = {}
        for e in ENGS:
            self.sems["c_" + e] = self.es.enter_context(self.nc.semaphore("c_" + e))
        self.dma_sems = {}
        self.dma_n = {}
        for q in ["sync", "gpsimd", "scalar"]:
            self.dma_sems[q] = [self.es.enter_context(self.nc.semaphore(f"d_{q}{i}")) for i in range(NDMA_SEMS)]
            self.dma_n[q] = 0
        self.lastw = {}
        self.readers = {}
        self.waited = {e: {} for e in ENGS}
        self.semobj = {}
        for k, v in self.sems.items():
            self.semobj[k] = v
        for q, lst in self.dma_sems.items():
            for i, s in enumerate(lst):
                self.semobj[f"d_{q}{i}"] = s
        self.ntiles = 0
        self.sb_bytes = 0
        self.cur = self.es
        self.scopes = []

    def sb(self, shape, dt=F32, name=None):
        self.ntiles += 1
        name = (name or "t") + f"_{self.ntiles}"
        per = int(np.prod(shape[1:])) * (2 if dt == BF16 else 4)
        self.sb_bytes += per
        return self.cur.enter_context(self.nc.sbuf_tensor(name, list(shape), dt))

    def ps(self, shape, dt=F32, name=None):
        self.ntiles += 1
        name = (name or "p") + f"_{self.ntiles}"
        full = self.cur.enter_context(self.nc.psum_tensor(name, [128, 512], F32))
        ap = full[:]
        if dt == BF16:
            ap = ap.bitcast(BF16)
        n = int(np.prod(shape[1:]))
        ap = ap[0:shape[0], 0:n]
        if len(shape) == 3:
            ap = ap.rearrange("p (a b) -> p a b", a=shape[1])
        elif len(shape) == 4:
            ap = ap.rearrange("p (a b c) -> p a b c", a=shape[1], b=shape[2])
        return ap

    def push_scope(self):
        self.scopes.append((self.cur, self.sb_bytes))
        self.cur = ExitStack()

    def pop_scope(self):
        self.barrier()
        self.cur.close()
        self.cur, self.sb_bytes = self.scopes.pop()

    def barrier(self):
        evs = []
        for e in ENGS:
            if self.cnt[e] > 0:
                evs.append(("c_" + e, self.cnt[e], e))
        for q in self.dma_sems:
            n = self.dma_n[q]
            for si in range(NDMA_SEMS):
                k = (n - 1 - si) // NDMA_SEMS + 1 if n > si else 0
                if k > 0:
                    evs.append((f"d_{q}{si}", 16 * k, "dma"))
        for e in ENGS:
            waits = []
            for ev in evs:
                self._need_force(e, ev, waits)
            if waits:
                self.ops[e].append((waits, None, None))

    def dram(self, name, shape, dt=F32, kind="Internal"):
        return self.nc.dram_tensor(name, list(shape), dt, kind=kind).ap()

    def _need(self, eng, ev, waits):
        if ev is None:
            return
        sem, val, src = ev
        if src == "tensor" and eng == "tensor":
            return
        if self.waited[eng].get(sem, 0) >= val:
            return
        self.waited[eng][sem] = val
        waits.append((sem, val))

    def _need_force(self, eng, ev, waits):
        sem, val, src = ev
        if self.waited[eng].get(sem, 0) >= val:
            return
        self.waited[eng][sem] = val
        waits.append((sem, val))

    def op(self, eng, fn, reads=(), writes=()):
        waits = []
        for k in reads:
            self._need(eng, self.lastw.get(k), waits)
        for k in writes:
            self._need(eng, self.lastw.get(k), waits)
            for ev in self.readers.get(k, ()):
                self._need(eng, ev, waits)
        self.cnt[eng] += 1
        ev = ("c_" + eng, self.cnt[eng], eng)
        for k in reads:
            self.readers.setdefault(k, []).append(ev)
        for k in writes:
            self.lastw[k] = ev
            self.readers[k] = []
        self.ops[eng].append((waits, fn, ("c_" + eng, 1)))
        return ev

    def dma(self, q, out, in_, reads=(), writes=(), **kw):
        waits = []
        n = self.dma_n[q]
        self.dma_n[q] += 1
        si = n % NDMA_SEMS
        sem = f"d_{q}{si}"
        rnd = n // NDMA_SEMS
        if rnd > 0:
            self._need(q, (sem, 16 * rnd, "dma"), waits)
        for k in reads:
            self._need(q, self.lastw.get(k), waits)
        for k in writes:
            self._need(q, self.lastw.get(k), waits)
            for ev in self.readers.get(k, ()):
                self._need(q, ev, waits)
        ev = (sem, 16 * (rnd + 1), "dma")
        for k in reads:
            self.readers.setdefault(k, []).append(ev)
        for k in writes:
            self.lastw[k] = ev
            self.readers[k] = []
        fn = lambda e, out=out, in_=in_, kw=kw: e.dma_start(out=out, in_=in_, **kw)
        self.ops[q].append((waits, fn, (sem, 16)))
        return ev

    def finish(self, final_events):
        waits = []
        for ev in final_events:
            self._need("sync", ev, waits)
        self.ops["sync"].append((waits, None, None))

    def emit(self):
        nc = self.nc
        with nc.Block() as block:
            def mk(engname):
                def body(e):
                    for waits, fn, inc in self.ops[engname]:
                        for sem, val in waits:
                            e.wait_ge(self.semobj[sem], val)
                        if fn is not None:
                            ins = fn(e)
                            ins.then_inc(self.semobj[inc[0]], inc[1])
                return body
            block.tensor(mk("tensor"))
            block.vector(mk("vector"))
            block.scalar(mk("scalar"))
            block.gpsimd(mk("gpsimd"))
            block.sync(mk("sync"))
        self.es.close()
        return nc


D = 2048
NCH = 16
NEXP = 16384
EB = 256
KPB = EB // 128
NEB = NEXP // EB
PEER_GATE_ENGINE = "vector"


def emit_consts(P):
    C = {}
    C["ident_f"] = P.sb([128, 128], F32, "ident_f")
    C["ident_b"] = P.sb([128, 128], BF16, "ident_b")
    C["ones_b"] = P.sb([128, 128], BF16, "ones_b")
    ident_d = P.dram("ident_in", [128, 128], F32, kind="ExternalInput")
    P.dma("sync", C["ident_f"][:], ident_d, writes=["ident_f"])
    P.op("vector", lambda e: e.tensor_copy(out=C["ident_b"][:], in_=C["ident_f"][:]), reads=["ident_f"], writes=["ident_b"])
    P.op("vector", lambda e: e.memset(C["ones_b"][:], 1.0), writes=["ones_b"])
    return C


def emit_peer(P, C, xin, xout, tile_kinds, modv, normg, wq_d, k1T_d, k2T_d, uT_d, v_d, pfx="pe"):
    nc = P.nc
    nt = len(tile_kinds)
    xin_v = xin.rearrange("(c p) t -> p c t", p=128)
    xout_v = xout.rearrange("(c p) t -> p c t", p=128)
    wq_v = wq_d.rearrange("(c p) n -> p c n", p=128)
    u16 = P.dram(pfx + "u16", [D, NEXP], BF16)
    v16 = P.dram(pfx + "v16", [NEXP, D], BF16)
    for i in range(16):
        P.dma("gpsimd", u16[i * 128:(i + 1) * 128, :], uT_d[i * 128:(i + 1) * 128, :], writes=[pfx + "u16"])
    for i in range(16):
        P.dma("gpsimd", v16[i * 1024:(i + 1) * 1024, :], v_d[i * 1024:(i + 1) * 1024, :], writes=[pfx + "v16"])
    uT_v = u16.rearrange("(c p) e -> p c e", p=128)
    v_v = v16.rearrange("(b k p) d -> b p k d", p=128, k=KPB)

    modA = P.sb([128, 2, NCH], F32, pfx + "modA")
    for s in range(2):
        P.op("vector", lambda e, s=s: e.scalar_tensor_tensor(
            out=modA[:, s, :], in0=modv[:, 64:80, s], scalar=1.0, in1=normg[:, :], op0=ALU.add, op1=ALU.mult),
            reads=["modv", "normg"], writes=[pfx + "modA"])
    k1T = P.sb([128, 8, 128], BF16, pfx + "k1T")
    k2T = P.sb([128, 8, 128], BF16, pfx + "k2T")
    P.dma("gpsimd", k1T[:], k1T_d.rearrange("h d k -> d h k"), writes=[pfx + "k1T"])
    P.dma("gpsimd", k2T[:], k2T_d.rearrange("h d k -> d h k"), writes=[pfx + "k2T"])

    x_t = [P.sb([128, NCH, 128], F32, f"{pfx}x{i}") for i in range(2)]
    sq = P.sb([128, NCH, 128], BF16, pfx + "sq")
    xo1 = P.sb([128, NCH, 128], F32, pfx + "xo")
    tmpf = P.sb([128, NCH, 128], F32, pfx + "tmpf")
    fo = tmpf[:].rearrange("p c t -> p (c t)")
    rstd = P.sb([128, 128], F32, pfx + "rstd")
    hTs = [P.sb([128, NCH, 128], BF16, f"{pfx}hT{i}") for i in range(2)]
    wq_t = [P.sb([128, NCH, 256], BF16, f"{pfx}wq{i}") for i in range(2)]
    qT = P.sb([128, 16, 128], BF16, pfx + "qT")
    S = P.sb([128, 16, 128], F32, pfx + "S")
    E = S
    t16 = P.sb([128, 16, 16], F32, pfx + "t16")
    e16 = P.sb([128, 16, 16], F32, pfx + "e16")
    negm = P.sb([128, 16], F32, pfx + "negm")
    mr = P.sb([128, 128], F32, pfx + "mr")
    cand = P.sb([128, 256], F32, pfx + "cand")
    cand2 = P.sb([128, 256], F32, pfx + "cand2")
    c16 = P.sb([128, 8, 16], F32, pfx + "c16")
    Zs = P.sb([128, 8], F32, pfx + "Zs")
    rZ = P.sb([128, 8], F32, pfx + "rZ")
    NPI = 8
    PI = 128 // NPI
    Pps = [P.sb([128, PI, 128], F32, f"{pfx}Pp{i}") for i in range(1)]
    Gs_ = [P.sb([128, NEXP], BF16, f"{pfx}G{i}") for i in range(2)]
    u_t = [P.sb([128, NCH, EB], BF16, f"{pfx}u{i}") for i in range(2)]
    v_t = [P.sb([128, KPB, D], BF16, f"{pfx}v{i}") for i in range(2)]
    gl = [P.sb([128, EB], F32, f"{pfx}gl{i}") for i in range(2)]
    Wb = [P.sb([128, EB], BF16, f"{pfx}Wb{i}") for i in range(2)]
    WT = [P.sb([128, KPB, 128], BF16, f"{pfx}WT{i}") for i in range(2)]
    out_ps = [P.ps([128, 512], F32, f"{pfx}ops{i}") for i in range(4)]
    a_ps = [P.ps([128, 512], F32, f"{pfx}aps{i}") for i in range(2)]
    t_ps = P.ps([128, KPB, 128], BF16, pfx + "tps")
    m_ps = P.ps([128, 512], F32, pfx + "mps")
    GENG = PEER_GATE_ENGINE

    def front(t):
        s = tile_kinds[t]
        xb = x_t[t % 2]; xk = f"{pfx}x{t%2}"
        hT = hTs[t % 2]; hk = f"{pfx}hT{t%2}"
        G = Gs_[t % 2]; gpre = f"{pfx}G{t%2}_"
        P.dma("sync", xb[:], xin_v[:, :, t * 128:(t + 1) * 128], writes=[xk])
        P.op("scalar", lambda e: e.activation(out=sq[:], in_=xb[:], func=AF.Square), reads=[xk], writes=[pfx + "sq"])
        for c in range(NCH):
            P.op("tensor", lambda e, c=c: e.matmul(m_ps[:, 0:128], C["ones_b"][:], sq[:, c, :], start=(c == 0), stop=(c == NCH - 1)),
                 reads=[pfx + "sq", "ones_b"], writes=[pfx + "mps"])
        P.op("vector", lambda e: e.tensor_scalar(out=rstd[:], in0=m_ps[:, 0:128], scalar1=1.0 / D, scalar2=1e-6, op0=ALU.mult, op1=ALU.add),
             reads=[pfx + "mps"], writes=[pfx + "rstd"])
        P.op("scalar", lambda e: e.activation(out=rstd[:], in_=rstd[:], func=AF.Sqrt), reads=[pfx + "rstd"], writes=[pfx + "rstd"])
        P.op("vector", lambda e: e.reciprocal(out=rstd[:], in_=rstd[:]), reads=[pfx + "rstd"], writes=[pfx + "rstd"])
        P.op("vector", lambda e: e.tensor_tensor(out=tmpf[:], in0=xb[:], in1=rstd[:].unsqueeze(1).to_broadcast([128, NCH, 128]), op=ALU.mult),
             reads=[xk, pfx + "rstd"], writes=[pfx + "tmpf"])
        P.op("vector", lambda e: e.tensor_tensor(out=tmpf[:], in0=tmpf[:], in1=modA[:, s, :].unsqueeze(2).to_broadcast([128, NCH, 128]), op=ALU.mult),
             reads=[pfx + "tmpf", pfx + "modA"], writes=[pfx + "tmpf"])
        P.op("vector", lambda e: e.tensor_tensor(out=hT[:], in0=tmpf[:], in1=modv[:, 48:64, s].unsqueeze(2).to_broadcast([128, NCH, 128]), op=ALU.add),
             reads=[pfx + "tmpf", "modv"], writes=[hk])
        yield
        for nb in range(8):
            wb = wq_t[nb % 2]; wk = f"{pfx}wq{nb%2}"
            P.dma("gpsimd", wb[:], wq_v[:, :, nb * 256:(nb + 1) * 256], writes=[wk])
            for jj in range(2):
                for c in range(NCH):
                    P.op("tensor", lambda e, wb=wb, jj=jj, c=c: e.matmul(m_ps[:, jj * 128:(jj + 1) * 128], wb[:, c, jj * 128:(jj + 1) * 128], hT[:, c, :],
                                                                       start=(c == 0), stop=(c == NCH - 1)),
                         reads=[wk, hk], writes=[pfx + "mps"])
            P.op("scalar", lambda e, nb=nb: e.copy(out=qT[:, nb * 2:(nb + 1) * 2, :], in_=m_ps[:, 0:256].rearrange("p (j t) -> p j t", j=2)),
                 reads=[pfx + "mps"], writes=[pfx + "qT"])
            yield
        for jb in range(4):
            for jj in range(4):
                j = jb * 4 + jj
                h, side = j // 2, j % 2
                kT = k1T if side == 0 else k2T
                P.op("tensor", lambda e, j=j, jj=jj, kT=kT, h=h: e.matmul(m_ps[:, jj * 128:(jj + 1) * 128], qT[:, j, :], kT[:, h, :], start=True, stop=True),
                     reads=[pfx + "qT", pfx + "k1T", pfx + "k2T"], writes=[pfx + "mps"])
            P.op("scalar", lambda e, jb=jb: e.copy(out=S[:, jb * 4:(jb + 1) * 4, :], in_=m_ps[:].rearrange("p (j t) -> p j t", j=4)),
                 reads=[pfx + "mps"], writes=[pfx + "S"])
            yield
        for j in range(16):
            P.op("vector", lambda e, j=j: e.max(out=t16[:, j, 0:8], in_=S[:, j, :]), reads=[pfx + "S"], writes=[pfx + "t16"])
            P.op("vector", lambda e, j=j: e.match_replace(out=mr[:], in_to_replace=t16[:, j, 0:8], in_values=S[:, j, :], imm_value=-1e30),
                 reads=[pfx + "S", pfx + "t16"], writes=[pfx + "mr"])
            P.op("vector", lambda e, j=j: e.max(out=t16[:, j, 8:16], in_=mr[:]), reads=[pfx + "mr"], writes=[pfx + "t16"])
            if j % 2 == 1:
                yield
        P.op("vector", lambda e: e.tensor_scalar(out=negm[:], in0=t16[:, :, 0], scalar1=-1.0, scalar2=None, op0=ALU.mult),
             reads=[pfx + "t16"], writes=[pfx + "negm"])
        for j in range(16):
            P.op("scalar", lambda e, j=j: e.activation(out=E[:, j, :], in_=S[:, j, :], func=AF.Exp, bias=negm[:, j:j + 1], scale=1.0),
                 reads=[pfx + "S", pfx + "negm"], writes=[pfx + "S"])
            P.op("scalar", lambda e, j=j: e.activation(out=e16[:, j, :], in_=t16[:, j, :], func=AF.Exp, bias=negm[:, j:j + 1], scale=1.0),
                 reads=[pfx + "t16", pfx + "negm"], writes=[pfx + "e16"])
            if j % 4 == 3:
                yield
        for h in range(8):
            P.op("vector", lambda e, h=h: e.tensor_tensor(out=cand[:].rearrange("p (a b) -> p a b", a=16),
                                                        in0=e16[:, 2 * h, :].unsqueeze(2).to_broadcast([128, 16, 16]),
                                                        in1=e16[:, 2 * h + 1, :].unsqueeze(1).to_broadcast([128, 16, 16]), op=ALU.mult),
                 reads=[pfx + "e16"], writes=[pfx + "cand"])
            P.op("vector", lambda e, h=h: e.max(out=c16[:, h, 0:8], in_=cand[:]), reads=[pfx + "cand"], writes=[pfx + "c16"])
            P.op("vector", lambda e, h=h: e.match_replace(out=cand2[:], in_to_replace=c16[:, h, 0:8], in_values=cand[:], imm_value=-1.0),
                 reads=[pfx + "cand", pfx + "c16"], writes=[pfx + "cand2"])
            P.op("vector", lambda e, h=h: e.max(out=c16[:, h, 8:16], in_=cand2[:]), reads=[pfx + "cand2"], writes=[pfx + "c16"])
            if h % 2 == 1:
                yield
        P.op("vector", lambda e: e.tensor_reduce(out=Zs[:], in_=c16[:], axis=AX.X, op=ALU.add), reads=[pfx + "c16"], writes=[pfx + "Zs"])
        P.op("vector", lambda e: e.reciprocal(out=rZ[:], in_=Zs[:]), reads=[pfx + "Zs"], writes=[pfx + "rZ"])
        for h in range(8):
            for q4 in range(NPI):
                pidx = (h * NPI + q4) % len(Pps)
                Pp = Pps[pidx]; pk = f"{pfx}Pp{pidx}"
                gk = gpre + str((q4 * PI * 128) // 4096)
                Gs = G[:, q4 * PI * 128:(q4 + 1) * PI * 128].rearrange("p (a b) -> p a b", a=PI)
                P.op("vector", lambda e, h=h, q4=q4, Pp=Pp: e.tensor_tensor(
                    out=Pp[:], in0=E[:, 2 * h, q4 * PI:(q4 + 1) * PI].unsqueeze(2).to_broadcast([128, PI, 128]),
                    in1=E[:, 2 * h + 1, :].unsqueeze(1).to_broadcast([128, PI, 128]), op=ALU.mult),
                    reads=[pfx + "S"], writes=[pk])
                P.op("vector", lambda e, h=h, Pp=Pp: e.scalar_tensor_tensor(
                    out=Pp[:], in0=Pp[:], scalar=c16[:, h, 15:16], in1=Pp[:], op0=ALU.is_ge, op1=ALU.mult),
                    reads=[pk, pfx + "c16"], writes=[pk])
                if h == 0:
                    P.op("vector", lambda e, h=h, Gs=Gs, Pp=Pp: e.tensor_scalar(
                        out=Gs, in0=Pp[:], scalar1=rZ[:, h:h + 1], scalar2=None, op0=ALU.mult),
                        reads=[pk, pfx + "rZ"], writes=[gk])
                else:
                    P.op("vector", lambda e, h=h, Gs=Gs, Pp=Pp: e.scalar_tensor_tensor(
                        out=Gs, in0=Pp[:], scalar=rZ[:, h:h + 1], in1=Gs, op0=ALU.mult, op1=ALU.add),
                        reads=[pk, pfx + "rZ", gk], writes=[gk])
                yield

    ublk = [0]

    def main(t, gen):
        s = tile_kinds[t]
        hT = hTs[t % 2]; hk = f"{pfx}hT{t%2}"
        G = Gs_[t % 2]; gpre = f"{pfx}G{t%2}_"

        def bufs(i):
            j = i % 2
            return (u_t[j], f"{pfx}u{j}", v_t[j], f"{pfx}v{j}", a_ps[j], f"{pfx}aps{j}", gl[j], f"{pfx}gl{j}", Wb[j], f"{pfx}Wb{j}", WT[j], f"{pfx}WT{j}")

        def stageA(b):
            ub, uk, vb, vk, ap_, ak = bufs(ublk[0] + b)[:6]
            P.dma("sync", ub[:], uT_v[:, :, b * EB:(b + 1) * EB], reads=[pfx + "u16"], writes=[uk])
            P.dma("gpsimd", vb[:], v_v[b], reads=[pfx + "v16"], writes=[vk])
            for c in range(NCH):
                P.op("tensor", lambda e, c=c: e.matmul(ap_[:, 0:EB], hT[:, c, :], ub[:, c, :], start=(c == 0), stop=(c == NCH - 1)),
                     reads=[hk, uk], writes=[ak])

        def blk(b):
            ub, uk, vb, vk, ap_, ak, glb, glk, wbb, wbk, wtb, wtk = bufs(ublk[0] + b)
            P.op("scalar", lambda e: e.activation(out=glb[:], in_=ap_[:, 0:EB], func=AF.Gelu_apprx_tanh), reads=[ak], writes=[glk])
            P.op("vector", lambda e: e.tensor_tensor(out=wbb[:], in0=glb[:], in1=G[:, b * EB:(b + 1) * EB], op=ALU.mult),
                 reads=[glk, gpre + str((b * EB) // 4096)], writes=[wbk])
            if b + 1 < NEB:
                stageA(b + 1)
            for k in range(KPB):
                P.op("tensor", lambda e, k=k: e.transpose(out=t_ps[:, k, :], in_=wbb[:, k * 128:(k + 1) * 128], identity=C["ident_b"][:]),
                     reads=[wbk, "ident_b"], writes=[pfx + "tps"])
            P.op("scalar", lambda e: e.copy(out=wtb[:], in_=t_ps[:]), reads=[pfx + "tps"], writes=[wtk])
            for k in range(KPB):
                for n in range(4):
                    P.op("tensor", lambda e, k=k, n=n: e.matmul(out_ps[n][:], wtb[:, k, :], vb[:, k, n * 512:(n + 1) * 512],
                                                              start=(b == 0 and k == 0), stop=(b == NEB - 1 and k == KPB - 1)),
                         reads=[wtk, vk], writes=[f"{pfx}ops{n}"])

        stageA(0)
        for b in range(NEB):
            blk(b)
            if gen is not None:
                for _ in range(2):
                    next(gen, None)
        ublk[0] += NEB
        if gen is not None:
            for _ in gen:
                pass

    def epilogue(t):
        s = tile_kinds[t]
        xb = x_t[t % 2]; xk = f"{pfx}x{t%2}"
        for n in range(4):
            P.op("scalar", lambda e, n=n: e.copy(out=fo[:, n * 512:(n + 1) * 512], in_=out_ps[n][:]), reads=[f"{pfx}ops{n}"], writes=[pfx + "tmpf"])
        xob = xo1; xok = pfx + "xo"
        for cb in range(4):
            for cc in range(4):
                c = cb * 4 + cc
                P.op("tensor", lambda e, c=c, cc=cc: e.transpose(out=m_ps[:, cc * 128:(cc + 1) * 128], in_=fo[:, c * 128:(c + 1) * 128], identity=C["ident_f"][:]),
                     reads=[pfx + "tmpf", "ident_f"], writes=[pfx + "mps"])
            for cc in range(4):
                c = cb * 4 + cc
                P.op("vector", lambda e, c=c, cc=cc: e.scalar_tensor_tensor(
                    out=xob[:, c, :], in0=m_ps[:, cc * 128:(cc + 1) * 128], scalar=modv[:, 80 + c:81 + c, s], in1=xb[:, c, :], op0=ALU.mult, op1=ALU.add),
                    reads=[pfx + "mps", "modv", xk], writes=[xok])
        P.dma("sync", xout_v[:, :, t * 128:(t + 1) * 128], xob[:], reads=[xok], writes=[pfx + "xout"])

    for _ in front(0):
        pass
    for t in range(nt):
        gen = front(t + 1) if t + 1 < nt else None
        main(t, gen)
        epilogue(t)
    return pfx + "xout"


D = 2048
NCH = 16
DEBUG = False


def load_layer_consts(P, modv_d, g_d, pfx=""):
    modv = P.sb([128, 96, 2], F32, pfx + "modv_sb")
    g = P.sb([128, NCH], F32, pfx + "g_sb")
    P.dma("sync", modv[:], modv_d, writes=["modv"])
    P.dma("sync", g[:], g_d, writes=["normg"])
    return modv, g


def make_modA(P, modv, g, sc_off, pfx):
    modA = P.sb([128, 2, NCH], F32, pfx + "modA")
    for s in range(2):
        P.op("vector", lambda e, s=s: e.scalar_tensor_tensor(
            out=modA[:, s, :], in0=modv[:, sc_off:sc_off + 16, s], scalar=1.0, in1=g[:, :], op0=ALU.add, op1=ALU.mult),
            reads=["modv", "normg"], writes=[pfx + "modA"])
    return modA


def rms_rstd(P, C, src, srck, nch, N, dim, sq, sqk, m_ps, mk, rstd, rk):
    P.op("scalar", lambda e: e.activation(out=sq[:, 0:nch, 0:N], in_=src[:, 0:nch, 0:N], func=AF.Square), reads=[srck], writes=[sqk])
    for c in range(nch):
        P.op("tensor", lambda e, c=c: e.matmul(m_ps[:, 0:N], C["ones_b"][:], sq[:, c, 0:N], start=(c == 0), stop=(c == nch - 1)),
             reads=[sqk, "ones_b"], writes=[mk])
    P.op("vector", lambda e: e.tensor_scalar(out=rstd[:, 0:N], in0=m_ps[:, 0:N], scalar1=1.0 / dim, scalar2=1e-6, op0=ALU.mult, op1=ALU.add),
         reads=[mk], writes=[rk])
    P.op("scalar", lambda e: e.activation(out=rstd[:, 0:N], in_=rstd[:, 0:N], func=AF.Sqrt), reads=[rk], writes=[rk])
    P.op("vector", lambda e: e.reciprocal(out=rstd[:, 0:N], in_=rstd[:, 0:N]), reads=[rk], writes=[rk])


def normmod(P, C, xb, xk, N, s, modA, modAk, modv, sh_off, tmp, tk, sq, sqk, m_ps, mk, rstd, rk, hT, hk):
    rms_rstd(P, C, xb, xk, NCH, N, D, sq, sqk, m_ps, mk, rstd, rk)
    P.op("vector", lambda e: e.tensor_tensor(out=tmp[:, :, 0:N], in0=xb[:, :, 0:N], in1=rstd[:, 0:N].unsqueeze(1).to_broadcast([128, NCH, N]), op=ALU.mult),
         reads=[xk, rk], writes=[tk])
    P.op("vector", lambda e: e.tensor_tensor(out=tmp[:, :, 0:N], in0=tmp[:, :, 0:N], in1=modA[:, s, :].unsqueeze(2).to_broadcast([128, NCH, N]), op=ALU.mult),
         reads=[tk, modAk], writes=[tk])
    P.op("vector", lambda e: e.tensor_tensor(out=hT[:, :, 0:N], in0=tmp[:, :, 0:N], in1=modv[:, sh_off:sh_off + 16, s].unsqueeze(2).to_broadcast([128, NCH, N]), op=ALU.add),
         reads=[tk, "modv"], writes=[hk])


def build_ada():
    P = Prog()
    cv_d = P.dram("cv", [128, NCH, 2], F32, kind="ExternalInput")
    w_d = P.dram("ada_w", [4, D, 3072], F32, kind="ExternalInput")
    b_d = P.dram("ada_b", [4, 128, 24], F32, kind="ExternalInput")
    o_d = P.dram("modv_out", [4, 128, 24, 2], F32, kind="ExternalOutput")
    cv = P.sb([128, NCH, 2], F32, "cv")
    sc = P.sb([128, NCH, 2], BF16, "sc")
    bb = P.sb([128, 4, 24], F32, "bb")
    res = P.sb([128, 4, 24, 2], F32, "res")
    wt = [P.sb([128, NCH, 512], BF16, f"w{i}") for i in range(2)]
    ps = [P.ps([128, 4, 2], F32, f"ps{i}") for i in range(2)]
    P.dma("sync", cv[:], cv_d, writes=["cv"])
    P.dma("sync", bb[:], b_d.rearrange("l p j -> p l j"), writes=["bb"])
    P.op("scalar", lambda e: e.activation(out=sc[:], in_=cv[:], func=AF.Silu), reads=["cv"], writes=["sc"])
    n = 0
    for l in range(4):
        wv = w_d[l].rearrange("(c p) n -> p c n", p=128)
        for jb in range(6):
            w = wt[n % 2]; wk = f"w{n%2}"; pt = ps[n % 2]; pk = f"ps{n%2}"; n += 1
            P.dma("gpsimd", w[:], wv[:, :, jb * 512:(jb + 1) * 512], writes=[wk])
            for jj in range(4):
                for c in range(NCH):
                    P.op("tensor", lambda e, w=w, pt=pt, jj=jj, c=c: e.matmul(pt[:, jj, :], w[:, c, jj * 128:(jj + 1) * 128], sc[:, c, :],
                                                                         start=(c == 0), stop=(c == NCH - 1)), reads=[wk, "sc"], writes=[pk])
            P.op("vector", lambda e, pt=pt, l=l, jb=jb: e.tensor_tensor(
                out=res[:, l, jb * 4:(jb + 1) * 4, :], in0=pt[:], in1=bb[:, l, jb * 4:(jb + 1) * 4].unsqueeze(2).to_broadcast([128, 4, 2]), op=ALU.add),
                reads=[pk, "bb"], writes=["res"])
    ev = P.dma("sync", o_d.rearrange("l p j s -> p l j s"), res[:], reads=["res"], writes=["out"])
    P.finish([ev])
    P.emit()
    return P.nc


def proj_fm(P, hT, hk, N, w_sb, wk, col0, ncols, ps_ap, pk, nk=NCH):
    for c in range(nk):
        P.op("tensor", lambda e, c=c: e.matmul(ps_ap, w_sb[:, c, col0:col0 + ncols], hT[:, c, 0:N], start=(c == 0), stop=(c == nk - 1)),
             reads=[wk, hk], writes=[pk])


TILES_STD = [(0, 512, 0), (512, 512, 0), (1024, 512, 0), (1536, 512, 0), (2048, 256, 1)]
NT_STD = 2304


def build_mla_a(tiles=TILES_STD, NT=NT_STD):
    P = Prog()
    C = emit_consts(P)
    xin = P.dram("xin", [D, NT], F32, kind="ExternalInput")
    modv_d = P.dram("modv", [128, 96, 2], F32, kind="ExternalInput")
    g_d = P.dram("normg", [128, NCH], F32, kind="ExternalInput")
    wa_d = P.dram("w_a", [D, 1152], F32, kind="ExternalInput")
    qg_d = P.dram("qkvg", [128, 8], F32, kind="ExternalInput")
    cos_d = P.dram("cosT", [64, NT], F32, kind="ExternalInput")
    sin_d = P.dram("sinT", [64, NT], F32, kind="ExternalInput")
    cn_d = P.dram("cnT", [1024, NT], F32, kind="ExternalOutput")
    kr_d = P.dram("krT", [64, NT], F32, kind="ExternalOutput")
    modv, g = load_layer_consts(P, modv_d, g_d)
    modA = make_modA(P, modv, g, 16, "a")
    wa = P.sb([128, NCH, 1152], BF16, "wa")
    P.dma("gpsimd", wa[:], wa_d.rearrange("(c p) n -> p c n", p=128), writes=["wa"])
    qg = P.sb([128, 8], F32, "qg")
    P.dma("sync", qg[:], qg_d, writes=["qg"])
    xb = [P.sb([128, NCH, 512], F32, f"x{i}") for i in range(2)]
    sq = P.sb([128, NCH, 512], BF16, "sq")
    rstd = P.sb([128, 512], F32, "rstd")
    hT = P.sb([128, NCH, 512], BF16, "hT")
    cT = P.sb([128, 8, 512], F32, "cT")
    cn = P.sb([128, 8, 512], F32, "cn")
    cs = P.sb([64, 2, 512], F32, "cs")
    kr = P.sb([64, 2, 512], F32, "kr")
    m_ps = P.ps([128, 512], F32, "mps")
    pp = [P.ps([128, 512], F32, f"pp{i}") for i in range(2)]
    xv = xin.rearrange("(c p) t -> p c t", p=128)
    n = 0
    nbox = [0]
    def tile_body(ti, t0, N, s):
        n = nbox[0]
        x = xb[ti % 2]; xk = f"x{ti%2}"
        P.dma("sync", x[:, :, 0:N], xv[:, :, t0:t0 + N], writes=[xk])
        P.dma("sync", cs[:, 0, 0:N], cos_d[:, t0:t0 + N], writes=["cs"])
        P.dma("sync", cs[:, 1, 0:N], sin_d[:, t0:t0 + N], writes=["cs"])
        normmod(P, C, x, xk, N, s, modA, "amodA", modv, 0, x, xk, sq, "sq", m_ps, "mps", rstd, "rstd", hT, "hT")
        if DEBUG and ti == 0:
            dh = P.dram("dbg_h", [128, NCH, 512], BF16, kind="ExternalOutput")
            dr = P.dram("dbg_r", [128, 512], F32, kind="ExternalOutput")
            P.dma("sync", dh, hT[:], reads=["hT"], writes=["dbgh"])
            P.dma("sync", dr, rstd[:], reads=["rstd"], writes=["dbgr"])
        for oc in range(8):
            p = pp[n % 2]; pk = f"pp{n%2}"; n += 1
            proj_fm(P, hT, "hT", N, wa, "wa", oc * 128, 128, p[:, 0:N], pk)
            P.op("scalar", lambda e, p=p, oc=oc: e.copy(out=cT[:, oc, 0:N], in_=p[:, 0:N]), reads=[pk], writes=["cT"])
        for v in range(2):
            p = pp[n % 2]; pk = f"pp{n%2}"; n += 1
            proj_fm(P, hT, "hT", N, wa, "wa", 1024 + v * 64, 64, p[0:64, 0:N], pk)
            P.op("vector", lambda e, p=p, v=v: e.tensor_tensor(out=kr[:, v, 0:N], in0=p[0:64, 0:N], in1=cs[:, v, 0:N], op=ALU.mult),
                 reads=[pk, "cs"], writes=["kr"])
        P.op("vector", lambda e: e.tensor_tensor(out=kr[:, 0, 0:N], in0=kr[:, 0, 0:N], in1=kr[:, 1, 0:N], op=ALU.add), reads=["kr"], writes=["kr"])
        P.dma("sync", kr_d[:, t0:t0 + N], kr[:, 0, 0:N], reads=["kr"], writes=["kr_out"])
        for half in range(2):
            rms_rstd(P, C, cT[:, half * 4:(half + 1) * 4, :], "cT", 4, N, 512, sq, "sq", m_ps, "mps", rstd, "rstd")
            P.op("vector", lambda e, half=half: e.tensor_tensor(out=cn[:, half * 4:(half + 1) * 4, 0:N], in0=cT[:, half * 4:(half + 1) * 4, 0:N],
                                                              in1=rstd[:, 0:N].unsqueeze(1).to_broadcast([128, 4, N]), op=ALU.mult),
                 reads=["cT", "rstd"], writes=["cn"])
            P.op("vector", lambda e, half=half: e.tensor_tensor(out=cn[:, half * 4:(half + 1) * 4, 0:N], in0=cn[:, half * 4:(half + 1) * 4, 0:N],
                                                              in1=qg[:, half * 4:(half + 1) * 4].unsqueeze(2).to_broadcast([128, 4, N]), op=ALU.mult),
                 reads=["cn", "qg"], writes=["cn"])
        P.dma("sync", cn_d.rearrange("(c p) t -> p c t", p=128)[:, :, t0:t0 + N], cn[:, :, 0:N], reads=["cn"], writes=["cn_out"])
        nbox[0] = n
    for ti, (t0, N, s) in enumerate(tiles):
        tile_body(ti, t0, N, s)
    P.finish([P.lastw["cn_out"], P.lastw["kr_out"]])
    P.emit()
    return P.nc


QB_STD = [(0, 512, 0), (512, 512, 0), (1024, 512, 0), (1536, 512, 0), (2048, 256, 1)]


def emit_outproj_residual(P, C, o_scr, nh, xin, xout, qblocks, modv, wo_d, pfx="op"):
    P.push_scope()
    wo = P.sb([128, nh, D], BF16, pfx + "wo")
    P.dma("gpsimd", wo[:], wo_d.rearrange("(h p) n -> p h n", p=128), writes=[pfx + "wo"])
    ot = [P.sb([128, nh, 512], BF16, f"{pfx}o{i}") for i in range(2)]
    xt = [P.sb([128, NCH, 512], F32, f"{pfx}x{i}") for i in range(2)]
    pp = [P.ps([128, 512], F32, f"{pfx}pp{i}") for i in range(2)]
    xv = xin.rearrange("(c p) t -> p c t", p=128)
    xov = xout.rearrange("(c p) t -> p c t", p=128)
    nb = [0]

    def body(qi, t0, N, s):
        o = ot[qi % 2]; ok = f"{pfx}o{qi%2}"; x = xt[qi % 2]; xk = f"{pfx}x{qi%2}"
        P.dma("sync", o[:, :, 0:N], o_scr.rearrange("h p t -> p h t")[:, :, t0:t0 + N], reads=["o_scr"], writes=[ok])
        P.dma("sync", x[:, :, 0:N], xv[:, :, t0:t0 + N], writes=[xk])
        for dc in range(NCH):
            p = pp[nb[0] % 2]; pk = f"{pfx}pp{nb[0]%2}"; nb[0] += 1
            for h in range(nh):
                P.op("tensor", lambda e, h=h, p=p, dc=dc: e.matmul(p[:, 0:N], wo[:, h, dc * 128:(dc + 1) * 128], o[:, h, 0:N], start=(h == 0), stop=(h == nh - 1)),
                     reads=[pfx + "wo", ok], writes=[pk])
            P.op("vector", lambda e, p=p, dc=dc: e.scalar_tensor_tensor(out=x[:, dc, 0:N], in0=p[:, 0:N], scalar=modv[:, 32 + dc:33 + dc, s], in1=x[:, dc, 0:N],
                                                                     op0=ALU.mult, op1=ALU.add), reads=[pk, "modv", xk], writes=[xk])
        P.dma("sync", xov[:, :, t0:t0 + N], x[:, :, 0:N], reads=[xk], writes=["xout"])
    for qi, (t0, N, s) in enumerate(qblocks):
        body(qi, t0, N, s)
    P.pop_scope()


def build_mla_b(qblocks=QB_STD, NT=NT_STD, NKL=8192, NKC=256):
    NK = NKL + NKC
    nkt = NK // 128
    P = Prog()
    C = emit_consts(P)
    xin = P.dram("xin", [D, NT], F32, kind="ExternalInput")
    xout = P.dram("xout", [D, NT], F32, kind="ExternalOutput")
    modv_d = P.dram("modv", [128, 96, 2], F32, kind="ExternalInput")
    cq_d = P.dram("cqT", [512, NT], F32, kind="ExternalInput")
    ckv_d = P.dram("ckvT", [512, NK], F32, kind="ExternalInput")
    kr_d = P.dram("krT", [64, NK], F32, kind="ExternalInput")
    cos_d = P.dram("cosT", [64, NT], F32, kind="ExternalInput")
    sin_d = P.dram("sinT", [64, NT], F32, kind="ExternalInput")
    wuq_d = P.dram("w_uq", [16, 512, 256], F32, kind="ExternalInput")
    wukv_d = P.dram("w_ukv", [16, 512, 256], F32, kind="ExternalInput")
    wo_d = P.dram("w_o", [D, D], F32, kind="ExternalInput")
    o_scr = P.dram("o_scr", [16, 128, NT], BF16)
    modv = P.sb([128, 96, 2], F32, "modv_sb")
    P.dma("sync", modv[:], modv_d, writes=["modv"])
    scale = 192.0 ** -0.5
    P.push_scope()
    ckv = P.sb([128, 4, NK], BF16, "ckv")
    cq = P.sb([128, 4, NT], BF16, "cq")
    kr = P.sb([64, NK], BF16, "kr")
    cs = P.sb([64, 2, NT], F32, "cs")
    P.dma("gpsimd", ckv[:], ckv_d.rearrange("(c p) t -> p c t", p=128), writes=["ckv"])
    P.dma("gpsimd", cq[:], cq_d.rearrange("(c p) t -> p c t", p=128), writes=["cq"])
    P.dma("gpsimd", kr[:], kr_d, writes=["kr"])
    P.dma("sync", cs[:, 0, :], cos_d, writes=["cs"])
    P.dma("sync", cs[:, 1, :], sin_d, writes=["cs"])
    KhT = P.sb([128, NK], BF16, "KhT")
    Vh = P.sb([128, nkt, 128], BF16, "Vh")
    QhT = P.sb([128, NT], BF16, "QhT")
    QrT = P.sb([64, NT], BF16, "QrT")
    qr1 = P.sb([64, 512], F32, "qr1")
    qr2 = P.sb([64, 512], F32, "qr2")
    wkv = [P.sb([128, 4, 256], BF16, f"wkv{i}") for i in range(2)]
    wq = [P.sb([128, 4, 256], BF16, f"wq{i}") for i in range(2)]
    PT = [P.sb([128, 512], BF16, f"PT{i}") for i in range(3)]
    oh = [P.sb([128, 512], BF16, f"oh{i}") for i in range(2)]
    rden = P.sb([128, 512], F32, "rden")
    s_ps = [P.ps([128, 512], F32, f"sps{i}") for i in range(3)]
    o_ps = P.ps([128, 512], F32, "ops")
    d_ps = P.ps([128, 512], F32, "dps")
    m_ps = [P.ps([128, 512], F32, f"mps{i}") for i in range(2)]
    cnt = {"m": 0, "s": 0, "o": 0}

    def head(h):
        wk_ = wkv[h % 2]; wkk = f"wkv{h%2}"; wq_ = wq[h % 2]; wqk = f"wq{h%2}"
        P.dma("gpsimd", wk_[:], wukv_d[h].rearrange("(c p) n -> p c n", p=128), writes=[wkk])
        P.dma("gpsimd", wq_[:], wuq_d[h].rearrange("(c p) n -> p c n", p=128), writes=[wqk])
        for kb in range((NK + 511) // 512):
            k0 = kb * 512; N = min(512, NK - k0)
            p = m_ps[cnt["m"] % 2]; pk = f"mps{cnt['m']%2}"; cnt["m"] += 1
            for c in range(4):
                P.op("tensor", lambda e, c=c, p=p, k0=k0, N=N: e.matmul(p[:, 0:N], wk_[:, c, 0:128], ckv[:, c, k0:k0 + N], start=(c == 0), stop=(c == 3)),
                     reads=[wkk, "ckv"], writes=[pk])
            P.op("scalar", lambda e, p=p, k0=k0, N=N: e.copy(out=KhT[:, k0:k0 + N], in_=p[:, 0:N]), reads=[pk], writes=["KhT"])
        for kg in range((nkt + 3) // 4):
            p = m_ps[cnt["m"] % 2]; pk = f"mps{cnt['m']%2}"; cnt["m"] += 1
            nn = min(4, nkt - kg * 4)
            for j in range(nn):
                kt = kg * 4 + j
                for c in range(4):
                    P.op("tensor", lambda e, c=c, p=p, kt=kt, j=j: e.matmul(p[:, j * 128:(j + 1) * 128], ckv[:, c, kt * 128:(kt + 1) * 128], wk_[:, c, 128:256],
                                                                         start=(c == 0), stop=(c == 3)), reads=[wkk, "ckv"], writes=[pk])
            P.op("vector", lambda e, p=p, kg=kg, nn=nn: e.tensor_copy(out=Vh[:, kg * 4:kg * 4 + nn, :], in_=p[:, 0:nn * 128].rearrange("p (j v) -> p j v", j=nn)),
                 reads=[pk], writes=["Vh"])
        for (t0, N, s) in qblocks:
            p = m_ps[cnt["m"] % 2]; pk = f"mps{cnt['m']%2}"; cnt["m"] += 1
            for c in range(4):
                P.op("tensor", lambda e, c=c, p=p, t0=t0, N=N: e.matmul(p[:, 0:N], wq_[:, c, 0:128], cq[:, c, t0:t0 + N], start=(c == 0), stop=(c == 3)),
                     reads=[wqk, "cq"], writes=[pk])
            P.op("scalar", lambda e, p=p, t0=t0, N=N: e.activation(out=QhT[:, t0:t0 + N], in_=p[:, 0:N], func=AF.Copy, scale=scale), reads=[pk], writes=["QhT"])
            for v, dst in ((0, qr1), (1, qr2)):
                p = m_ps[cnt["m"] % 2]; pk = f"mps{cnt['m']%2}"; cnt["m"] += 1
                for c in range(4):
                    P.op("tensor", lambda e, c=c, p=p, t0=t0, N=N, v=v: e.matmul(p[0:64, 0:N], wq_[:, c, 128 + 64 * v:192 + 64 * v], cq[:, c, t0:t0 + N], start=(c == 0), stop=(c == 3)),
                         reads=[wqk, "cq"], writes=[pk])
                P.op("vector", lambda e, p=p, t0=t0, N=N, v=v, dst=dst: e.tensor_tensor(out=dst[:, 0:N], in0=p[0:64, 0:N], in1=cs[:, v, t0:t0 + N], op=ALU.mult),
                     reads=[pk, "cs"], writes=["qr%d" % v])
            P.op("vector", lambda e, t0=t0, N=N: e.tensor_tensor(out=qr1[:, 0:N], in0=qr1[:, 0:N], in1=qr2[:, 0:N], op=ALU.add), reads=["qr0", "qr1"], writes=["qr0"])
            P.op("scalar", lambda e, t0=t0, N=N: e.activation(out=QrT[:, t0:t0 + N], in_=qr1[:, 0:N], func=AF.Copy, scale=scale), reads=["qr0"], writes=["QrT"])
        for (t0, N, s) in qblocks:
            kts = list(range(nkt)) if s == 0 else list(range(NKL // 128, nkt))
            attend_block(t0, N, kts)

    def attend_block(t0, N, kts):
        base = cnt["s"]; n = len(kts)
        cnt["s"] += n

        def bufs(ii):
            j = (base + ii) % 3
            return s_ps[j], f"sps{j}", PT[j], f"PT{j}"

        def score(ii):
            sp, sk, pt, ptk = bufs(ii); kt = kts[ii]
            P.op("tensor", lambda e: e.matmul(sp[:, 0:N], KhT[:, kt * 128:(kt + 1) * 128], QhT[:, t0:t0 + N], start=True, stop=False),
                 reads=["KhT", "QhT"], writes=[sk])
            P.op("tensor", lambda e: e.matmul(sp[:, 0:N], kr[:, kt * 128:(kt + 1) * 128], QrT[:, t0:t0 + N], start=False, stop=True),
                 reads=["kr", "QrT"], writes=[sk])

        def rest(ii):
            sp, sk, pt, ptk = bufs(ii); kt = kts[ii]; last = (ii == n - 1)
            P.op("scalar", lambda e: e.activation(out=pt[:, 0:N], in_=sp[:, 0:N], func=AF.Exp), reads=[sk], writes=[ptk])
            P.op("tensor", lambda e: e.matmul(o_ps[:, 0:N], Vh[:, kt, :], pt[:, 0:N], start=(ii == 0), stop=last), reads=["Vh", ptk], writes=["ops"])
            P.op("tensor", lambda e: e.matmul(d_ps[:, 0:N], C["ones_b"][:], pt[:, 0:N], start=(ii == 0), stop=last), reads=["ones_b", ptk], writes=["dps"])
        for ii in range(min(2, n)):
            score(ii)
        for ii in range(n):
            if ii + 2 < n:
                score(ii + 2)
            rest(ii)
        o_ = oh[cnt["o"] % 2]; ok = f"oh{cnt['o']%2}"; cnt["o"] += 1
        P.op("vector", lambda e: e.reciprocal(out=rden[:, 0:N], in_=d_ps[:, 0:N]), reads=["dps"], writes=["rden"])
        P.op("vector", lambda e: e.tensor_tensor(out=o_[:, 0:N], in0=o_ps[:, 0:N], in1=rden[:, 0:N], op=ALU.mult), reads=["ops", "rden"], writes=[ok])
        P.dma("sync", o_scr[cur_h[0], :, t0:t0 + N], o_[:, 0:N], reads=[ok], writes=["o_scr"])
    cur_h = [0]
    for h in range(16):
        cur_h[0] = h
        head(h)
    P.pop_scope()
    emit_outproj_residual(P, C, o_scr, 16, xin, xout, qblocks, modv, wo_d)
    P.finish([P.lastw["xout"]])
    print("mla_b ops", {e: len(v_) for e, v_ in P.ops.items()})
    P.emit()
    return P.nc


def emit_front_h(P, C, xin, tiles, NT, modv, g, sc_off, sh_off, pfx="f"):
    modA = make_modA(P, modv, g, sc_off, pfx)
    hT = P.sb([128, NCH, NT], BF16, pfx + "hT")
    P.push_scope()
    xb = [P.sb([128, NCH, 512], F32, f"{pfx}x{i}") for i in range(2)]
    sq = P.sb([128, NCH, 512], BF16, pfx + "sq")
    rstd = P.sb([128, 512], F32, pfx + "rstd")
    m_ps = P.ps([128, 512], F32, pfx + "mps")
    xv = xin.rearrange("(c p) t -> p c t", p=128)

    def body(ti, t0, N, s):
        x = xb[ti % 2]; xk = f"{pfx}x{ti%2}"
        P.dma("sync", x[:, :, 0:N], xv[:, :, t0:t0 + N], writes=[xk])
        normmod(P, C, x, xk, N, s, modA, pfx + "modA", modv, sh_off, x, xk, sq, pfx + "sq", m_ps, pfx + "mps", rstd, pfx + "rstd",
                hT[:, :, t0:t0 + N], pfx + "hT")
    for ti, (t0, N, s) in enumerate(tiles):
        body(ti, t0, N, s)
    P.pop_scope()
    return hT


def build_win_a(tiles=TILES_STD, NT=NT_STD):
    P = Prog()
    C = emit_consts(P)
    xin = P.dram("xin", [D, NT], F32, kind="ExternalInput")
    modv_d = P.dram("modv", [128, 96, 2], F32, kind="ExternalInput")
    g_d = P.dram("normg", [128, NCH], F32, kind="ExternalInput")
    wqk_d = P.dram("w_qk", [D, 20, 256], F32, kind="ExternalInput")
    wv_d = P.dram("w_v", [D, 512], F32, kind="ExternalInput")
    cos_d = P.dram("cos2", [128, NT], F32, kind="ExternalInput")
    sin_d = P.dram("sin2", [128, NT], F32, kind="ExternalInput")
    qk_d = P.dram("QKT", [20 * 128, NT], F32, kind="ExternalOutput")
    v_d = P.dram("V", [NT, 512], F32, kind="ExternalOutput")
    modv, g = load_layer_consts(P, modv_d, g_d)
    hT = emit_front_h(P, C, xin, tiles, NT, modv, g, 16, 0)
    cs = P.sb([128, 2, NT], F32, "cs")
    P.dma("sync", cs[:, 0, :], cos_d, writes=["cs"])
    P.dma("sync", cs[:, 1, :], sin_d, writes=["cs"])
    w = [P.sb([128, NCH, 256], BF16, f"w{i}") for i in range(2)]
    wv = P.sb([128, NCH, 512], BF16, "wv")
    P.dma("gpsimd", wv[:], wv_d.rearrange("(c p) n -> p c n", p=128), writes=["wv"])
    r1 = [P.sb([128, 512], F32, f"r1_{i}") for i in range(2)]
    r2 = P.sb([128, 512], F32, "r2")
    vt = [P.sb([128, 512], F32, f"vt{i}") for i in range(2)]
    pp = [P.ps([128, 512], F32, f"pp{i}") for i in range(4)]
    cnt = [0, 0]
    scale = 64.0 ** -0.5

    def chunk(oc):
        wb = w[oc % 2]; wk = f"w{oc%2}"
        P.dma("gpsimd", wb[:], wqk_d.rearrange("(c p) o n -> p c o n", p=128)[:, :, oc, :], writes=[wk])
        for (t0, N, s) in tiles:
            pa = pp[cnt[0] % 4]; pak = f"pp{cnt[0]%4}"; cnt[0] += 1
            pb = pp[cnt[0] % 4]; pbk = f"pp{cnt[0]%4}"; cnt[0] += 1
            proj_fm(P, hT[:, :, t0:t0 + N], "fhT", N, wb, wk, 0, 128, pa[:, 0:N], pak)
            proj_fm(P, hT[:, :, t0:t0 + N], "fhT", N, wb, wk, 128, 128, pb[:, 0:N], pbk)
            ra = r1[cnt[1] % 2]; rak = f"r1_{cnt[1]%2}"; cnt[1] += 1
            P.op("vector", lambda e, pa=pa, ra=ra, t0=t0, N=N: e.tensor_tensor(out=ra[:, 0:N], in0=pa[:, 0:N], in1=cs[:, 0, t0:t0 + N], op=ALU.mult), reads=[pak, "cs"], writes=[rak])
            P.op("vector", lambda e, pb=pb, t0=t0, N=N: e.tensor_tensor(out=r2[:, 0:N], in0=pb[:, 0:N], in1=cs[:, 1, t0:t0 + N], op=ALU.mult), reads=[pbk, "cs"], writes=["r2"])
            P.op("vector", lambda e, ra=ra, N=N: e.scalar_tensor_tensor(out=ra[:, 0:N], in0=ra[:, 0:N], scalar=(scale if oc < 16 else 1.0), in1=r2[:, 0:N], op0=ALU.mult, op1=ALU.add)
                 if False else e.tensor_tensor(out=ra[:, 0:N], in0=ra[:, 0:N], in1=r2[:, 0:N], op=ALU.add), reads=[rak, "r2"], writes=[rak])
            if oc < 16:
                P.op("scalar", lambda e, ra=ra, N=N: e.mul(out=ra[:, 0:N], in_=ra[:, 0:N], mul=scale), reads=[rak], writes=[rak])
            P.dma("sync", qk_d[oc * 128:(oc + 1) * 128, t0:t0 + N], ra[:, 0:N], reads=[rak], writes=["qk_out"])
    for oc in range(20):
        chunk(oc)

    def vtile(i):
        p = pp[cnt[0] % 4]; pk = f"pp{cnt[0]%4}"; cnt[0] += 1
        for c in range(NCH):
            P.op("tensor", lambda e, c=c, p=p: e.matmul(p[:], hT[:, c, i * 128:(i + 1) * 128], wv[:, c, :], start=(c == 0), stop=(c == NCH - 1)),
                 reads=["fhT", "wv"], writes=[pk])
        v_ = vt[i % 2]; vk = f"vt{i%2}"
        P.op("scalar", lambda e, p=p, v_=v_: e.copy(out=v_[:], in_=p[:]), reads=[pk], writes=[vk])
        P.dma("sync", v_d[i * 128:(i + 1) * 128, :], v_[:], reads=[vk], writes=["v_out"])
    for i in range(NT // 128):
        vtile(i)
    P.finish([P.lastw["qk_out"], P.lastw["v_out"]])
    P.emit()
    return P.nc


def build_win_b(qblocks=QB_STD, NT=NT_STD):
    nlat = sum(N for (_, N, s) in qblocks if s == 0)
    nown = nlat // 128
    nke = nown + 4
    NKE = nke * 128
    nqb = len(qblocks)
    P = Prog()
    C = emit_consts(P)
    xin = P.dram("xin", [D, NT], F32, kind="ExternalInput")
    xout = P.dram("xout", [D, NT], F32, kind="ExternalOutput")
    modv_d = P.dram("modv", [128, 96, 2], F32, kind="ExternalInput")
    q_d = P.dram("QT", [2048, NT], F32, kind="ExternalInput")
    k_d = P.dram("KT", [512, NKE], F32, kind="ExternalInput")
    v_d = P.dram("V", [NKE, 512], F32, kind="ExternalInput")
    mask_d = P.dram("masks", [nqb, 6, 128, 512], F32, kind="ExternalInput")
    sink_d = P.dram("sinks", [64, 32], F32, kind="ExternalInput")
    wo_d = P.dram("w_o", [D, D], F32, kind="ExternalInput")
    o_scr = P.dram("o_scr", [16, 128, NT], BF16)
    modv = P.sb([128, 96, 2], F32, "modv_sb")
    P.dma("sync", modv[:], modv_d, writes=["modv"])
    P.push_scope()
    masks = P.sb([128, nqb, 6, 512], BF16, "masks")
    P.dma("gpsimd", masks[:], mask_d.rearrange("b j p q -> p b j q"), writes=["masks"])
    sk = P.sb([64, 32], F32, "sk")
    P.dma("sync", sk[:], sink_d, writes=["sk"])
    P.op("scalar", lambda e: e.activation(out=sk[:], in_=sk[:], func=AF.Exp), reads=["sk"], writes=["sk"])
    Kk = [P.sb([64, NKE], BF16, f"Kk{i}") for i in range(2)]
    Vv = [P.sb([128, nke, 64], BF16, f"Vv{i}") for i in range(2)]
    Qh = [P.sb([64, NT], BF16, f"Qh{i}") for i in range(2)]
    PT = [P.sb([128, 512], BF16, f"PT{i}") for i in range(3)]
    oh = [P.sb([64, 512], BF16, f"oh{i}") for i in range(2)]
    rden = P.sb([64, 512], F32, "rden")
    s_ps = [P.ps([128, 512], F32, f"sps{i}") for i in range(3)]
    o_ps = P.ps([128, 512], F32, "ops")
    d_ps = P.ps([128, 512], F32, "dps")
    cnt = {"s": 0, "o": 0}

    def head(h):
        kvh = h // 4
        kk = Kk[kvh % 2]; kkk = f"Kk{kvh%2}"; vv = Vv[kvh % 2]; vvk = f"Vv{kvh%2}"
        if h % 4 == 0:
            P.dma("gpsimd", kk[:], k_d[kvh * 64:(kvh + 1) * 64, :], writes=[kkk])
            P.dma("gpsimd", vv[:], v_d.rearrange("(t p) d -> p t d", p=128)[:, :, kvh * 64:(kvh + 1) * 64], writes=[vvk])
        qh = Qh[h % 2]; qk = f"Qh{h%2}"
        P.dma("gpsimd", qh[:], q_d[h * 64:(h + 1) * 64, :], writes=[qk])
        for bi, (t0, N, s) in enumerate(qblocks):
            if s == 0:
                b = t0 // 512
                slots = [(4 * b + j, j) for j in range(6)] + [(nown + 2, None), (nown + 3, None)]
            else:
                slots = [(nown + 2, None), (nown + 3, None)]
            base = cnt["s"]; n = len(slots)
            cnt["s"] += n

            def wbufs(ii, base=base):
                j = (base + ii) % 3
                return s_ps[j], f"sps{j}", PT[j], f"PT{j}"

            def score(ii, slots=slots, t0=t0, N=N, wbufs=wbufs):
                sp, spk, pt, ptk = wbufs(ii); e_ = slots[ii][0]
                P.op("tensor", lambda e: e.matmul(sp[:, 0:N], kk[:, e_ * 128:(e_ + 1) * 128], qh[:, t0:t0 + N], start=True, stop=True),
                     reads=[kkk, qk], writes=[spk])

            def rest(ii, slots=slots, t0=t0, N=N, wbufs=wbufs, bi=bi, n=n):
                sp, spk, pt, ptk = wbufs(ii); e_, mj = slots[ii]; last = (ii == n - 1)
                P.op("scalar", lambda e: e.activation(out=pt[:, 0:N], in_=sp[:, 0:N], func=AF.Exp), reads=[spk], writes=[ptk])
                if mj is not None:
                    P.op("vector", lambda e: e.tensor_tensor(out=pt[:, 0:N], in0=pt[:, 0:N], in1=masks[:, bi, mj, 0:N], op=ALU.mult),
                         reads=[ptk, "masks"], writes=[ptk])
                P.op("tensor", lambda e: e.matmul(o_ps[0:64, 0:N], vv[:, e_, :], pt[:, 0:N], start=(ii == 0), stop=last), reads=[vvk, ptk], writes=["ops"])
                P.op("tensor", lambda e: e.matmul(d_ps[0:64, 0:N], C["ones_b"][:, 0:64], pt[:, 0:N], start=(ii == 0), stop=last), reads=["ones_b", ptk], writes=["dps"])
            for ii in range(min(2, n)):
                score(ii)
            for ii in range(n):
                if ii + 2 < n:
                    score(ii + 2)
                rest(ii)
            o_ = oh[cnt["o"] % 2]; ok = f"oh{cnt['o']%2}"; cnt["o"] += 1
            P.op("vector", lambda e, N=N: e.tensor_scalar(out=rden[:, 0:N], in0=d_ps[0:64, 0:N], scalar1=sk[:, h:h + 1], scalar2=None, op0=ALU.add), reads=["dps", "sk"], writes=["rden"])
            P.op("vector", lambda e, N=N: e.reciprocal(out=rden[:, 0:N], in_=rden[:, 0:N]), reads=["rden"], writes=["rden"])
            P.op("vector", lambda e, N=N, o_=o_: e.tensor_tensor(out=o_[:, 0:N], in0=o_ps[0:64, 0:N], in1=rden[:, 0:N], op=ALU.mult), reads=["ops", "rden"], writes=[ok])
            P.dma("sync", o_scr[h // 2, (h % 2) * 64:(h % 2) * 64 + 64, t0:t0 + N], o_[:, 0:N], reads=[ok], writes=["o_scr"])
    for h in range(32):
        head(h)
    P.pop_scope()
    emit_outproj_residual(P, C, o_scr, 16, xin, xout, qblocks, modv, wo_d)
    P.finish([P.lastw["xout"]])
    print("win_b ops", {e: len(v_) for e, v_ in P.ops.items()})
    P.emit()
    return P.nc


def build_final(tiles=TILES_STD[:4], NT=2048):
    P = Prog()
    C = emit_consts(P)
    xin = P.dram("xin", [D, NT], F32, kind="ExternalInput")
    g_d = P.dram("normg", [128, NCH], F32, kind="ExternalInput")
    xout = P.dram("xout", [D, NT], F32, kind="ExternalOutput")
    g = P.sb([128, NCH], F32, "g_sb")
    P.dma("sync", g[:], g_d, writes=["normg"])
    xb = [P.sb([128, NCH, 512], F32, f"x{i}") for i in range(2)]
    sq = P.sb([128, NCH, 512], BF16, "sq")
    rstd = P.sb([128, 512], F32, "rstd")
    m_ps = P.ps([128, 512], F32, "mps")
    xv = xin.rearrange("(c p) t -> p c t", p=128)
    xov = xout.rearrange("(c p) t -> p c t", p=128)

    def body(ti, t0, N):
        x = xb[ti % 2]; xk = f"x{ti%2}"
        P.dma("sync", x[:, :, 0:N], xv[:, :, t0:t0 + N], writes=[xk])
        rms_rstd(P, C, x, xk, NCH, N, D, sq, "sq", m_ps, "mps", rstd, "rstd")
        P.op("vector", lambda e: e.tensor_tensor(out=x[:, :, 0:N], in0=x[:, :, 0:N], in1=rstd[:, 0:N].unsqueeze(1).to_broadcast([128, NCH, N]), op=ALU.mult),
             reads=[xk, "rstd"], writes=[xk])
        P.op("vector", lambda e: e.tensor_tensor(out=x[:, :, 0:N], in0=x[:, :, 0:N], in1=g[:, :].unsqueeze(2).to_broadcast([128, NCH, N]), op=ALU.mult),
             reads=[xk, "normg"], writes=[xk])
        P.dma("sync", xov[:, :, t0:t0 + N], x[:, :, 0:N], reads=[xk], writes=["xout"])
    for ti, (t0, N, s) in enumerate(tiles):
        body(ti, t0, N)
    P.finish([P.lastw["xout"]])
    P.emit()
    return P.nc


def build_peer(kinds, NT):
    P = Prog()
    C = emit_consts(P)
    xin = P.dram("xin", [D, NT], F32, kind="ExternalInput")
    xout = P.dram("xout", [D, NT], F32, kind="ExternalOutput")
    modv_d = P.dram("modv", [128, 96, 2], F32, kind="ExternalInput")
    g_d = P.dram("normg", [128, NCH], F32, kind="ExternalInput")
    wq_d = P.dram("wq", [D, 2048], F32, kind="ExternalInput")
    k1T_d = P.dram("k1T", [8, 128, 128], F32, kind="ExternalInput")
    k2T_d = P.dram("k2T", [8, 128, 128], F32, kind="ExternalInput")
    uT_d = P.dram("uT", [D, 16384], F32, kind="ExternalInput")
    v_d = P.dram("v", [16384, D], F32, kind="ExternalInput")
    modv, g = load_layer_consts(P, modv_d, g_d)
    k = emit_peer(P, C, xin, xout, kinds, modv, g, wq_d, k1T_d, k2T_d, uT_d, v_d)
    P.finish([P.lastw[k]])
    P.emit()
    return P.nc


def build_ssd_s(n_ctx_chunks=2, n_lat_chunks=64):
    NCHK = n_ctx_chunks + n_lat_chunks
    NTS = NCHK * 128
    fwd_order = list(range(NCHK))
    bwd_order = list(range(n_ctx_chunks - 1, -1, -1)) + list(range(NCHK - 1, n_ctx_chunks - 1, -1))
    P = Prog()
    C = emit_consts(P)
    x_d = P.dram("x_tm", [NCHK, 128, 1024], F32, kind="ExternalInput")
    b_d = P.dram("B_tm", [NCHK, 128, 256], F32, kind="ExternalInput")
    bt_d = P.dram("BT", [2, 128, NTS], F32, kind="ExternalInput")
    ct_d = P.dram("CT", [2, 128, NTS], F32, kind="ExternalInput")
    dt_d = P.dram("dt_tm", [NCHK, 128, 32], F32, kind="ExternalInput")
    al_d = P.dram("alog", [128, 32], F32, kind="ExternalInput")
    dr_d = P.dram("Drep", [128, 16], F32, kind="ExternalInput")
    tri_d = P.dram("tri", [2, 128, 128], F32, kind="ExternalInput")
    y_d = P.dram("y_tm", [NCHK, 128, 1024], F32, kind="ExternalOutput")

    A = P.sb([128, 32], F32, "A")
    Dr = P.sb([128, 16], F32, "Dr")
    tri = P.sb([128, 2, 128], F32, "tri")
    ones_f = P.sb([128, 128], F32, "ones_f")
    P.dma("sync", A[:], al_d, writes=["A"])
    P.dma("sync", Dr[:], dr_d, writes=["Dr"])
    P.dma("sync", tri[:], tri_d.rearrange("d j i -> j d i"), writes=["tri"])
    P.op("scalar", lambda e: e.activation(out=A[:], in_=A[:], func=AF.Exp), reads=["A"], writes=["A"])
    P.op("vector", lambda e: e.tensor_scalar(out=A[:], in0=A[:], scalar1=-1.0, scalar2=None, op0=ALU.mult), reads=["A"], writes=["A"])
    P.op("vector", lambda e: e.memset(ones_f[:], 1.0), writes=["ones_f"])

    x32 = [P.sb([128, 1024], F32, f"x32_{i}") for i in range(2)]
    xcb = [P.sb([128, 1024], BF16, f"xcb{i}") for i in range(2)]
    Bc = [P.sb([128, 256], BF16, f"Bc{i}") for i in range(2)]
    BTc = [P.sb([128, 2, 128], BF16, f"BTc{i}") for i in range(2)]
    CTc = [P.sb([128, 2, 128], BF16, f"CTc{i}") for i in range(2)]
    CTf = [P.sb([128, 2, 128], F32, f"CTf{i}") for i in range(2)]
    dtc = [P.sb([128, 32], F32, f"dtc{i}") for i in range(2)]
    dA = P.sb([128, 16], F32, "dA")
    dAb = P.sb([128, 16, 128], F32, "dAb")
    acs = P.sb([128, 16], F32, "acs")
    wst = P.sb([128, 16], F32, "wst")
    etot = P.sb([128, 16], F32, "etot")
    cbm = P.sb([128, 2, 128], F32, "cbm")
    dec = [P.sb([128, 128], F32, f"dec{i}") for i in range(2)]
    Mh = [P.sb([128, 128], BF16, f"Mh{i}") for i in range(2)]
    erow = [P.sb([128, 128], F32, f"erow{i}") for i in range(2)]
    CTe = [P.sb([128, 128], BF16, f"CTe{i}") for i in range(2)]
    S = P.sb([128, 2, 512], F32, "S")
    Sb = P.sb([128, 2, 512], BF16, "Sb")
    xw = P.sb([128, 2, 512], BF16, "xw")
    xD = P.sb([128, 1024], F32, "xD")
    ysb = [P.sb([128, 1024], F32, f"ysb{i}") for i in range(2)]
    yprev = [P.sb([128, 1024], F32, f"yprev{i}") for i in range(2)]
    row_ps = [P.ps([128, 512], F32, f"rowps{i}") for i in range(2)]
    y_ps = [P.ps([128, 512], F32, f"yps{i}") for i in range(2)]
    cb_ps = P.ps([128, 2, 128], F32, "cbps")
    sn_ps = P.ps([128, 512], F32, "snps")
    sm_ps = P.ps([128, 2, 16], F32, "smps")
    cnt = {"c": 0, "h": 0}

    def chunk(d, c):
        i2 = cnt["c"] % 2; cnt["c"] += 1
        x3 = x32[i2]; x3k = f"x32_{i2}"; xb = xcb[i2]; xbk = f"xcb{i2}"
        bc = Bc[i2]; bck = f"Bc{i2}"; btc = BTc[i2]; btk = f"BTc{i2}"; ctc = CTc[i2]; ctk = f"CTc{i2}"; ctf = CTf[i2]; ctfk = f"CTf{i2}"
        dc = dtc[i2]; dck = f"dtc{i2}"
        P.dma("sync", x3[:], x_d[c], writes=[x3k])
        P.dma("gpsimd", bc[:], b_d[c], writes=[bck])
        P.dma("gpsimd", btc[:], bt_d[:, :, c * 128:(c + 1) * 128].rearrange("g n t -> n g t"), writes=[btk])
        P.dma("sync", ctf[:], ct_d[:, :, c * 128:(c + 1) * 128].rearrange("g n t -> n g t"), writes=[ctfk])
        P.dma("sync", dc[:], dt_d[c], writes=[dck])
        P.op("scalar", lambda e: e.copy(out=xb[:], in_=x3[:]), reads=[x3k], writes=[xbk])
        P.op("scalar", lambda e: e.copy(out=ctc[:], in_=ctf[:]), reads=[ctfk], writes=[ctk])
        dts = dc[:, d * 16:(d + 1) * 16]
        P.op("vector", lambda e: e.tensor_tensor(out=dA[:], in0=dts, in1=A[:, d * 16:(d + 1) * 16], op=ALU.mult), reads=[dck, "A"], writes=["dA"])
        P.op("vector", lambda e: e.tensor_copy(out=dAb[:], in_=dA[:].unsqueeze(2).to_broadcast([128, 16, 128])), reads=["dA"], writes=["dAb"])
        P.op("tensor", lambda e: e.matmul(sm_ps[:, 0, :], tri[:, d, :], dA[:], start=True, stop=True), reads=["tri", "dA"], writes=["smps"])
        P.op("tensor", lambda e: e.matmul(sm_ps[:, 1, :], ones_f[:], dA[:], start=True, stop=True), reads=["ones_f", "dA"], writes=["smps"])
        P.op("vector", lambda e: e.tensor_copy(out=acs[:], in_=sm_ps[:, 0, :]), reads=["smps"], writes=["acs"])
        P.op("vector", lambda e: e.tensor_tensor(out=wst[:], in0=sm_ps[:, 1, :], in1=acs[:], op=ALU.subtract), reads=["smps", "acs"], writes=["wst"])
        P.op("scalar", lambda e: e.activation(out=wst[:], in_=wst[:], func=AF.Exp), reads=["wst"], writes=["wst"])
        P.op("vector", lambda e: e.tensor_tensor(out=wst[:], in0=wst[:], in1=dts, op=ALU.mult), reads=["wst", dck], writes=["wst"])
        P.op("scalar", lambda e: e.activation(out=etot[:], in_=sm_ps[:, 1, :], func=AF.Exp), reads=["smps"], writes=["etot"])
        for g in range(2):
            P.op("tensor", lambda e, g=g: e.matmul(cb_ps[:, g, :], btc[:, g, :], ctc[:, g, :], start=True, stop=True), reads=[btk, ctk], writes=["cbps"])
        P.op("vector", lambda e: e.tensor_tensor(out=cbm[:], in0=cb_ps[:], in1=tri[:, d, :].unsqueeze(1).to_broadcast([128, 2, 128]), op=ALU.mult),
             reads=["cbps", "tri"], writes=["cbm"])
        hbase = cnt["h"]; cnt["h"] += 16

        def hb(h):
            j2 = (hbase + h) % 2
            return row_ps[j2], f"rowps{j2}", dec[j2], f"dec{j2}", Mh[j2], f"Mh{j2}", erow[j2], f"erow{j2}", CTe[j2], f"CTe{j2}"

        def rowmm(h):
            rp, rpk = hb(h)[:2]
            P.op("tensor", lambda e: e.matmul(rp[:, 0:128], dAb[:, h, :], tri[:, d, :], start=True, stop=True), reads=["dAb", "tri"], writes=[rpk])

        def hrest(h):
            g = h // 8
            rp, rpk, de, dek, mh, mhk, er, erk, ce, cek = hb(h)
            P.op("vector", lambda e: e.tensor_scalar(out=de[:], in0=rp[:, 0:128], scalar1=acs[:, h:h + 1], scalar2=0.0, op0=ALU.subtract, op1=ALU.min),
                 reads=[rpk, "acs"], writes=[dek])
            P.op("scalar", lambda e: e.activation(out=de[:], in_=de[:], func=AF.Exp), reads=[dek], writes=[dek])
            P.op("vector", lambda e: e.scalar_tensor_tensor(out=mh[:], in0=de[:], scalar=dts[:, h:h + 1], in1=cbm[:, g, :], op0=ALU.mult, op1=ALU.mult),
                 reads=[dek, dck, "cbm"], writes=[mhk])
            P.op("scalar", lambda e: e.activation(out=er[:], in_=rp[:, 0:128], func=AF.Exp), reads=[rpk], writes=[erk])
            P.op("vector", lambda e: e.tensor_tensor(out=ce[:], in0=er[:], in1=ctf[:, g, :], op=ALU.mult), reads=[erk, ctfk], writes=[cek])
            hs = slice((h % 8) * 64, (h % 8) * 64 + 64)
            P.op("tensor", lambda e: e.matmul(y_ps[g][:, hs], mh[:], xb[:, h * 64:(h + 1) * 64], start=True, stop=False),
                 reads=[mhk, xbk], writes=[f"yps{g}"])
            P.op("tensor", lambda e: e.matmul(y_ps[g][:, hs], ce[:], Sb[:, g, hs], start=False, stop=True),
                 reads=[cek, "Sb"], writes=[f"yps{g}"])
        rowmm(0)
        for h in range(16):
            if h + 1 < 16:
                rowmm(h + 1)
            hrest(h)
        ys = ysb[i2]; ysk = f"ysb{i2}"
        if d == 0:
            P.op("vector", lambda e: e.tensor_tensor(out=xD[:].rearrange("p (h q) -> p h q", h=16), in0=x3[:].rearrange("p (h q) -> p h q", h=16),
                                                   in1=Dr[:].unsqueeze(2).to_broadcast([128, 16, 64]), op=ALU.mult), reads=[x3k, "Dr"], writes=["xD"])
            for g in range(2):
                P.op("vector", lambda e, g=g: e.tensor_tensor(out=ys[:, g * 512:(g + 1) * 512], in0=y_ps[g][:], in1=xD[:, g * 512:(g + 1) * 512], op=ALU.add),
                     reads=[f"yps{g}", "xD"], writes=[ysk])
        else:
            yp = yprev[i2]; ypk = f"yprev{i2}"
            P.dma("sync", yp[:], y_d[c], reads=[f"y{c}"], writes=[ypk])
            for g in range(2):
                P.op("vector", lambda e, g=g: e.tensor_tensor(out=ys[:, g * 512:(g + 1) * 512], in0=y_ps[g][:], in1=yp[:, g * 512:(g + 1) * 512], op=ALU.add),
                     reads=[f"yps{g}", ypk], writes=[ysk])
        P.dma("sync", y_d[c], ys[:], reads=[ysk], writes=[f"y{c}"])
        for g in range(2):
            P.op("vector", lambda e, g=g: e.tensor_tensor(out=xw[:, g, :].rearrange("p (h q) -> p h q", h=8),
                                                        in0=xb[:, g * 512:(g + 1) * 512].rearrange("p (h q) -> p h q", h=8),
                                                        in1=wst[:, g * 8:(g + 1) * 8].unsqueeze(2).to_broadcast([128, 8, 64]), op=ALU.mult),
                 reads=[xbk, "wst"], writes=["xw"])
            P.op("tensor", lambda e, g=g: e.matmul(sn_ps[:], bc[:, g * 128:(g + 1) * 128], xw[:, g, :], start=True, stop=True), reads=[bck, "xw"], writes=["snps"])
            P.op("vector", lambda e, g=g: e.tensor_tensor(out=S[:, g, :].rearrange("p (h q) -> p h q", h=8), in0=S[:, g, :].rearrange("p (h q) -> p h q", h=8),
                                                        in1=etot[:, g * 8:(g + 1) * 8].unsqueeze(2).to_broadcast([128, 8, 64]), op=ALU.mult),
                 reads=["S", "etot"], writes=["S"])
            P.op("vector", lambda e, g=g: e.tensor_tensor(out=S[:, g, :], in0=S[:, g, :], in1=sn_ps[:], op=ALU.add), reads=["S", "snps"], writes=["S"])
            P.op("scalar", lambda e, g=g: e.copy(out=Sb[:, g, :], in_=S[:, g, :]), reads=["S"], writes=["Sb"])

    for d, order in ((0, fwd_order), (1, bwd_order)):
        P.op("vector", lambda e: e.memset(S[:], 0.0), writes=["S"])
        P.op("vector", lambda e: e.memset(Sb[:], 0.0), writes=["Sb"])
        for c in order:
            chunk(d, c)
    P.finish([P.lastw[f"y{c}"] for c in range(NCHK)])
    print("ssd_s ops", {e: len(v_) for e, v_ in P.ops.items()})
    P.emit()
    return P.nc


def build_ssd_a1(tiles=TILES_STD, NT=NT_STD):
    NOUT = 10368
    P = Prog()
    C = emit_consts(P)
    xin = P.dram("xin", [D, NT], F32, kind="ExternalInput")
    modv_d = P.dram("modv", [128, 96, 2], F32, kind="ExternalInput")
    g_d = P.dram("normg", [128, NCH], F32, kind="ExternalInput")
    w_d = P.dram("w_in", [D, NOUT], F32, kind="ExternalInput")
    z_d = P.dram("ZT", [NOUT, NT], F32, kind="ExternalOutput")
    modv, g = load_layer_consts(P, modv_d, g_d)
    hT = emit_front_h(P, C, xin, tiles, NT, modv, g, 16, 0)
    w = [P.sb([128, NCH, 512], BF16, f"w{i}") for i in range(2)]
    st = [P.sb([128, 4, 512], F32, f"st{i}") for i in range(2)]
    pp = [P.ps([128, 512], F32, f"pp{i}") for i in range(4)]
    cnt = [0, 0]
    wv = w_d.rearrange("(c p) n -> p c n", p=128)
    zv = z_d.rearrange("(o p) t -> p o t", p=128)

    def block(ob):
        c0 = ob * 512; ncols = min(512, NOUT - c0); nch = ncols // 128
        wb = w[ob % 2]; wk = f"w{ob%2}"
        P.dma("gpsimd", wb[:, :, 0:ncols], wv[:, :, c0:c0 + ncols], writes=[wk])
        for (t0, N, s) in tiles:
            sb_ = st[cnt[1] % 2]; sk = f"st{cnt[1]%2}"; cnt[1] += 1
            for j in range(nch):
                p = pp[cnt[0] % 4]; pk = f"pp{cnt[0]%4}"; cnt[0] += 1
                proj_fm(P, hT[:, :, t0:t0 + N], "fhT", N, wb, wk, j * 128, 128, p[:, 0:N], pk)
                P.op("scalar", lambda e, p=p, j=j, N=N, sb_=sb_: e.copy(out=sb_[:, j, 0:N], in_=p[:, 0:N]), reads=[pk], writes=[sk])
            P.dma("sync", zv[:, ob * 4:ob * 4 + nch, t0:t0 + N], sb_[:, 0:nch, 0:N], reads=[sk], writes=["z_out"])
    for ob in range((NOUT + 511) // 512):
        block(ob)
    P.finish([P.lastw["z_out"]])
    P.emit()
    return P.nc


def build_ssd_a2(segs=((0, 256, 0), (260, 8192, 256)), LP=8456, NTS=8448):
    P = Prog()
    xp_d = P.dram("xbc_pre", [1536, LP], F32, kind="ExternalInput")
    cw_d = P.dram("conv_w", [128, 12, 5], F32, kind="ExternalInput")
    cb_d = P.dram("conv_b", [128, 12], F32, kind="ExternalInput")
    dr_d = P.dram("dt_raw", [32, NTS], F32, kind="ExternalInput")
    db_d = P.dram("dt_bias", [32, 1], F32, kind="ExternalInput")
    xo_d = P.dram("xbc_post", [1536, NTS], F32, kind="ExternalOutput")
    dt_d = P.dram("dt", [32, NTS], F32, kind="ExternalOutput")
    cw = P.sb([128, 12, 5], F32, "cw"); cb = P.sb([128, 12], F32, "cb"); db = P.sb([32, 1], F32, "db")
    P.dma("sync", cw[:], cw_d, writes=["cw"]); P.dma("sync", cb[:], cb_d, writes=["cb"]); P.dma("sync", db[:], db_d, writes=["db"])
    BL = 2048
    xin = [P.sb([128, BL + 4], F32, f"xin{i}") for i in range(2)]
    acc = [P.sb([128, BL], F32, f"acc{i}") for i in range(2)]
    dtt = P.sb([32, NTS], F32, "dtt")
    cnt = [0]

    def block(cc, ioff, n, ooff):
        i2 = cnt[0] % 2; cnt[0] += 1
        xi = xin[i2]; xk = f"xin{i2}"; ac = acc[i2]; ak = f"acc{i2}"
        P.dma("sync", xi[:, 0:n + 4], xp_d[cc * 128:(cc + 1) * 128, ioff:ioff + n + 4], writes=[xk])
        P.op("vector", lambda e: e.tensor_scalar(out=ac[:, 0:n], in0=xi[:, 0:n], scalar1=cw[:, cc, 0:1], scalar2=None, op0=ALU.mult), reads=[xk, "cw"], writes=[ak])
        for k in range(1, 5):
            P.op("vector", lambda e, k=k: e.scalar_tensor_tensor(out=ac[:, 0:n], in0=xi[:, k:k + n], scalar=cw[:, cc, k:k + 1], in1=ac[:, 0:n], op0=ALU.mult, op1=ALU.add),
                 reads=[xk, "cw", ak], writes=[ak])
        P.op("scalar", lambda e: e.activation(out=ac[:, 0:n], in_=ac[:, 0:n], func=AF.Silu, bias=cb[:, cc:cc + 1], scale=1.0), reads=[ak, "cb"], writes=[ak])
        P.dma("sync", xo_d[cc * 128:(cc + 1) * 128, ooff:ooff + n], ac[:, 0:n], reads=[ak], writes=["xo_out"])
    for cc in range(12):
        for (ioff, ln, ooff) in segs:
            for b0 in range(0, ln, BL):
                n = min(BL, ln - b0)
                block(cc, ioff + b0, n, ooff + b0)
    P.dma("sync", dtt[:], dr_d, writes=["dtt"])
    P.op("scalar", lambda e: e.activation(out=dtt[:], in_=dtt[:], func=AF.Exp, bias=db[:, 0:1], scale=1.0), reads=["dtt", "db"], writes=["dtt"])
    P.op("scalar", lambda e: e.activation(out=dtt[:], in_=dtt[:], func=AF.Ln, bias=1.0, scale=1.0), reads=["dtt"], writes=["dtt"])
    P.dma("sync", dt_d, dtt[:], reads=["dtt"], writes=["dt_out"])
    P.finish([P.lastw["xo_out"], P.lastw["dt_out"]])
    P.emit()
    return P.nc


def build_ssd_b(qblocks=QB_STD, NT=NT_STD):
    P = Prog()
    C = emit_consts(P)
    xin = P.dram("xin", [D, NT], F32, kind="ExternalInput")
    xout = P.dram("xout", [D, NT], F32, kind="ExternalOutput")
    modv_d = P.dram("modv", [128, 96, 2], F32, kind="ExternalInput")
    y_d = P.dram("yT", [4096, NT], F32, kind="ExternalInput")
    z_d = P.dram("zT", [4096, NT], F32, kind="ExternalInput")
    ng_d = P.dram("norm_g", [128, 32], F32, kind="ExternalInput")
    wo_d = P.dram("w_out", [4096, D], F32, kind="ExternalInput")
    o_scr = P.dram("o_scr", [32, 128, NT], BF16)
    modv = P.sb([128, 96, 2], F32, "modv_sb")
    ng = P.sb([128, 32], F32, "ng")
    P.dma("sync", modv[:], modv_d, writes=["modv"])
    P.dma("sync", ng[:], ng_d, writes=["ng"])
    yv = y_d.rearrange("(c p) t -> p c t", p=128)
    zv = z_d.rearrange("(c p) t -> p c t", p=128)
    P.push_scope()
    ya = P.sb([128, 32, 512], F32, "ya")
    zt = [P.sb([128, 16, 512], F32, f"zt{i}") for i in range(2)]
    sq = P.sb([128, 16, 512], BF16, "sq")
    ob = P.sb([128, 32, 512], BF16, "ob")
    rstd = P.sb([128, 512], F32, "rstd")
    m_ps = P.ps([128, 512], F32, "mps")

    def gate(t0, N, s):
        for hf in range(2):
            cs_ = slice(hf * 16, (hf + 1) * 16)
            z = zt[hf]; zk = f"zt{hf}"
            P.dma("sync", ya[:, cs_, 0:N], yv[:, cs_, t0:t0 + N], writes=[f"ya{hf}"])
            P.dma("sync", z[:, :, 0:N], zv[:, cs_, t0:t0 + N], writes=[zk])
            P.op("scalar", lambda e, z=z: e.activation(out=z[:, :, 0:N], in_=z[:, :, 0:N], func=AF.Silu), reads=[zk], writes=[zk])
            P.op("vector", lambda e, z=z, cs_=cs_: e.tensor_tensor(out=ya[:, cs_, 0:N], in0=ya[:, cs_, 0:N], in1=z[:, :, 0:N], op=ALU.mult), reads=[f"ya{hf}", zk], writes=[f"ya{hf}"])
            P.op("scalar", lambda e, cs_=cs_: e.activation(out=sq[:, :, 0:N], in_=ya[:, cs_, 0:N], func=AF.Square), reads=[f"ya{hf}"], writes=["sq"])
            for c in range(16):
                P.op("tensor", lambda e, c=c, hf=hf: e.matmul(m_ps[:, 0:N], C["ones_b"][:], sq[:, c, 0:N], start=(hf == 0 and c == 0), stop=(hf == 1 and c == 15)),
                     reads=["sq", "ones_b"], writes=["mps"])
        P.op("vector", lambda e: e.tensor_scalar(out=rstd[:, 0:N], in0=m_ps[:, 0:N], scalar1=1.0 / 4096, scalar2=1e-6, op0=ALU.mult, op1=ALU.add), reads=["mps"], writes=["rstd"])
        P.op("scalar", lambda e: e.activation(out=rstd[:, 0:N], in_=rstd[:, 0:N], func=AF.Sqrt), reads=["rstd"], writes=["rstd"])
        P.op("vector", lambda e: e.reciprocal(out=rstd[:, 0:N], in_=rstd[:, 0:N]), reads=["rstd"], writes=["rstd"])
        for hf in range(2):
            cs_ = slice(hf * 16, (hf + 1) * 16)
            P.op("vector", lambda e, cs_=cs_: e.tensor_tensor(out=ya[:, cs_, 0:N], in0=ya[:, cs_, 0:N], in1=rstd[:, 0:N].unsqueeze(1).to_broadcast([128, 16, N]), op=ALU.mult),
                 reads=[f"ya{hf}", "rstd"], writes=[f"ya{hf}"])
            P.op("vector", lambda e, cs_=cs_: e.tensor_tensor(out=ob[:, cs_, 0:N], in0=ya[:, cs_, 0:N], in1=ng[:, cs_].unsqueeze(2).to_broadcast([128, 16, N]), op=ALU.mult),
                 reads=[f"ya{hf}", "ng"], writes=["ob"])
        P.dma("sync", o_scr.rearrange("h p t -> p h t")[:, :, t0:t0 + N], ob[:, :, 0:N], reads=["ob"], writes=["o_scr"])
    for (t0, N, s) in qblocks:
        gate(t0, N, s)
    P.pop_scope()
    P.push_scope()
    wo = [P.sb([128, 32, 128], BF16, f"wo{i}") for i in range(2)]
    ot = [P.sb([128, 32, 512], BF16, f"o{i}") for i in range(2)]
    xt = [P.sb([128, NCH, 512], F32, f"x{i}") for i in range(2)]
    pp = [P.ps([128, 512], F32, f"pp{i}") for i in range(2)]
    xv = xin.rearrange("(c p) t -> p c t", p=128)
    xov = xout.rearrange("(c p) t -> p c t", p=128)
    wov = wo_d.rearrange("(h p) n -> p h n", p=128)
    nb = [0]

    def body(qi, t0, N, s):
        o = ot[qi % 2]; ok = f"o{qi%2}"; x = xt[qi % 2]; xk = f"x{qi%2}"
        P.dma("sync", o[:, :, 0:N], o_scr.rearrange("h p t -> p h t")[:, :, t0:t0 + N], reads=["o_scr"], writes=[ok])
        P.dma("sync", x[:, :, 0:N], xv[:, :, t0:t0 + N], writes=[xk])
        for dc in range(NCH):
            w_ = wo[nb[0] % 2]; wk = f"wo{nb[0]%2}"; p = pp[nb[0] % 2]; pk = f"pp{nb[0]%2}"; nb[0] += 1
            P.dma("gpsimd", w_[:], wov[:, :, dc * 128:(dc + 1) * 128], writes=[wk])
            for h in range(32):
                P.op("tensor", lambda e, h=h, p=p, w_=w_: e.matmul(p[:, 0:N], w_[:, h, :], o[:, h, 0:N], start=(h == 0), stop=(h == 31)), reads=[wk, ok], writes=[pk])
            P.op("vector", lambda e, p=p, dc=dc: e.scalar_tensor_tensor(out=x[:, dc, 0:N], in0=p[:, 0:N], scalar=modv[:, 32 + dc:33 + dc, s], in1=x[:, dc, 0:N],
                                                                     op0=ALU.mult, op1=ALU.add), reads=[pk, "modv", xk], writes=[xk])
        P.dma("sync", xov[:, :, t0:t0 + N], x[:, :, 0:N], reads=[xk], writes=["xout"])
    for qi, (t0, N, s) in enumerate(qblocks):
        body(qi, t0, N, s)
    P.pop_scope()
    P.finish([P.lastw["xout"]])
    P.emit()
    return P.nc


from concourse.bass_utils import run_bass_kernel_spmd

_PROGS = {}
_DEPTH = 4


def _prog(name, fn):
    if name not in _PROGS:
        _PROGS[name] = fn
    return _PROGS[name]()


def _run(nc, in_maps):
    res = run_bass_kernel_spmd(nc, in_maps, core_ids=list(range(8)))
    return res.results


def _rope_tables(pos_r, pos_c, valid):
    n = len(pos_r)
    cosT = np.ones((64, n), np.float32); sinT = np.zeros((64, n), np.float32)
    freqs = (10000.0 ** (-np.arange(16, dtype=np.float32) / 16)).astype(np.float32)
    for f in range(64):
        seg, r = f // 32, f % 32
        i, first = r % 16, r < 16
        pos = pos_r if seg == 0 else pos_c
        ang = pos.astype(np.float32) * freqs[i]
        cosT[f] = np.where(valid, np.cos(ang), 1.0)
        sinT[f] = np.where(valid, (-1.0 if first else 1.0) * np.sin(ang), 0.0)
    return cosT, sinT


_PARTNER = np.array([f + 16 if (f % 32) < 16 else f - 16 for f in range(64)])
_IDENT = np.eye(128, dtype=np.float32)


def _pvec(v):
    return np.ascontiguousarray(np.asarray(v, np.float32).reshape(16, 128).T)


_TRI = np.zeros((2, 128, 128), np.float32)
_jj, _ii = np.meshgrid(np.arange(128), np.arange(128), indexing="ij")
_TRI[0] = (_jj <= _ii); _TRI[1] = (_jj >= _ii)


def ssd_mixer_host(run, xT_cores, mv_cores, gmix, NB, QN, LAT, tiles, qblocks, NT,
                   w_in, conv_w, conv_b, a_log, dt_bias, d_skip, norm_g, w_out):
    f32 = np.float32
    LQ = LAT // QN
    ntc = NB * QN
    nc = build_ssd_a1(tiles, NT)
    w_in = np.ascontiguousarray(np.asarray(w_in, f32))
    oa = run(nc, [{"ident_in": _IDENT, "xin": xT_cores[r], "modv": mv_cores[r], "normg": gmix, "w_in": w_in} for r in range(ntc)], ntc)
    Z = [oa[r]["ZT"] for r in range(ntc)]
    NTS = 256 + LAT
    Zb = []
    for b in range(NB):
        Zb.append(np.concatenate([Z[b * QN][:, LQ:]] + [Z[b * QN + q][:, :LQ] for q in range(QN)], 1))
    LP = 2 + 256 + 2 + 2 + LAT + 2
    segs = ((0, 256, 0), (260, LAT, 256))
    conv_w = np.asarray(conv_w, f32); conv_b = np.asarray(conv_b, f32)
    ims = []
    for b in range(NB):
        for gp in range(4):
            ch = np.concatenate([4096 + np.arange(1024 * gp, 1024 * (gp + 1)), 4096 + 4096 + np.arange(256 * gp, 256 * (gp + 1)),
                                 4096 + 4096 + 1024 + np.arange(256 * gp, 256 * (gp + 1))])
            pre = np.zeros((1536, LP), f32)
            pre[:, 2:258] = Zb[b][ch, :256]
            pre[:, 262:262 + LAT] = Zb[b][ch, 256:]
            cch = ch - 4096
            dtr = np.concatenate([10240 + 16 * gp + np.arange(16), 10240 + 64 + 16 * gp + np.arange(16)])
            dtb = np.concatenate([np.asarray(dt_bias, f32)[0, 16 * gp:16 * gp + 16], np.asarray(dt_bias, f32)[1, 16 * gp:16 * gp + 16]])
            ims.append({"xbc_pre": pre, "conv_w": np.ascontiguousarray(conv_w[:, cch].T.reshape(12, 128, 5).transpose(1, 0, 2)),
                        "conv_b": np.ascontiguousarray(conv_b[cch].reshape(12, 128).T), "dt_raw": np.ascontiguousarray(Zb[b][dtr]),
                        "dt_bias": np.ascontiguousarray(dtb.reshape(32, 1))})
    nc = build_ssd_a2(segs, LP, NTS)
    o2 = run(nc, ims, NB * 4)
    nchk = NTS // 128
    ims = []
    for b in range(NB):
        for gp in range(4):
            xp = o2[b * 4 + gp]["xbc_post"]; dtp = o2[b * 4 + gp]["dt"]
            al = np.concatenate([np.asarray(a_log, f32)[0, 16 * gp:16 * gp + 16], np.asarray(a_log, f32)[1, 16 * gp:16 * gp + 16]])
            ims.append({"ident_in": _IDENT,
                        "x_tm": np.ascontiguousarray(xp[0:1024].T.reshape(nchk, 128, 1024)),
                        "B_tm": np.ascontiguousarray(xp[1024:1280].T.reshape(nchk, 128, 256)),
                        "BT": np.ascontiguousarray(xp[1024:1280].reshape(2, 128, NTS)),
                        "CT": np.ascontiguousarray(xp[1280:1536].reshape(2, 128, NTS)),
                        "dt_tm": np.ascontiguousarray(dtp.T.reshape(nchk, 128, 32)),
                        "alog": np.ascontiguousarray(np.tile(al[None, :], (128, 1))),
                        "Drep": np.ascontiguousarray(np.tile(np.asarray(d_skip, f32)[None, 16 * gp:16 * gp + 16], (128, 1))),
                        "tri": _TRI})
    nc = build_ssd_s(2, LAT // 128)
    o3 = run(nc, ims, NB * 4)
    ng = np.ascontiguousarray(np.asarray(norm_g, f32).reshape(32, 128).T)
    wo = np.ascontiguousarray(np.asarray(w_out, f32))
    ims = []
    for b in range(NB):
        yb = np.concatenate([o3[b * 4 + gp]["y_tm"].reshape(NTS, 1024) for gp in range(4)], 1)
        for q in range(QN):
            r = b * QN + q
            yT = np.ascontiguousarray(np.concatenate([yb[256 + q * LQ:256 + (q + 1) * LQ], yb[:256]], 0).T)
            ims.append({"ident_in": _IDENT, "xin": xT_cores[r], "modv": mv_cores[r], "yT": yT, "zT": np.ascontiguousarray(Z[r][:4096]),
                        "norm_g": ng, "w_out": wo})
    nc = build_ssd_b(qblocks, NT)
    o4 = run(nc, ims, ntc)
    return [o4[r]["xout"] for r in range(ntc)]


def kernel(x, c, ctx, c_ctx, ada_w, ada_b, norm_mix_g, norm_ffn_g, mla_w_a, mla_q_norm_g, mla_kv_norm_g,
           mla_w_uq, mla_w_ukv, mla_w_o, ssd_w_in, ssd_conv_w, ssd_conv_b, ssd_a_log, ssd_dt_bias, ssd_d_skip,
           ssd_norm_g, ssd_w_out, win_w_qkv, win_sinks, win_w_o, peer_w_q, peer_k1, peer_k2, peer_u, peer_v,
           final_norm_g):
    f32 = np.float32
    xs = [np.array(x[b], f32) for b in range(2)]
    cs = [np.array(ctx[b], f32) for b in range(2)]
    c = np.asarray(c, f32); c_ctx = np.asarray(c_ctx, f32)
    cores = [(r // 4, r % 4) for r in range(8)]

    def xT_core(r):
        b, q = cores[r]
        return np.ascontiguousarray(np.concatenate([xs[b][q * 2048:(q + 1) * 2048], cs[b]], 0).T)

    def absorb(outs):
        for r, (b, q) in enumerate(cores):
            o = outs[r]["xout"]
            xs[b][q * 2048:(q + 1) * 2048] = o[:, :2048].T
            if q == 0:
                cs[b] = np.ascontiguousarray(o[:, 2048:].T)

    nc = build_ada()
    ims = []
    for r, (b, q) in enumerate(cores):
        cv = np.stack([c[b], c_ctx], 1).reshape(16, 128, 2).transpose(1, 0, 2)
        ims.append({"cv": np.ascontiguousarray(cv),
                    "ada_w": np.ascontiguousarray(np.asarray(ada_w, f32)[:, :, q * 3072:(q + 1) * 3072]),
                    "ada_b": np.ascontiguousarray(np.asarray(ada_b, f32)[:, q * 3072:(q + 1) * 3072].reshape(4, 24, 128).transpose(0, 2, 1))})
    outs = _run(nc, ims)
    modv = []
    for b in range(2):
        modv.append(np.concatenate([outs[b * 4 + q]["modv_out"] for q in range(4)], 2))

    pos_lat = [np.arange(q * 2048, (q + 1) * 2048) for q in range(4)]
    valid = np.array([True] * 2048 + [False] * 256)
    ropes = []
    for q in range(4):
        p = np.concatenate([pos_lat[q], np.zeros(256, np.int64)])
        ropes.append(_rope_tables(p // 64, p % 64, valid))

    kinds18 = [0] * 16 + [1] * 2
    n_mla = n_ssd = n_win = 0
    for i in range(_DEPTH):
        kind = i % 3
        mv = [np.ascontiguousarray(modv[b][i]) for b in range(2)]
        gmix = _pvec(norm_mix_g[i])
        if kind == 0:
            j = i // 3
            w_a = np.asarray(mla_w_a[j], f32)
            wa_ext = np.ascontiguousarray(np.concatenate([w_a, w_a[:, 1024 + _PARTNER]], 1))
            qkvg = np.ascontiguousarray(np.concatenate([np.asarray(mla_q_norm_g[j], f32), np.asarray(mla_kv_norm_g[j], f32)]).reshape(8, 128).T)
            nc = build_mla_a()
            ims = [{"ident_in": _IDENT, "xin": xT_core(r), "modv": mv[cores[r][0]], "normg": gmix, "w_a": wa_ext, "qkvg": qkvg,
                    "cosT": ropes[cores[r][1]][0], "sinT": ropes[cores[r][1]][1]} for r in range(8)]
            oa = _run(nc, ims)
            ckv_all, kr_all = [], []
            for b in range(2):
                ckv_all.append(np.ascontiguousarray(np.concatenate([oa[b * 4 + q]["cnT"][512:, :2048] for q in range(4)] + [oa[b * 4]["cnT"][512:, 2048:]], 1)))
                kr_all.append(np.ascontiguousarray(np.concatenate([oa[b * 4 + q]["krT"][:, :2048] for q in range(4)] + [oa[b * 4]["krT"][:, 2048:]], 1)))
            wuq_h = np.asarray(mla_w_uq[j], f32).reshape(512, 16, 192).transpose(1, 0, 2)
            wuq_ext = np.ascontiguousarray(np.concatenate([wuq_h, wuq_h[:, :, 128 + _PARTNER]], 2))
            wukv_h = np.ascontiguousarray(np.asarray(mla_w_ukv[j], f32).reshape(512, 16, 256).transpose(1, 0, 2))
            wo = np.ascontiguousarray(np.asarray(mla_w_o[j], f32))
            nc = build_mla_b()
            ims = [{"ident_in": _IDENT, "xin": xT_core(r), "modv": mv[cores[r][0]], "cqT": np.ascontiguousarray(oa[r]["cnT"][:512]),
                    "ckvT": ckv_all[cores[r][0]], "krT": kr_all[cores[r][0]], "cosT": ropes[cores[r][1]][0], "sinT": ropes[cores[r][1]][1],
                    "w_uq": wuq_ext, "w_ukv": wukv_h, "w_o": wo} for r in range(8)]
            absorb(_run(nc, ims))
        elif kind == 1:
            j = i // 3
            run3 = lambda nc_, ims_, n_: run_bass_kernel_spmd(nc_, ims_, core_ids=list(range(n_))).results
            xo = ssd_mixer_host(run3, [xT_core(r) for r in range(8)], [mv[cores[r][0]] for r in range(8)], gmix, 2, 4, 8192,
                                TILES_STD, QB_STD, NT_STD, ssd_w_in[j], ssd_conv_w[j], ssd_conv_b[j], ssd_a_log[j], ssd_dt_bias[j],
                                ssd_d_skip[j], ssd_norm_g[j], ssd_w_out[j])
            absorb([{"xout": o_} for o_ in xo])
        else:
            j = i // 3
            w_qkv = np.asarray(win_w_qkv[j], f32)
            wq, wk, wv = w_qkv[:, :2048], w_qkv[:, 2048:2560], w_qkv[:, 2560:]

            def cws(w, nh):
                ws = w.reshape(2048, nh, 64)[:, :, _PARTNER].reshape(2048, nh * 64)
                return np.stack([w.reshape(2048, -1, 128), ws.reshape(2048, -1, 128)], 2).reshape(2048, -1, 256)
            wqk = np.ascontiguousarray(np.concatenate([cws(wq, 32), cws(wk, 8)], 1))
            nc = build_win_a()
            ims = [{"ident_in": _IDENT, "xin": xT_core(r), "modv": mv[cores[r][0]], "normg": gmix, "w_qk": wqk, "w_v": np.ascontiguousarray(wv),
                    "cos2": np.ascontiguousarray(np.concatenate([ropes[cores[r][1]][0]] * 2, 0)),
                    "sin2": np.ascontiguousarray(np.concatenate([ropes[cores[r][1]][1]] * 2, 0))} for r in range(8)]
            oa = _run(nc, ims)
            wo = np.ascontiguousarray(np.asarray(win_w_o[j], f32))
            sk = np.ascontiguousarray(np.tile(np.asarray(win_sinks[j], f32)[None, :], (64, 1)))
            ims = []
            for r, (b, q) in enumerate(cores):
                KT = oa[r]["QKT"][2048:]; V = oa[r]["V"]
                if q > 0:
                    kp, vp = oa[r - 1]["QKT"][2048:, 1920:2048], oa[r - 1]["V"][1920:2048]
                else:
                    kp, vp = np.zeros((512, 128), f32), np.zeros((128, 512), f32)
                if q < 3:
                    kn, vn = oa[r + 1]["QKT"][2048:, 0:128], oa[r + 1]["V"][0:128]
                else:
                    kn, vn = np.zeros((512, 128), f32), np.zeros((128, 512), f32)
                KT_ext = np.ascontiguousarray(np.concatenate([kp, KT[:, :2048], kn, KT[:, 2048:]], 1))
                V_ext = np.ascontiguousarray(np.concatenate([vp, V[:2048], vn, V[2048:]], 0))
                masks = np.zeros((5, 6, 128, 512), f32)
                for bq in range(4):
                    qp = 512 * bq + np.arange(512)
                    for jj in range(6):
                        e_ = 4 * bq + jj
                        if (e_ == 0 and q == 0) or (e_ == 17 and q == 3):
                            continue
                        kpz = (e_ - 1) * 128 + np.arange(128)
                        masks[bq, jj] = (np.abs(kpz[:, None] - qp[None, :]) <= 128)
                ims.append({"ident_in": _IDENT, "xin": xT_core(r), "modv": mv[b], "QT": np.ascontiguousarray(oa[r]["QKT"][:2048]),
                            "KT": KT_ext, "V": V_ext, "masks": masks, "sinks": sk, "w_o": wo})
            nc = build_win_b()
            absorb(_run(nc, ims))
        nc = build_peer(kinds18, 2304)
        uT = np.ascontiguousarray(np.asarray(peer_u[i], f32).T)
        vv = np.ascontiguousarray(np.asarray(peer_v[i], f32))
        wq_ = np.ascontiguousarray(np.asarray(peer_w_q[i], f32))
        k1T = np.ascontiguousarray(np.asarray(peer_k1[i], f32).transpose(0, 2, 1))
        k2T = np.ascontiguousarray(np.asarray(peer_k2[i], f32).transpose(0, 2, 1))
        gffn = _pvec(norm_ffn_g[i])
        ims = [{"ident_in": _IDENT, "xin": xT_core(r), "modv": mv[cores[r][0]], "normg": gffn, "wq": wq_, "k1T": k1T, "k2T": k2T,
                "uT": uT, "v": vv} for r in range(8)]
        absorb(_run(nc, ims))
    nc = build_final()
    gf = _pvec(final_norm_g)
    ims = [{"ident_in": _IDENT, "xin": np.ascontiguousarray(xs[b][q * 2048:(q + 1) * 2048].T), "normg": gf} for (b, q) in cores]
    outs = _run(nc, ims)
    out = np.zeros((2, 8192, 2048), f32)
    for r, (b, q) in enumerate(cores):
        out[b, q * 2048:(q + 1) * 2048] = outs[r]["xout"].T
    return out
```
